# Optimizing a Trainium2 kernel written in Bass

```python
import math
import jax, jax.numpy as jnp
from jax import lax
import numpy as np

D_MODEL = 1024
BATCH = 32
SEQ = 2048
DEPTH = 1

MEM_LEN = 256
CHUNK = 128
RET_HEADS = 4
RET_DK = 64
RET_DV = 128
ML_HEADS = 4
ML_DK = 128
ML_DV = 128
CONV_W = 4
XA_HEADS = 4
XA_DH = D_MODEL // XA_HEADS
N_GROUPS = 4
EXP_PER_GROUP = 8
N_EXPERTS = N_GROUPS * EXP_PER_GROUP
TOP_K = 2
D_EXPERT = 512
MOE_BLOCK = 128
ROPE_BASE = 10000.0
EPS = 1e-6

RET_QK = RET_HEADS * RET_DK
RET_V = RET_HEADS * RET_DV
ML_QK = ML_HEADS * ML_DK
ML_V = ML_HEADS * ML_DV
MIX_WIDTH = RET_V + ML_V
SPLITS = (RET_QK, RET_QK, RET_V, RET_V, 2 * ML_QK, ML_V, ML_V, 2 * ML_HEADS)
IN_WIDTH = sum(SPLITS)

kernel_name = "hymba_retnet_mlstm_xattn_hmoe"


def rmsnorm(x, w):
    xf = x.astype(jnp.float32)
    y = xf * lax.rsqrt(jnp.mean(xf * xf, axis=-1, keepdims=True) + EPS)
    return (y * w.astype(jnp.float32)).astype(x.dtype)


def head_norm(h, w):
    mu = jnp.mean(h, axis=-1, keepdims=True)
    var = jnp.mean(jnp.square(h - mu), axis=-1, keepdims=True)
    y = (h - mu) * lax.rsqrt(var + EPS)
    B, S, H, d = h.shape
    return y.reshape(B, S, H * d) * w.astype(jnp.float32)


def rope(x, pos):
    half = x.shape[-1] // 2
    inv = ROPE_BASE ** (-jnp.arange(half, dtype=jnp.float32) / half)
    ang = pos.astype(jnp.float32)[:, None] * inv[None, :]
    cos = jnp.cos(ang)[None, :, None, :]
    sin = jnp.sin(ang)[None, :, None, :]
    x1, x2 = x[..., :half], x[..., half:]
    return jnp.concatenate([x1 * cos - x2 * sin, x1 * sin + x2 * cos], axis=-1)


def causal_conv(x, w, b):
    C = x.shape[-1]
    y = lax.conv_general_dilated(x, w[:, None, :].astype(x.dtype), window_strides=(1,),
                                 padding=[(CONV_W - 1, 0)],
                                 dimension_numbers=("NWC", "WIO", "NWC"),
                                 feature_group_count=C)
    return y + b.astype(x.dtype)


def retention(q, k, v):
    B, S, H, dk = q.shape
    dv = v.shape[-1]
    L = CHUNK
    nc = S // L
    log_g = jnp.log1p(-jnp.exp2(-5.0 - jnp.arange(H, dtype=jnp.float32)))
    q = q.reshape(B, nc, L, H, dk)
    k = k.reshape(B, nc, L, H, dk)
    v = v.reshape(B, nc, L, H, dv)
    n = jnp.arange(L, dtype=jnp.float32)
    diff = n[:, None] - n[None, :]
    dmat = jnp.where(diff >= 0, jnp.exp(log_g[:, None, None] * jnp.maximum(diff, 0.0)[None]), 0.0)
    scores = jnp.einsum('bclhd,bcmhd->bchlm', q, k) * dmat
    inner = jnp.einsum('bchlm,bcmhe->bclhe', scores, v)
    k_dec = k * jnp.exp((L - 1 - n)[:, None] * log_g[None, :])[:, :, None]
    kv = jnp.einsum('bclhd,bclhe->cbhde', k_dec, v)
    chunk_decay = jnp.exp(L * log_g)[None, :, None, None]

    def step(R, kv_c):
        return chunk_decay * R + kv_c, R

    _, R_prev = lax.scan(step, jnp.zeros((B, H, dk, dv), jnp.float32), kv)
    q_dec = q * jnp.exp((n + 1)[:, None] * log_g[None, :])[:, :, None]
    cross = jnp.einsum('bclhd,cbhde->bclhe', q_dec, R_prev)
    return (inner + cross).reshape(B, S, H, dv)


def mlstm(q, k, v, i_pre, f_pre):
    B, S, H, dk = q.shape
    dv = v.shape[-1]
    L = CHUNK
    nc = S // L

    def chunked(t):
        return t.reshape(B, nc, L, H, -1).transpose(0, 1, 3, 2, 4)

    q = chunked(q)
    k = chunked(k) * (dk ** -0.5)
    v = chunked(v)
    logf = jax.nn.log_sigmoid(f_pre).reshape(B, nc, L, H).transpose(0, 1, 3, 2)
    ig = i_pre.reshape(B, nc, L, H).transpose(0, 1, 3, 2)
    b = jnp.cumsum(logf, axis=-1)
    b_tot = b[..., -1]
    causal = jnp.tril(jnp.ones((L, L), bool))
    logD = jnp.where(causal, b[..., :, None] - b[..., None, :] + ig[..., None, :], -jnp.inf)
    m_intra = jnp.max(logD, axis=-1)
    a = b_tot[..., None] - b + ig
    m_loc = jnp.max(a, axis=-1)
    wa = jnp.exp(a - m_loc[..., None])
    kw = k * wa[..., None]
    kv_loc = jnp.einsum('bchld,bchle->cbhde', kw, v)
    n_loc = jnp.moveaxis(jnp.sum(kw, axis=3), 1, 0)

    def step(carry, inp):
        C, nvec, m = carry
        g, ml, kvc, nlc = inp
        m_new = jnp.maximum(g + m, ml)
        s_old = jnp.exp(g + m - m_new)
        s_loc = jnp.exp(ml - m_new)
        C_new = s_old[..., None, None] * C + s_loc[..., None, None] * kvc
        n_new = s_old[..., None] * nvec + s_loc[..., None] * nlc
        return (C_new, n_new, m_new), (C, nvec, m)

    init = (jnp.zeros((B, H, dk, dv), jnp.float32), jnp.zeros((B, H, dk), jnp.float32),
            jnp.zeros((B, H), jnp.float32))
    _, (C_prev, n_prev, m_prev) = lax.scan(
        step, init, (jnp.moveaxis(b_tot, 1, 0), jnp.moveaxis(m_loc, 1, 0), kv_loc, n_loc))
    m_prev = jnp.moveaxis(m_prev, 0, 1)
    m_inter = b + m_prev[..., None]
    m_t = jnp.maximum(m_intra, m_inter)
    s = jnp.einsum('bchtd,bchsd->bchts', q, k) * jnp.exp(logD - m_t[..., None])
    inter = jnp.exp(m_inter - m_t)
    num = jnp.einsum('bchts,bchse->bchte', s, v) + inter[..., None] * jnp.einsum('bchtd,cbhde->bchte', q, C_prev)
    den = jnp.sum(s, axis=-1) + inter * jnp.einsum('bchtd,cbhd->bcht', q, n_prev)
    h = num / jnp.maximum(jnp.abs(den), jnp.exp(-m_t))[..., None]
    return h.transpose(0, 1, 3, 2, 4).reshape(B, S, H, dv)


def hybrid_mixer(h, w_in, ret_norm_w, ml_conv_w, ml_conv_b, ml_gate_b, ml_norm_w, w_out):
    B, S, _ = h.shape
    proj = h @ w_in
    r_q, r_k, r_v, r_g, m_qk, m_v, m_o, m_if = jnp.split(proj, [int(c) for c in np.cumsum(SPLITS)[:-1]], axis=-1)
    pos = jnp.arange(S)
    rq = rope(r_q.reshape(B, S, RET_HEADS, RET_DK).astype(jnp.float32), pos)
    rk = rope(r_k.reshape(B, S, RET_HEADS, RET_DK).astype(jnp.float32), pos) * (RET_DK ** -0.5)
    rv = r_v.reshape(B, S, RET_HEADS, RET_DV).astype(jnp.float32)
    ret = head_norm(retention(rq, rk, rv), ret_norm_w) * jax.nn.silu(r_g.astype(jnp.float32))
    qk = jax.nn.silu(causal_conv(m_qk, ml_conv_w, ml_conv_b)).astype(jnp.float32)
    mq = qk[..., :ML_QK].reshape(B, S, ML_HEADS, ML_DK)
    mk = qk[..., ML_QK:].reshape(B, S, ML_HEADS, ML_DK)
    mv = m_v.reshape(B, S, ML_HEADS, ML_DV).astype(jnp.float32)
    gates = m_if.astype(jnp.float32) + ml_gate_b.astype(jnp.float32)
    hm = mlstm(mq, mk, mv, gates[..., :ML_HEADS], gates[..., ML_HEADS:])
    ml = jax.nn.sigmoid(m_o.astype(jnp.float32)) * head_norm(hm, ml_norm_w)
    return jnp.concatenate([ret, ml], axis=-1).astype(h.dtype) @ w_out


def mem_cross_attention(h, mem_n, wq, wkv, wo):
    B, S, D = h.shape
    M = mem_n.shape[1]
    q = (h @ wq).reshape(B, S, XA_HEADS, XA_DH)
    kv = mem_n @ wkv
    k = kv[..., :D].reshape(B, M, XA_HEADS, XA_DH)
    v = kv[..., D:].reshape(B, M, XA_HEADS, XA_DH)
    logits = jnp.einsum('bshd,bmhd->bhsm', q, k).astype(jnp.float32) * (XA_DH ** -0.5)
    p = jax.nn.softmax(logits, axis=-1).astype(h.dtype)
    o = jnp.einsum('bhsm,bmhd->bshd', p, v).reshape(B, S, D)
    return o @ wo


def hier_moe(h, w_group, b_group, w_router, b_router, w_gate, w_up, w_down):
    B, S, D = h.shape
    N = B * S
    xt = h.reshape(N, D)
    xf = xt.astype(jnp.float32)
    g_logits = xf @ w_group.astype(jnp.float32) + b_group.astype(jnp.float32)
    g_prob = jax.nn.softmax(g_logits, axis=-1)
    g_sel = jnp.argmax(g_logits, axis=-1)
    g_w = jnp.take_along_axis(g_prob, g_sel[:, None], axis=-1)[:, 0]
    e_logits = (xf @ w_router.astype(jnp.float32) + b_router.astype(jnp.float32)).reshape(N, N_GROUPS, EXP_PER_GROUP)
    e_logits = jnp.take_along_axis(e_logits, g_sel[:, None, None], axis=1)[:, 0]
    e_prob = jax.nn.softmax(e_logits, axis=-1)
    top_p, top_i = lax.top_k(e_prob, TOP_K)
    top_p = top_p / jnp.sum(top_p, axis=-1, keepdims=True)
    weights = g_w[:, None] * top_p
    expert = g_sel[:, None] * EXP_PER_GROUP + top_i
    NK = N * TOP_K
    flat_e = expert.reshape(NK).astype(jnp.int32)
    flat_tok = jnp.repeat(jnp.arange(N, dtype=jnp.int32), TOP_K)
    order = jnp.argsort(flat_e)
    sorted_e = flat_e[order]
    counts = jnp.zeros((N_EXPERTS,), jnp.int32).at[flat_e].add(1)
    starts = jnp.cumsum(counts) - counts
    padded = (counts + MOE_BLOCK - 1) // MOE_BLOCK * MOE_BLOCK
    pstarts = jnp.cumsum(padded) - padded
    pends = pstarts + padded
    dest_sorted = pstarts[sorted_e] + (jnp.arange(NK, dtype=jnp.int32) - starts[sorted_e])
    cap = NK + N_EXPERTS * MOE_BLOCK
    nblk = cap // MOE_BLOCK
    row_tok = jnp.full((cap,), N, jnp.int32).at[dest_sorted].set(flat_tok[order])
    blk_start = jnp.arange(nblk, dtype=jnp.int32) * MOE_BLOCK
    blk_e = jnp.minimum(jnp.sum(blk_start[:, None] >= pends[None, :], axis=1), N_EXPERTS - 1).astype(jnp.int32)
    x_pad = jnp.concatenate([xt, jnp.zeros((1, D), xt.dtype)], axis=0)
    xs = x_pad[row_tok].reshape(nblk, MOE_BLOCK, D)

    def expert_block(args):
        xb, e = args
        hid = jax.nn.silu(xb @ w_gate[e]) * (xb @ w_up[e])
        return hid @ w_down[e]

    ys = lax.map(expert_block, (xs, blk_e)).reshape(cap, D)
    dest = jnp.zeros((NK,), jnp.int32).at[order].set(dest_sorted)
    y = ys[dest].reshape(N, TOP_K, D)
    y = jnp.sum(y * weights[..., None].astype(y.dtype), axis=1)
    return y.reshape(B, S, D)


def setup_inputs(seed: int = 0) -> dict:
    key = jax.random.key(seed)
    ks = jax.random.split(key, 24)
    f32 = jnp.float32

    def nrm(k, shape, scale):
        return jax.random.normal(k, shape, f32) * scale

    def gain(k, shape):
        return 1.0 + 0.05 * jax.random.normal(k, shape, f32)

    Dp = DEPTH
    i_bias = 0.1 * jax.random.normal(ks[6], (Dp, ML_HEADS), f32)
    f_bias = jnp.linspace(3.0, 6.0, ML_HEADS, dtype=f32)[None] + 0.1 * jax.random.normal(ks[7], (Dp, ML_HEADS), f32)
    return {
        "x": jax.random.normal(ks[0], (BATCH, SEQ, D_MODEL), f32),
        "mem": jax.random.normal(ks[1], (BATCH, MEM_LEN, D_MODEL), f32),
        "norm_mix_w": gain(ks[2], (Dp, D_MODEL)),
        "w_in": nrm(ks[3], (Dp, D_MODEL, IN_WIDTH), D_MODEL ** -0.5),
        "ret_norm_w": gain(ks[4], (Dp, RET_V)),
        "ml_conv_w": nrm(ks[5], (Dp, CONV_W, 2 * ML_QK), CONV_W ** -0.5),
        "ml_conv_b": nrm(ks[8], (Dp, 2 * ML_QK), 0.01),
        "ml_gate_b": jnp.concatenate([i_bias, f_bias], axis=-1),
        "ml_norm_w": gain(ks[9], (Dp, ML_V)),
        "w_out": nrm(ks[10], (Dp, MIX_WIDTH, D_MODEL), MIX_WIDTH ** -0.5),
        "norm_xa_w": gain(ks[11], (Dp, D_MODEL)),
        "norm_mem_w": gain(ks[12], (Dp, D_MODEL)),
        "xa_wq": nrm(ks[13], (Dp, D_MODEL, D_MODEL), D_MODEL ** -0.5),
        "xa_wkv": nrm(ks[14], (Dp, D_MODEL, 2 * D_MODEL), D_MODEL ** -0.5),
        "xa_wo": nrm(ks[15], (Dp, D_MODEL, D_MODEL), D_MODEL ** -0.5),
        "norm_moe_w": gain(ks[16], (Dp, D_MODEL)),
        "moe_w_group": nrm(ks[17], (Dp, D_MODEL, N_GROUPS), D_MODEL ** -0.5),
        "moe_b_group": nrm(ks[18], (Dp, N_GROUPS), 0.01),
        "moe_w_router": nrm(ks[19], (Dp, D_MODEL, N_EXPERTS), D_MODEL ** -0.5),
        "moe_b_router": nrm(ks[20], (Dp, N_EXPERTS), 0.01),
        "moe_w_gate": nrm(ks[21], (Dp, N_EXPERTS, D_MODEL, D_EXPERT), D_MODEL ** -0.5),
        "moe_w_up": nrm(ks[22], (Dp, N_EXPERTS, D_MODEL, D_EXPERT), D_MODEL ** -0.5),
        "moe_w_down": nrm(ks[23], (Dp, N_EXPERTS, D_EXPERT, D_MODEL), D_EXPERT ** -0.5),
        "norm_final_w": gain(jax.random.fold_in(key, 99), (D_MODEL,)),
    }


def reference(x, mem, norm_mix_w, w_in, ret_norm_w, ml_conv_w, ml_conv_b, ml_gate_b, ml_norm_w, w_out,
              norm_xa_w, norm_mem_w, xa_wq, xa_wkv, xa_wo, norm_moe_w, moe_w_group, moe_b_group,
              moe_w_router, moe_b_router, moe_w_gate, moe_w_up, moe_w_down, norm_final_w):
    for l in range(DEPTH):
        h = rmsnorm(x, norm_mix_w[l])
        x = x + hybrid_mixer(h, w_in[l], ret_norm_w[l], ml_conv_w[l], ml_conv_b[l], ml_gate_b[l],
                             ml_norm_w[l], w_out[l])
        h = rmsnorm(x, norm_xa_w[l])
        x = x + mem_cross_attention(h, rmsnorm(mem, norm_mem_w[l]), xa_wq[l], xa_wkv[l], xa_wo[l])
        h = rmsnorm(x, norm_moe_w[l])
        x = x + hier_moe(h, moe_w_group[l], moe_b_group[l], moe_w_router[l], moe_b_router[l],
                         moe_w_gate[l], moe_w_up[l], moe_w_down[l])
    return rmsnorm(x, norm_final_w)
```

```python
from contextlib import ExitStack
import numpy as np
import concourse.bass as bass
import concourse.mybir as mybir
from concourse.bass_utils import run_bass_kernel_spmd

F32 = mybir.dt.float32
BF16 = mybir.dt.bfloat16
I32 = mybir.dt.int32
U8 = mybir.dt.uint8
ALU = mybir.AluOpType
AF = mybir.ActivationFunctionType
AX = mybir.AxisListType

ENGS = ("pe", "act", "dve", "pool", "sp")
D = 1024
S = 2048
CAP = 768
NEXP = 32
EPS = 1e-6


class _Op:
    __slots__ = ("eng", "fn", "deps", "is_dma", "chan", "sig", "val", "waits", "bar", "f32", "chain")


class Prog:
    def __init__(self, nc):
        self.nc = nc
        self.ops = []
        self.last_w = {}
        self.readers = {}
        self._rec = None
        self._stack = []
        self.keymap = None

    def record(self):
        self._stack.append(self._rec)
        self._rec = []

    def stop(self):
        r = self._rec
        self._rec = self._stack.pop()
        return r

    def replay(self, items):
        for it in items:
            self._add(*it)

    @staticmethod
    def _block_heads(l):
        heads = []
        for j, it in enumerate(l):
            if j > 0 and it[0] == "pe" and l[j - 1][0] == "pe" and it[3] and l[j - 1][3] and it[3][0] == l[j - 1][3][0]:
                heads.append(heads[-1])
            else:
                heads.append(j)
        return heads

    def merge_list(self, lists):
        allops = []
        for li, l in enumerate(lists):
            n = max(len(l), 1)
            hd = self._block_heads(l)
            for j, it in enumerate(l):
                allops.append(((hd[j] + 0.5) / n, li, j, it))
        allops.sort(key=lambda t: (t[0], t[1], t[2]))
        return [t[3] for t in allops]

    def merge(self, lists):
        allops = []
        for li, l in enumerate(lists):
            n = max(len(l), 1)
            hd = self._block_heads(l)
            for j, it in enumerate(l):
                allops.append(((hd[j] + 0.5) / n, li, j, it))
        allops.sort(key=lambda t: (t[0], t[1], t[2]))
        self.replay([t[3] for t in allops])

    def schedule(self, lists, win=3):
        from collections import defaultdict
        COST = {"pe": 0.16, "act": 0.45, "dve": 0.35, "pool": 0.9, "sp": 0.05}

        class _Rec:
            def __getattr__(self, name):
                def f(*a, **kw):
                    o = kw.get("out", a[0] if a else None)
                    try:
                        sz = 1
                        for d in list(o.shape)[1:]:
                            sz *= int(d)
                    except Exception:
                        sz = 256
                    return (name, sz)
                return f

        _rec = _Rec()
        cache = {}

        def opcost(it):
            if it[4]:
                return 2.5
            key = id(it[1])
            if key in cache:
                return cache[key]
            try:
                name, sz = it[1](_rec)
            except Exception:
                name, sz = "x", 256
            eng = it[0]
            if eng == "pe":
                c = 0.1 if name == "transpose" else 0.07 + sz / 1800.0
            elif eng == "act":
                c = 0.25 + sz / 2200.0
            elif eng == "dve":
                c = 0.15 + sz / 900.0
            elif eng == "pool":
                c = 0.25 + sz / 850.0
            else:
                c = 0.3
            cache[key] = c
            return c
        lists = [[(it[0], it[1], self._expand(it[2]), self._expand(it[3]), it[4], it[5]) for it in l] for l in lists]
        written = set()
        for l in lists:
            for it in l:
                written.update(it[3])
        cnt = defaultdict(lambda: defaultdict(int))
        opkeys = []
        for t, l in enumerate(lists):
            ok = []
            for it in l:
                ks = [k for k in set(it[2]) | set(it[3]) if k in written]
                ok.append(ks)
                for k in ks:
                    cnt[k][t] += 1
            opkeys.append(ok)
        tiles_of = {k: sorted(d) for k, d in cnt.items()}
        fptr = {k: 0 for k in cnt}

        def front(k):
            arr = tiles_of[k]
            p = fptr[k]
            while p < len(arr) and cnt[k][arr[p]] == 0:
                p += 1
            fptr[k] = p
            return arr[p] if p < len(arr) else 1 << 30

        heads = [self._block_heads(l) for l in lists]
        pos = [0] * len(lists)
        eng_free = defaultdict(float)
        kw = defaultdict(float)
        kr = defaultdict(float)
        order = []
        lo = 0
        n = len(lists)
        while lo < n:
            while lo < n and pos[lo] >= len(lists[lo]):
                lo += 1
            if lo >= n:
                break
            best = None
            for t in range(lo, min(n, lo + win)):
                if pos[t] >= len(lists[t]):
                    continue
                j = pos[t]
                blocked = False
                jj = j
                while True:
                    if any(front(k) < t for k in opkeys[t][jj]):
                        blocked = True
                        break
                    jj += 1
                    if jj >= len(lists[t]) or heads[t][jj] != heads[t][j]:
                        break
                if blocked:
                    continue
                it = lists[t][j]
                st_ = eng_free[it[0]]
                for r in it[2]:
                    st_ = max(st_, kw[r])
                for w in it[3]:
                    st_ = max(st_, kw[w], kr[w])
                if best is None or st_ < best[0]:
                    best = (st_, t)
            t = best[1]
            j = pos[t]
            hd = heads[t][j]
            while True:
                it = lists[t][j]
                eng = it[0]
                st_ = eng_free[eng]
                for r in it[2]:
                    st_ = max(st_, kw[r])
                for w in it[3]:
                    st_ = max(st_, kw[w], kr[w])
                if it[4]:
                    eng_free[eng] = st_ + 0.05
                    fin = st_ + 2.5
                else:
                    fin = st_ + opcost(it)
                    eng_free[eng] = fin
                for r in it[2]:
                    kr[r] = max(kr[r], fin + 0.1)
                for w in it[3]:
                    kw[w] = fin + 0.1
                    kr[w] = 0.0
                for k in opkeys[t][j]:
                    cnt[k][t] -= 1
                order.append(it)
                j += 1
                if j >= len(lists[t]) or heads[t][j] != hd:
                    break
            pos[t] = j
        self.replay(order)

    def pipeline(self, lists, frac=0.5):
        allops = []
        L = max(len(l) for l in lists)
        stride = L * frac
        for t, l in enumerate(lists):
            hd = self._block_heads(l)
            for j, it in enumerate(l):
                allops.append((t * stride + hd[j], t, j, it))
        allops.sort(key=lambda t: (t[0], t[1], t[2]))
        self.replay([t[3] for t in allops])

    def _add(self, eng, fn, reads, writes, is_dma=False, chan=None):
        if self.keymap is not None:
            ks, sfx = self.keymap
            reads = [k + sfx if k in ks else k for k in reads]
            writes = [k + sfx if k in ks else k for k in writes]
            if chan is not None and chan != "f32" and not chan.startswith("chain") and chan[2:] in ks:
                chan = chan + sfx
        if self._rec is not None:
            km, self.keymap = self.keymap, None
            self._rec.append((eng, fn, reads, writes, is_dma, chan))
            self.keymap = km
            return None
        km, self.keymap = self.keymap, None
        try:
            return self._add2(eng, fn, reads, writes, is_dma, chan)
        finally:
            self.keymap = km

    @staticmethod
    def _expand(keys):
        out = []
        for k in keys:
            if len(k) == 3 and k[:2] == "pp" and k[2].isdigit():
                out.append(k + "a")
                out.append(k + "b")
            else:
                out.append(k)
        return out

    def _add2(self, eng, fn, reads, writes, is_dma=False, chan=None):
        reads = self._expand(reads)
        writes = self._expand(writes)
        o = _Op()
        o.eng, o.fn, o.is_dma, o.chan = eng, fn, is_dma, chan
        o.f32 = (not is_dma) and chan == "f32"
        o.chain = chan[5:] if ((not is_dma) and chan is not None and chan.startswith("chain")) else ""
        if o.f32 and eng == "pe":
            o.chain = "A"
        o.sig = False
        o.val = None
        if eng == "pe" and not is_dma:
            lp = getattr(self, "_last_pe", None)
            self._last_pe = o
        else:
            lp = None
        deps = []
        for k in reads:
            w = self.last_w.get(k)
            if w is not None:
                deps.append(w)
        keep_readers = set()
        for k in writes:
            w = self.last_w.get(k)
            if w is not None:
                if is_dma and w.is_dma and w.chan == chan:
                    keep_readers.add(k)
                else:
                    deps.append(w)
            deps.extend(self.readers.get(k, ()))
        seen = set()
        o.deps = []
        for d in deps:
            if d is o or id(d) in seen:
                continue
            if o.chain and getattr(d, "chain", "") == o.chain:
                continue
            seen.add(id(d))
            o.deps.append(d)
        for k in writes:
            self.last_w[k] = o
            if k not in keep_readers:
                self.readers[k] = []
        for k in reads:
            if k not in writes:
                self.readers.setdefault(k, []).append(o)
        self.ops.append(o)
        return o

    def op(self, eng, fn, reads=(), writes=(), f32=False, chain=False):
        if chain is True:
            chain = "A"
        return self._add(eng, fn, list(reads), list(writes), False, "f32" if f32 else (("chain" + chain) if chain else None))

    def dma(self, eng, out, in_, reads=(), writes=(), chan=None, **kw):
        reads, writes = list(reads), list(writes)
        if chan is None:
            chan = "c:" + (writes[0] if writes else reads[0])
        nc = self.nc
        q = {"pool": nc.gpsimd, "sp": nc.sync, "act": nc.scalar}[eng]
        fn = lambda e: q.dma_start(out=out, in_=in_, **kw)
        return self._add(eng, fn, reads, writes, is_dma=True, chan=chan)

    def dma_fn(self, eng, fn, reads=(), writes=(), chan=None):
        reads, writes = list(reads), list(writes)
        if chan is None:
            chan = "c:" + (writes[0] if writes else reads[0])
        return self._add(eng, fn, reads, writes, is_dma=True, chan=chan)

    def wait_for(self, eng, keys):
        return self._add(eng, None, list(keys), [])

    def barrier(self):
        last = {}
        for o in self.ops:
            if o.fn is None:
                continue
            last[(o.chan if o.is_dma else o.eng)] = o
        deps = list(last.values())
        for e in ENGS:
            o = _Op()
            o.eng, o.fn, o.is_dma, o.chan, o.sig, o.val = e, None, False, None, False, None
            o.deps = deps
            o.f32 = False
            o.chain = ""
            o.waits = None
            self.ops.append(o)

    def emit(self, stack):
        nc = self.nc
        for o in self.ops:
            for d in o.deps:
                d.sig = True
        cnt = {e: 0 for e in ENGS}
        chan_cnt = {}
        for o in self.ops:
            if o.fn is None:
                continue
            if o.is_dma:
                chan_cnt[o.chan] = chan_cnt.get(o.chan, 0) + 16
                o.val = chan_cnt[o.chan]
            elif o.sig:
                cnt[o.eng] += 1
                o.val = cnt[o.eng]
        sems = {}
        for e in ENGS:
            sems["e:" + e] = stack.enter_context(nc.semaphore("sem_" + e))
        for i, c in enumerate(sorted(chan_cnt)):
            sems[c] = stack.enter_context(nc.semaphore("semc_%d" % i))
        self.n_sems = len(sems)
        waited = {e: {} for e in ENGS}
        per_eng = {e: [] for e in ENGS}
        for o in self.ops:
            ws = {}
            for d in o.deps:
                if d.val is None:
                    continue
                s = d.chan if d.is_dma else "e:" + d.eng
                if ws.get(s, 0) < d.val:
                    ws[s] = d.val
            o.waits = []
            isbar = (o.fn is None and len(o.deps) > 8)
            for s, v in ws.items():
                if isbar or waited[o.eng].get(s, 0) < v:
                    waited[o.eng][s] = v
                    o.waits.append((s, v))
            per_eng[o.eng].append(o)
        block = stack.enter_context(nc.Block())
        self.counts = {e: len(per_eng[e]) for e in ENGS}

        def run(e, eng):
            for o in per_eng[e]:
                for s, v in o.waits:
                    eng.wait_ge(sems[s], v)
                if o.fn is None:
                    continue
                ins = o.fn(eng)
                if o.is_dma:
                    ins.then_inc(sems[o.chan], 16)
                elif o.sig:
                    ins.then_inc(sems["e:" + e], 1)

        @block.tensor
        def _(eng):
            run("pe", eng)

        @block.scalar
        def _(eng):
            run("act", eng)

        @block.vector
        def _(eng):
            run("dve", eng)

        @block.gpsimd
        def _(eng):
            run("pool", eng)

        @block.sync
        def _(eng):
            run("sp", eng)


_DTSZ = {F32: 4, BF16: 2, I32: 4}


class SB:
    def __init__(self, nc, nbytes):
        self.t = nc.alloc_sbuf_tensor("sbuf_all", [128, nbytes], U8)
        self.off = 0
        self.cap = nbytes
        self.hi = 0

    def a(self, free, dt, parts=128):
        if isinstance(free, int):
            free = [free]
        n = int(np.prod(free)) * _DTSZ[dt]
        off = (self.off + 31) // 32 * 32
        self.off = off + n
        self.hi = max(self.hi, self.off)
        assert self.off <= self.cap, ("SBUF overflow", self.off, self.cap)
        ap = self.t[0:parts, off:off + n].bitcast(dt)
        if len(free) == 2:
            ap = ap.rearrange("p (a b) -> p a b", a=free[0], b=free[1])
        elif len(free) == 3:
            ap = ap.rearrange("p (a b c) -> p a b c", a=free[0], b=free[1], c=free[2])
        return ap


def bc(ap, shape):
    return ap.to_broadcast(list(shape))


def host_consts():
    c = {}
    c["identb"] = np.eye(128, dtype=np.float32)
    log_g = np.log1p(-np.exp2(-5.0 - np.arange(4, dtype=np.float64)))
    n = np.arange(128, dtype=np.float64)
    diff = n[:, None] - n[None, :]
    dmat = np.where(diff[None] >= 0, np.exp(log_g[:, None, None] * np.maximum(diff, 0.0)[None]), 0.0)
    c["dmatT"] = np.ascontiguousarray(dmat.transpose(2, 0, 1)).astype(np.float32)
    gq = np.exp((n[None, :] + 1) * log_g[:, None])
    gqT = np.zeros((128, 2, 128))
    for p in range(2):
        gqT[:64, p, :] = gq[2 * p][None, :]
        gqT[64:, p, :] = gq[2 * p + 1][None, :]
    c["gqT"] = gqT.astype(np.float32)
    c["gkc"] = (np.exp((127 - n)[:, None] * log_g[None, :]) * 0.125).astype(np.float32)
    dc = np.zeros((128, 2))
    for p in range(2):
        dc[:64, p] = np.exp(128 * log_g[2 * p])
        dc[64:, p] = np.exp(128 * log_g[2 * p + 1])
    c["dc"] = dc.astype(np.float32)
    half = 32
    inv = 10000.0 ** (-np.arange(half, dtype=np.float64) / half)
    ang = np.arange(S, dtype=np.float64)[:, None] * inv[None, :]
    ang = ang.astype(np.float32).astype(np.float64)
    c["cos"] = np.ascontiguousarray(np.cos(ang).reshape(16, 128, 32).transpose(1, 0, 2)).astype(np.float32)
    c["sin"] = np.ascontiguousarray(np.sin(ang).reshape(16, 128, 32).transpose(1, 0, 2)).astype(np.float32)
    s_idx = np.arange(128)
    c["maskneg"] = np.where(s_idx[:, None] <= s_idx[None, :], 0.0, -30000.0).astype(np.float32)
    eh = np.zeros((4, 4, 128), np.float32)
    for h in range(4):
        eh[h, h, :] = 1.0
    c["eh"] = eh
    c["ident4"] = np.eye(4, dtype=np.float32)
    c["triu"] = (s_idx[:, None] < s_idx[None, :]).astype(np.float32)
    c["ecap"] = np.broadcast_to((np.arange(NEXP) * CAP).astype(np.float32)[None, :], (128, NEXP)).copy()
    return c


CONST_SHAPES = {
    "identb": [128, 128], "dmatT": [128, 4, 128], "gqT": [128, 2, 128], "gkc": [128, 4], "dc": [128, 2],
    "cos": [128, 16, 32], "sin": [128, 16, 32], "maskneg": [128, 128], "eh": [4, 4, 128], "ident4": [4, 4],
    "triu": [128, 128], "ecap": [128, NEXP],
}


def build_program(NS=4, NCH=16, phases=("mix", "xa", "moe"), dbg=False):
    SL = NCH * 128
    NT = NS * NCH
    NTOK = NT * 128
    nc = bass.Bass("TRN2", target_bir_lowering=False)

    def din(name, shape, dt=F32):
        return nc.dram_tensor(name, list(shape), dt, kind="ExternalInput").ap()

    x_d = din("x", [NS, SL, D])
    mem_d = din("mem", [NS, 256, D])
    w_in_d = din("w_in", [D, 3592])
    w_out_d = din("w_out", [D, D])
    wq_d = din("xa_wq", [D, D])
    wkv_d = din("xa_wkv", [D, 2 * D])
    wo_d = din("xa_wo", [D, D])
    wg_d = din("moe_w_gate", [NEXP, D, 512])
    wu_d = din("moe_w_up", [NEXP, D, 512])
    wd_d = din("moe_w_down", [NEXP, 512, D])
    gains_d = din("gains", [6, 128, D])
    convw_d = din("convw", [128, 8, 5])
    gateb_d = din("gateb", [4, 2])
    wr_d = din("wr", [D, 36])
    rb_d = din("rb", [128, 36])
    cd = {k: din("c_" + k, v) for k, v in CONST_SHAPES.items()}
    out_d = nc.dram_tensor("out", [NS, SL, D], F32, kind="ExternalOutput").ap()
    x1_d = nc.dram_tensor("x1_scr", [NTOK, D], F32, kind="Internal").ap()
    x2_d = nc.dram_tensor("x2_scr", [NTOK, D], F32, kind="Internal").ap()
    xs_d = nc.dram_tensor("xs_scr", [NEXP * CAP, D], BF16, kind="Internal").ap()
    ys_d = nc.dram_tensor("ys_scr", [NEXP * CAP, D], BF16, kind="Internal").ap()

    st = ExitStack()
    P = Prog(nc)
    sb = SB(nc, 212800)
    pp = [nc.alloc_psum_tensor("pp%d" % i, [128, 1024], F32) for i in range(4)]
    ppb = [t[:, 0:512].bitcast(BF16).rearrange("p (a b) -> p a b", a=8, b=128) for t in pp]
    ppb2 = [t[:, 512:1024].bitcast(BF16).rearrange("p (a b) -> p a b", a=8, b=128) for t in pp]
    rot = {}
    ppset = [(0, 1, 2, 3)]

    def nextpp(full=True):
        sset = ppset[0]
        if len(sset) == 1 and not full:
            r = rot.get((sset, "h"), 0)
            rot[(sset, "h")] = r + 1
            i = sset[0]
            if r % 2 == 0:
                return pp[i][:, 0:512], "pp%da" % i
            return pp[i][:, 512:1024], "pp%db" % i
        r = rot.get(sset, 0)
        rot[sset] = r + 1
        i = sset[r % len(sset)]
        return pp[i], "pp%d" % i

    def nextpt():
        sset = ppset[0]
        if len(sset) == 1:
            r = rot.get((sset, "h"), 0)
            rot[(sset, "h")] = r + 1
            i = sset[0]
            if r % 2 == 0:
                return ppb[i], "pp%da" % i
            return ppb2[i], "pp%db" % i
        r = rot.get(sset, 0)
        rot[sset] = r + 1
        i = sset[r % len(sset)]
        return ppb[i], "pp%d" % i

    def load_const(name, dt=F32, parts=128, eng="sp"):
        shp = CONST_SHAPES[name]
        t = sb.a(shp[1:], dt, parts=shp[0])
        if dt == F32:
            P.dma(eng, t, cd[name], writes=[name])
        else:
            P.dma("pool", t, cd[name], writes=[name])
        return t

    identb = load_const("identb", BF16)
    gains = {}

    def load_gain(i, name):
        t = sb.a([D], F32)
        P.dma("sp", t, gains_d[i], writes=[name])
        gains[name] = t
        return t

    dbg_out = {}
    mark0 = sb.off

    def phase_mix():
        w_in = sb.a([8, 3592], BF16)
        w_out = sb.a([8, D], BF16)
        wv = w_in_d.rearrange("(kc p) n -> p kc n", p=128)
        for kc in range(8):
            P.dma("pool", w_in[:, kc, 0:2048], wv[:, kc, 0:2048], writes=["w_in"])
            P.dma("pool", w_in[:, kc, 2048:3592], wv[:, kc, 2048:3592], writes=["w_in"])
        wv = w_out_d.rearrange("(kc p) n -> p kc n", p=128)
        for kc in range(8):
            P.dma("pool", w_out[:, kc, :], wv[:, kc, :], writes=["w_out"])
        gmix = load_gain(0, "gmix")
        ghn = load_gain(5, "ghn")
        dmatT = load_const("dmatT")
        gqT = load_const("gqT")
        gkc = load_const("gkc")
        dcc = load_const("dc")
        cosT = load_const("cos")
        sinT = load_const("sin")
        maskneg = load_const("maskneg", BF16)
        eh = load_const("eh")
        ident4 = load_const("ident4")
        convw = sb.a([8, 5], F32)
        P.dma("sp", convw, convw_d, writes=["convw"])
        gateb = sb.a([2], F32, parts=4)
        P.dma("sp", gateb, gateb_d, writes=["gateb"])
        ones4 = sb.a([128], F32, parts=4)
        P.op("pool", lambda e: e.memset(ones4, 1.0), writes=["ones4"])
        epsc = sb.a([1], F32)
        P.op("pool", lambda e: e.memset(epsc, EPS), writes=["epsc"])

        SLOTKA = {"xa", "ss", "rstd", "hb", "hT", "qk_f", "rt0", "rt1", "qkr", "qT", "kT", "qdT", "kdec", "rv", "sc_bf", "ro",
                  "cenR", "sqR", "st4R", "nm4R", "rs4R", "cenM", "sqM", "st4M", "nm4M", "rs4M", "sg", "mix", "cb", "acc", "tmpc",
                  "mqk_tok", "qkT", "mv", "so", "g_ig", "g_fp", "g_t1", "g_t2", "g_b", "g_u", "g_cu", "g_M", "g_nM", "g_in",
                  "g_fp", "g_ig", "g_t1", "s4", "dg", "cols", "sbcs", "DT", "Pm", "qTs", "kw", "hm", "dn4", "tmpC"}
        ST = [(sb.a([2, 128], F32), sb.a([2, 128], BF16), sb.a([4, 129], F32), sb.a([4, 129], BF16), sb.a([1], F32, parts=4))
              for _ in range(2)]
        CBS = [None, None]

        def make_slot(slot):
            xt = sb.a([D], F32)
            ss = sb.a([1], F32)
            rstd = sb.a([1], F32)
            hb = sb.a([D], BF16)
            hT = sb.a([8, 128], BF16)
            qk_f = sb.a([8, 2, 32], BF16)
            rt = [sb.a([8, 32], BF16) for _ in range(2)]
            qkr = sb.a([8, 2, 32], BF16)
            qT = sb.a([4, 128], BF16)
            kT = sb.a([2, 128], BF16)
            qdT = sb.a([4, 128], BF16)
            kdec = sb.a([4, 64], BF16)
            rv = sb.a([512], BF16)
            sc_bf = sb.a([4, 128], BF16)
            ro = sb.a([4, 128], F32)
            cen = sb.a([4, 128], F32)
            sq = sb.a([4, 128], BF16)
            st4 = sb.a([4], F32)
            nm4 = sb.a([4], F32)
            rs4 = sb.a([4], F32)
            sg = sb.a([512], BF16)
            mix = sb.a([D], BF16)
            cb = sb.a([8, 131], BF16)
            CBS[slot] = cb
            acc = sb.a([8, 128], F32)
            tmpc = sb.a([8, 128], BF16)
            mqk_tok = sb.a([D], BF16)
            junk = mqk_tok
            qkT = sb.a([8, 128], BF16)
            mv = sb.a([4, 129], BF16)
            so = sb.a([512], BF16)
            g_ig = sb.a([128], F32, parts=4)
            g_fp = sb.a([128], F32, parts=4)
            g_t1 = sb.a([128], F32, parts=4)
            g_t2 = sb.a([128], F32, parts=4)
            g_b = sb.a([128], F32, parts=4)
            g_u = sb.a([128], F32, parts=4)
            g_cu = sb.a([128], F32, parts=4)
            g_M = sb.a([128], F32, parts=4)
            g_nM = sb.a([128], F32, parts=4)
            g_in = sb.a([128], F32, parts=4)
            g_em = g_fp
            g_wa = g_ig
            g_mb = g_t1
            s4 = sb.a([4], F32, parts=4)
            dg = sb.a([2, 4], F32, parts=4)
            cols = sb.a([3, 4], F32)
            sbcs = sb.a([2, 4], F32)
            DT = sb.a([4, 128], BF16)
            Pm = sb.a([4, 128], BF16)
            qTs = sb.a([4, 128], BF16)
            kw = sb.a([4, 128], BF16)
            hm = sb.a([4, 128], F32)
            dn4 = sb.a([4], F32)
            tmpC = sb.a([4, 129], BF16)
            y1 = xt

            P.keymap = (SLOTKA, "_%d" % slot)
            P.op("pool", lambda e: e.memset(mv, 1.0), writes=["mv"])
            P.op("pool", lambda e: e.memset(qT, 0.0), writes=["qT"])
            P.op("pool", lambda e: e.memset(qdT, 0.0), writes=["qdT"])
            P.keymap = None
            DKS = 128.0 ** -0.5

            def rmsnorm_to_hb(src, srck, gain, gk):
                P.op("act", lambda e: e.activation(out=junk, in_=src, func=AF.Square, accum_out=ss),
                     reads=[srck], writes=["mqk_tok", "ss"])
                P.op("act", lambda e: e.activation(out=rstd, in_=ss, func=AF.Sqrt, bias=epsc, scale=1.0 / D),
                     reads=["ss", "epsc"], writes=["rstd"])
                P.op("dve", lambda e: e.reciprocal(out=rstd, in_=rstd), reads=["rstd"], writes=["rstd"])
                P.op("dve", lambda e: e.scalar_tensor_tensor(out=hb, in0=src, scalar=rstd, in1=gain,
                                                               op0=ALU.mult, op1=ALU.mult),
                     reads=[srck, "rstd", gk], writes=["hb"])

            def transpose8(src, srck, dst, dstk):
                ptile, ptk = nextpt()
                for j in range(8):
                    P.op("pe", lambda e, j=j: e.transpose(out=ptile[:, j, :], in_=src[:, j * 128:(j + 1) * 128],
                                                          identity=identb),
                         reads=[srck, "identb"], writes=[ptk], chain=True)
                P.op("act", lambda e: e.copy(out=dst, in_=ptile), reads=[ptk], writes=[dstk])

            hn_tmp = {"R": (st4, nm4, rs4, cen, sq),
                      "M": (sb.a([4], F32), sb.a([4], F32), sb.a([4], F32), sb.a([4, 128], F32), sb.a([4, 128], BF16))}

            def head_norm(src, srck, gain_sl, gate, gatek, dst_sl, br="R"):
                st4, nm4, rs4, cen, sq = hn_tmp[br]
                return head_norm_(src, srck, gain_sl, gate, gatek, dst_sl, st4, nm4, rs4, cen, sq, br)

            def head_norm_(src, srck, gain_sl, gate, gatek, dst_sl, st4, nm4, rs4, cen, sq, br):
                head_norm__(src, srck, gain_sl, gate, gatek, dst_sl, st4, nm4, rs4, cen, sq, br)

            def head_norm__(src, srck, gain_sl, gate, gatek, dst_sl, st4, nm4, rs4, cen, sq, br):
                P.op("dve", lambda e: e.tensor_reduce(out=st4, in_=src, axis=AX.X, op=ALU.add),
                     reads=[srck], writes=["st4" + br])
                P.op("dve", lambda e: e.tensor_scalar(out=nm4, in0=st4, scalar1=-1.0 / 128, scalar2=None, op0=ALU.mult),
                     reads=["st4" + br], writes=["nm4" + br])
                P.op("dve", lambda e: e.tensor_tensor(out=cen, in0=src, in1=bc(nm4.unsqueeze(2), [128, 4, 128]),
                                                      op=ALU.add), reads=[srck, "nm4" + br], writes=["cen" + br])
                P.op("pool", lambda e: e.tensor_tensor(out=sq, in0=cen, in1=cen, op=ALU.mult),
                     reads=["cen" + br], writes=["sq" + br])
                P.op("dve", lambda e: e.tensor_reduce(out=st4, in_=sq, axis=AX.X, op=ALU.add),
                     reads=["sq" + br], writes=["st4" + br])
                P.op("act", lambda e: e.activation(out=rs4, in_=st4, func=AF.Sqrt, bias=epsc, scale=1.0 / 128),
                     reads=["st4" + br, "epsc"], writes=["rs4" + br])
                P.op("dve", lambda e: e.reciprocal(out=rs4, in_=rs4), reads=["rs4" + br], writes=["rs4" + br])
                P.op("dve", lambda e: e.tensor_tensor(out=cen, in0=cen, in1=bc(rs4.unsqueeze(2), [128, 4, 128]),
                                                      op=ALU.mult), reads=["cen" + br, "rs4" + br], writes=["cen" + br])
                P.op("pool", lambda e: e.tensor_tensor(out=cen, in0=cen, in1=gain_sl, op=ALU.mult),
                     reads=["cen" + br, "ghn"], writes=["cen" + br])
                P.op("dve", lambda e: e.tensor_tensor(out=dst_sl, in0=cen, in1=gate, op=ALU.mult),
                     reads=["cen" + br, gatek], writes=["mix"])

            def mix_tile(b, c):
                if True:
                    it = b * NCH + c
                    Rf, Rbf, Cf, Cbf, mprev = ST[b % 2]
                    sk = lambda n: n + str(b % 2)
                    xk = "xa"
                    PA, PB = (2 * slot,), (2 * slot + 1,)
                    if c == 0:
                        P.op("pool", lambda e: e.memset(Rf, 0.0), writes=[sk("Rf")])
                        P.op("pool", lambda e: e.memset(Rbf, 0.0), writes=[sk("Rbf")])
                        P.op("pool", lambda e: e.memset(Cf, 0.0), writes=[sk("Cf")])
                        P.op("pool", lambda e: e.memset(Cbf, 0.0), writes=[sk("Cbf")])
                        P.op("pool", lambda e: e.memset(mprev, 0.0), writes=[sk("mprev")])
                        P.op("pool", lambda e: e.memset(cb[:, :, 0:3], 0.0), writes=["cbh%d" % slot])
                    P.dma("sp", xt, x_d[b, c * 128:(c + 1) * 128, :], writes=[xk])
                    ppset[0] = PA
                    rmsnorm_to_hb(xt, xk, gmix, "gmix")
                    transpose8(hb, "hb", hT, "hT")

                    def proj_tok(lo):
                        pst, psk = nextpp(full=False)
                        for kc in range(8):
                            P.op("pe", lambda e, kc=kc: e.matmul(pst[:, 0:512], lhsT=hT[:, kc, :],
                                                                 rhs=w_in[:, kc, lo:lo + 512],
                                                                 start=(kc == 0), stop=(kc == 7)),
                                 reads=["hT", "w_in"], writes=[psk], chain=True)
                        return pst, psk

                    G.pre = P.stop()
                    P.record()
                    ppset[0] = PA
                    pqk, pqkk = proj_tok(0)
                    P.op("act", lambda e: e.copy(out=qk_f.rearrange("p a b c -> p (a b c)"), in_=pqk[:, 0:512]),
                         reads=[pqkk], writes=["qk_f"])
                    cs = bc(cosT[:, c, :].unsqueeze(1), [128, 8, 32])
                    sn = bc(sinT[:, c, :].unsqueeze(1), [128, 8, 32])
                    x1v, x2v = qk_f[:, :, 0, :], qk_f[:, :, 1, :]
                    P.op("dve", lambda e: e.tensor_tensor(out=rt[0], in0=x1v, in1=cs, op=ALU.mult),
                         reads=["qk_f", "cos"], writes=["rt0"])
                    P.op("pool", lambda e: e.tensor_tensor(out=rt[1], in0=x2v, in1=sn, op=ALU.mult),
                         reads=["qk_f", "sin"], writes=["rt1"])
                    P.op("dve", lambda e: e.tensor_tensor(out=qkr[:, :, 0, :], in0=rt[0], in1=rt[1], op=ALU.subtract),
                         reads=["rt0", "rt1"], writes=["qkr"])
                    P.op("pool", lambda e: e.tensor_tensor(out=rt[0], in0=x1v, in1=sn, op=ALU.mult),
                         reads=["qk_f", "sin"], writes=["rt0"])
                    P.op("dve", lambda e: e.tensor_tensor(out=rt[1], in0=x2v, in1=cs, op=ALU.mult),
                         reads=["qk_f", "cos"], writes=["rt1"])
                    P.op("dve", lambda e: e.tensor_tensor(out=qkr[:, :, 1, :], in0=rt[0], in1=rt[1], op=ALU.add),
                         reads=["rt0", "rt1"], writes=["qkr"])
                    qkr2 = qkr.rearrange("p a b c -> p (a b c)")
                    ptq, ptqk = nextpt()
                    for j in range(4):
                        P.op("pe", lambda e, j=j: e.transpose(out=ptq[:, j, :], in_=qkr2[:, j * 128:(j + 1) * 128],
                                                              identity=identb),
                             reads=["qkr", "identb"], writes=[ptqk], chain=True)
                    P.op("act", lambda e: e.copy(out=qT[0:64, 0:4:2, :], in_=ptq[0:64, 0:2, :]), reads=[ptqk], writes=["qT"])
                    P.op("act", lambda e: e.copy(out=qT[64:128, 1:4:2, :], in_=ptq[64:128, 0:2, :]), reads=[ptqk], writes=["qT"])
                    import os as _os
                    _v = int(_os.environ.get("DBGV", "0"))
                    if _v != 1:
                        P.op("act", lambda e: e.mul(out=kT, in_=ptq[:, 2:4, :], mul=0.125), reads=[ptqk], writes=["kT"])
                    P.op("dve", lambda e: e.tensor_tensor(out=qdT[0:64, 0:4:2, :], in0=qT[0:64, 0:4:2, :], in1=gqT[0:64, :, :],
                                                          op=ALU.mult), reads=["qT", "gqT"], writes=["qdT"])
                    P.op("dve", lambda e: e.tensor_tensor(out=qdT[64:128, 1:4:2, :], in0=qT[64:128, 1:4:2, :],
                                                          in1=gqT[64:128, :, :], op=ALU.mult), reads=["qT", "gqT"], writes=["qdT"])
                    P.op("dve", lambda e: e.tensor_tensor(out=kdec, in0=qkr2[:, 256:512].rearrange("p (h d) -> p h d", h=4),
                                                           in1=bc(gkc.unsqueeze(2), [128, 4, 64]), op=ALU.mult),
                         reads=["qkr", "gkc"], writes=["kdec"])
                    prv, prvk = proj_tok(512)
                    P.op("act", lambda e: e.copy(out=rv, in_=prv[:, 0:512]), reads=[prvk], writes=["rv"])
                    prg, prgk = proj_tok(1024)
                    P.op("act", lambda e: e.activation(out=sg, in_=prg[:, 0:512], func=AF.Silu),
                         reads=[prgk], writes=["sg"])
                    psc, psck = nextpp(full=False)
                    for h in range(4):
                        p_, off = h // 2, (h % 2) * 64
                        P.op("pe", lambda e, h=h, p_=p_, off=off: e.matmul(
                            psc[:, h * 128:(h + 1) * 128], lhsT=kT[:, p_, :], rhs=qT[:, h, :],
                            start=True, stop=True), reads=["kT", "qT"], writes=[psck], chain=True)
                    P.op("dve", lambda e: e.tensor_tensor(out=sc_bf.rearrange("p a b -> p (a b)"), in0=psc[:, 0:512],
                                                          in1=dmatT.rearrange("p a b -> p (a b)"), op=ALU.mult),
                         reads=[psck, "dmatT"], writes=["sc_bf"])
                    pro, prok = nextpp(full=False)
                    for h in range(4):
                        p_, off = h // 2, (h % 2) * 64
                        P.op("pe", lambda e, h=h: e.matmul(pro[:, h * 128:(h + 1) * 128], lhsT=sc_bf[:, h, :],
                                                           rhs=rv[:, h * 128:(h + 1) * 128], start=True, stop=False),
                             reads=["sc_bf", "rv"], writes=[prok], chain=True)
                        P.op("pe", lambda e, h=h, p_=p_, off=off: e.matmul(
                            pro[:, h * 128:(h + 1) * 128], lhsT=qdT[:, h, :], rhs=Rbf[:, p_, :],
                            start=False, stop=True), reads=["qdT", sk("Rbf")], writes=[prok], chain=True)
                    P.op("act", lambda e: e.copy(out=ro.rearrange("p a b -> p (a b)"), in_=pro[:, 0:512]),
                         reads=[prok], writes=["ro"])
                    pkv, pkvk = nextpp(full=False)
                    for h in range(4):
                        p_ = h // 2
                        P.op("pe", lambda e, h=h, p_=p_: e.matmul(
                            pkv[:, h * 128:(h + 1) * 128], lhsT=kdec[:, 2 * p_:2 * p_ + 2, :].rearrange("p a b -> p (a b)"),
                            rhs=rv[:, h * 128:(h + 1) * 128], start=True, stop=True),
                            reads=["kdec", "rv"], writes=[pkvk], chain=True)
                    for h in range(4):
                        p_, off = h // 2, (h % 2) * 64
                        P.op("dve", lambda e, h=h, p_=p_, off=off: e.scalar_tensor_tensor(
                            out=Rf[off:off + 64, p_, :], in0=Rf[off:off + 64, p_, :], scalar=dcc[off:off + 64, p_:p_ + 1],
                            in1=pkv[off:off + 64, h * 128:(h + 1) * 128], op0=ALU.mult, op1=ALU.add),
                            reads=[sk("Rf"), "dc", pkvk], writes=[sk("Rf")])
                    P.op("pool", lambda e: e.tensor_copy(out=Rbf, in_=Rf), reads=[sk("Rf")], writes=[sk("Rbf")])
                    head_norm(ro, "ro", ghn[:, 0:512].rearrange("p (a b) -> p a b", a=4),
                              sg.rearrange("p (a b) -> p a b", a=4), "sg",
                              mix[:, 0:512].rearrange("p (a b) -> p a b", a=4))

                    listR = P.stop()
                    P.record()
                    ppset[0] = PB
                    pg, pgk = nextpp(full=False)
                    for gi in range(2):
                        for kc in range(8):
                            P.op("pe", lambda e, gi=gi, kc=kc: e.matmul(
                                pg[0:4, gi * 128:(gi + 1) * 128], lhsT=w_in[:, kc, 3584 + 4 * gi:3588 + 4 * gi],
                                rhs=hT[:, kc, :], start=(kc == 0), stop=(kc == 7)),
                                reads=["hT", "w_in"], writes=[pgk], chain=True)
                    P.op("act", lambda e: e.activation(out=g_ig, in_=pg[0:4, 0:128], func=AF.Identity, bias=gateb[:, 0:1]),
                         reads=[pgk, "gateb"], writes=["g_ig"])
                    P.op("act", lambda e: e.activation(out=g_fp, in_=pg[0:4, 128:256], func=AF.Identity, bias=gateb[:, 1:2]),
                         reads=[pgk, "gateb"], writes=["g_fp"])
                    P.op("act", lambda e: e.activation(out=g_t1, in_=g_fp, func=AF.Abs),
                         reads=["g_fp"], writes=["g_t1"])
                    P.op("act", lambda e: e.activation(out=g_t1, in_=g_t1, func=AF.Exp, scale=-1.0),
                         reads=["g_t1"], writes=["g_t1"])
                    P.op("act", lambda e: e.activation(out=g_t1, in_=g_t1, func=AF.Ln, bias=1.0),
                         reads=["g_t1"], writes=["g_t1"])
                    P.op("dve", lambda e: e.tensor_scalar(out=g_t2, in0=g_fp, scalar1=0.0, scalar2=None, op0=ALU.min),
                         reads=["g_fp"], writes=["g_t2"])
                    P.op("dve", lambda e: e.tensor_tensor(out=g_t2, in0=g_t2, in1=g_t1, op=ALU.subtract),
                         reads=["g_t2", "g_t1"], writes=["g_t2"])
                    P.op("dve", lambda e: e.tensor_tensor_scan(out=g_b, data0=ones4, data1=g_t2, initial=0.0,
                                                               op0=ALU.mult, op1=ALU.add),
                         reads=["ones4", "g_t2"], writes=["g_b"])
                    P.op("dve", lambda e: e.tensor_tensor(out=g_u, in0=g_ig, in1=g_b, op=ALU.subtract),
                         reads=["g_ig", "g_b"], writes=["g_u"])
                    P.op("dve", lambda e: e.tensor_tensor_scan(out=g_cu, data0=ones4, data1=g_u, initial=-1e30,
                                                               op0=ALU.mult, op1=ALU.max),
                         reads=["ones4", "g_u"], writes=["g_cu"])
                    P.op("dve", lambda e: e.tensor_scalar(out=g_M, in0=g_cu, scalar1=mprev, scalar2=None, op0=ALU.max),
                         reads=["g_cu", sk("mprev")], writes=["g_M"])
                    P.op("dve", lambda e: e.tensor_scalar(out=g_nM, in0=g_M, scalar1=-1.0, scalar2=None, op0=ALU.mult),
                         reads=["g_M"], writes=["g_nM"])
                    P.op("act", lambda e: e.activation(out=g_in, in_=g_M, func=AF.Exp, scale=-1.0, bias=mprev),
                         reads=["g_M", sk("mprev")], writes=["g_in"])
                    P.op("dve", lambda e: e.tensor_tensor(out=g_mb, in0=g_M, in1=g_b, op=ALU.add),
                         reads=["g_M", "g_b"], writes=["g_t1"])
                    P.op("act", lambda e: e.activation(out=g_em, in_=g_mb, func=AF.Exp, scale=-1.0),
                         reads=["g_t1"], writes=["g_fp"])
                    P.op("dve", lambda e: e.tensor_scalar(out=s4[:, 1:2], in0=g_cu[:, 127:128],
                                                          scalar1=-1.0, scalar2=None, op0=ALU.mult),
                         reads=["g_cu"], writes=["s4"])
                    P.op("dve", lambda e: e.tensor_scalar(out=s4[:, 2:3], in0=g_M[:, 127:128], scalar1=-1.0, scalar2=None,
                                                          op0=ALU.mult), reads=["g_M"], writes=["s4"])
                    P.op("act", lambda e: e.activation(out=g_wa, in_=g_u, func=AF.Exp, bias=s4[:, 1:2]),
                         reads=["g_u", "s4"], writes=["g_ig"])
                    P.op("act", lambda e: e.activation(out=s4[:, 3:4], in_=g_cu[:, 127:128], func=AF.Exp, bias=s4[:, 2:3]),
                         reads=["g_cu", "s4"], writes=["s4"])
                    P.op("dve", lambda e: e.tensor_scalar(out=dg[:, 0, :], in0=ident4, scalar1=g_in[:, 127:128], scalar2=None,
                                                          op0=ALU.mult), reads=["ident4", "g_in"], writes=["dg"])
                    P.op("dve", lambda e: e.tensor_scalar(out=dg[:, 1, :], in0=ident4, scalar1=s4[:, 3:4], scalar2=None,
                                                          op0=ALU.mult), reads=["ident4", "s4"], writes=["dg"])
                    P.op("dve", lambda e: e.tensor_copy(out=mprev, in_=g_mb[:, 127:128]),
                         reads=["g_t1", "g_in", "g_M"], writes=[sk("mprev")])
                    pc, pck = nextpp(full=False)
                    P.op("pe", lambda e: e.matmul(pc[:, 0:4], lhsT=g_wa, rhs=ident4, start=True, stop=True),
                         reads=["g_ig", "ident4"], writes=[pck], f32=True)
                    P.op("pe", lambda e: e.matmul(pc[:, 8:12], lhsT=g_em, rhs=ident4, start=True, stop=True),
                         reads=["g_fp", "ident4"], writes=[pck], f32=True)
                    P.op("pe", lambda e: e.matmul(pc[:, 16:20], lhsT=ones4, rhs=dg[:, 0, :], start=True, stop=True),
                         reads=["ones4", "dg"], writes=[pck], f32=True)
                    P.op("pe", lambda e: e.matmul(pc[:, 20:24], lhsT=ones4, rhs=dg[:, 1, :], start=True, stop=True),
                         reads=["ones4", "dg"], writes=[pck], f32=True)
                    P.op("dve", lambda e: e.tensor_scalar(out=cols[:, 0, :], in0=pc[:, 0:4], scalar1=DKS, scalar2=None,
                                                          op0=ALU.mult), reads=[pck], writes=["cols"])
                    P.op("dve", lambda e: e.tensor_copy(out=cols[:, 2, :], in_=pc[:, 8:12]), reads=[pck], writes=["cols"])
                    P.op("dve", lambda e: e.tensor_copy(out=sbcs.rearrange("p a b -> p (a b)"), in_=pc[:, 16:24]),
                         reads=[pck], writes=["sbcs"])

                    pm, pmk = nextpp()
                    for g in range(2):
                        for kc in range(8):
                            P.op("pe", lambda e, g=g, kc=kc: e.matmul(
                                pm[:, g * 512:(g + 1) * 512], lhsT=hT[:, kc, :],
                                rhs=w_in[:, kc, 1536 + g * 512:1536 + (g + 1) * 512], start=(kc == 0), stop=(kc == 7)),
                                reads=["hT", "w_in"], writes=[pmk], chain=True)
                    P.op("act", lambda e: e.copy(out=mqk_tok, in_=pm[:, :]), reads=[pmk], writes=["mqk_tok"])
                    ptm, ptmk = nextpt()
                    for j in range(8):
                        P.op("pe", lambda e, j=j: e.transpose(out=ptm[:, j, :], in_=mqk_tok[:, j * 128:(j + 1) * 128],
                                                              identity=identb),
                             reads=["mqk_tok", "identb"], writes=[ptmk], chain=True)
                    P.op("act", lambda e: e.copy(out=cb[:, :, 3:131], in_=ptm), reads=[ptmk], writes=["cb"])
                    if c + 1 < NCH:
                        P.op("pool", lambda e: e.tensor_copy(out=CBS[1 - slot][:, :, 0:3], in_=cb[:, :, 128:131]),
                             reads=["cb"], writes=["cbh%d" % (1 - slot)])
                    P.op("dve", lambda e: e.tensor_tensor(out=acc, in0=cb[:, :, 3:131],
                                                          in1=bc(convw[:, :, 3:4], [128, 8, 128]), op=ALU.mult),
                         reads=["cb", "cbh%d" % slot, "convw"], writes=["acc"])
                    for tap in range(3):
                        P.op("pool", lambda e, tap=tap: e.tensor_tensor(out=tmpc, in0=cb[:, :, tap:tap + 128],
                                                                        in1=bc(convw[:, :, tap:tap + 1], [128, 8, 128]),
                                                                        op=ALU.mult),
                             reads=["cb", "cbh%d" % slot, "convw"], writes=["tmpc"])
                        P.op("dve", lambda e: e.tensor_tensor(out=acc, in0=acc, in1=tmpc, op=ALU.add),
                             reads=["acc", "tmpc"], writes=["acc"])
                    for j in range(8):
                        P.op("act", lambda e, j=j: e.activation(out=qkT[:, j, :], in_=acc[:, j, :], func=AF.Silu,
                                                                bias=convw[:, j, 4:5]),
                             reads=["acc", "convw"], writes=["qkT"])
                    pmv, pmvk = proj_tok(2560)
                    P.op("act", lambda e: e.copy(out=mv[:, :, 0:128], in_=pmv[:, 0:512].rearrange("p (a b) -> p a b", a=4)),
                         reads=[pmvk], writes=["mv"])
                    pmo, pmok = proj_tok(3072)
                    P.op("act", lambda e: e.activation(out=so, in_=pmo[:, 0:512], func=AF.Sigmoid),
                         reads=[pmok], writes=["so"])
                    ptk_, ptkk = nextpt()
                    for h in range(4):
                        P.op("pe", lambda e, h=h: e.transpose(out=ptk_[:, h, :], in_=qkT[:, 4 + h, :], identity=identb),
                             reads=["qkT", "identb"], writes=[ptkk], chain=True)
                    P.op("act", lambda e: e.copy(out=kw, in_=ptk_[:, 0:4, :]), reads=[ptkk], writes=["kw"])
                    P.op("dve", lambda e: e.tensor_tensor(out=kw, in0=kw,
                                                          in1=bc(cols[:, 0, :].unsqueeze(2), [128, 4, 128]), op=ALU.mult),
                         reads=["kw", "cols"], writes=["kw"])
                    ps_, psk_ = nextpp()
                    for h in range(4):
                        P.op("pe", lambda e, h=h: e.matmul(ps_[:, h * 128:(h + 1) * 128], lhsT=qkT[:, 4 + h, :],
                                                           rhs=qkT[:, h, :], start=True, stop=True),
                             reads=["qkT"], writes=[psk_], chain=True)
                    for h in range(4):
                        P.op("pe", lambda e, h=h: e.matmul(ps_[:, 512 + h * 128:512 + (h + 1) * 128], lhsT=g_u,
                                                           rhs=eh[:, h, :], start=True, stop=False),
                             reads=["g_u", "eh"], writes=[psk_], f32=True)
                        P.op("pe", lambda e, h=h: e.matmul(ps_[:, 512 + h * 128:512 + (h + 1) * 128], lhsT=eh[:, h, :],
                                                           rhs=g_nM, start=False, stop=False),
                             reads=["g_nM", "eh"], writes=[psk_], f32=True)
                        P.op("pe", lambda e, h=h: e.matmul(ps_[:, 512 + h * 128:512 + (h + 1) * 128], lhsT=identb,
                                                           rhs=maskneg, start=False, stop=True),
                             reads=["identb", "maskneg"], writes=[psk_], chain=True)
                    P.op("act", lambda e: e.activation(out=DT.rearrange("p a b -> p (a b)"), in_=ps_[:, 512:1024], func=AF.Exp),
                         reads=[psk_], writes=["DT"])
                    P.op("dve", lambda e: e.scalar_tensor_tensor(out=Pm.rearrange("p a b -> p (a b)"), in0=ps_[:, 0:512],
                                                                 scalar=DKS, in1=DT.rearrange("p a b -> p (a b)"),
                                                                 op0=ALU.mult, op1=ALU.mult),
                         reads=[psk_, "DT"], writes=["Pm"])
                    pib, pibk = nextpp(full=False)
                    for h in range(4):
                        P.op("pe", lambda e, h=h: e.matmul(pib[:, h * 128:(h + 1) * 128], lhsT=eh[:, h, :], rhs=g_in,
                                                           start=True, stop=True), reads=["eh", "g_in"], writes=[pibk], f32=True)
                    P.op("dve", lambda e: e.tensor_tensor(out=qTs.rearrange("p a b -> p (a b)"),
                                                          in0=qkT[:, 0:4, :].rearrange("p a b -> p (a b)"),
                                                          in1=pib[:, 0:512], op=ALU.mult),
                         reads=["qkT", pibk], writes=["qTs"])
                    pn_, pnk = nextpp()
                    for h in range(4):
                        P.op("pe", lambda e, h=h: e.matmul(pn_[:, h * 256:h * 256 + 129], lhsT=Pm[:, h, :], rhs=mv[:, h, :],
                                                           start=True, stop=False), reads=["Pm", "mv"], writes=[pnk], chain=True)
                        P.op("pe", lambda e, h=h: e.matmul(pn_[:, h * 256:h * 256 + 129], lhsT=qTs[:, h, :], rhs=Cbf[:, h, :],
                                                           start=False, stop=True), reads=["qTs", sk("Cbf")], writes=[pnk], chain=True)
                    pn3 = pn_[:, :].rearrange("p (a b) -> p a b", a=4)
                    P.op("act", lambda e: e.activation(out=dn4, in_=pn3[:, :, 128], func=AF.Abs),
                         reads=[pnk], writes=["dn4"])
                    P.op("dve", lambda e: e.tensor_tensor(out=dn4, in0=dn4, in1=cols[:, 2, :], op=ALU.max),
                         reads=["dn4", "cols"], writes=["dn4"])
                    P.op("dve", lambda e: e.reciprocal(out=dn4, in_=dn4), reads=["dn4"], writes=["dn4"])
                    P.op("dve", lambda e: e.tensor_tensor(out=hm, in0=pn3[:, :, 0:128],
                                                          in1=bc(dn4.unsqueeze(2), [128, 4, 128]), op=ALU.mult),
                         reads=[pnk, "dn4"], writes=["hm"])
                    pkc, pkck = nextpp()
                    for h in range(4):
                        P.op("pe", lambda e, h=h: e.matmul(pkc[:, h * 256:h * 256 + 129], lhsT=kw[:, h, :], rhs=mv[:, h, :],
                                                           start=True, stop=True), reads=["kw", "mv"], writes=[pkck], chain=True)
                    pk3 = pkc[:, :].rearrange("p (a b) -> p a b", a=4)
                    P.op("pool", lambda e: e.tensor_tensor(out=Cf, in0=Cf, in1=bc(sbcs[:, 0, :].unsqueeze(2), [128, 4, 129]),
                                                           op=ALU.mult), reads=[sk("Cf"), "sbcs", sk("Cbf")], writes=[sk("Cf")])
                    P.op("dve", lambda e: e.tensor_tensor(out=tmpC, in0=pk3[:, :, 0:129],
                                                          in1=bc(sbcs[:, 1, :].unsqueeze(2), [128, 4, 129]), op=ALU.mult),
                         reads=[pkck, "sbcs"], writes=["tmpC"])
                    P.op("pool", lambda e: e.tensor_tensor(out=Cf, in0=Cf, in1=tmpC, op=ALU.add),
                         reads=[sk("Cf"), "tmpC"], writes=[sk("Cf")])
                    P.op("pool", lambda e: e.tensor_copy(out=Cbf, in_=Cf), reads=[sk("Cf")], writes=[sk("Cbf")])
                    head_norm(hm, "hm", ghn[:, 512:1024].rearrange("p (a b) -> p a b", a=4),
                              so.rearrange("p (a b) -> p a b", a=4), "so",
                              mix[:, 512:1024].rearrange("p (a b) -> p a b", a=4), br="M")
                    listM = P.stop()
                    ppset[0] = PA
                    G.RM = (listR, listM)
                    P.record()

                    transpose8(mix, "mix", hT, "hT")
                    py, pyk = nextpp()
                    for g in range(2):
                        for kc in range(8):
                            P.op("pe", lambda e, g=g, kc=kc: e.matmul(py[:, g * 512:(g + 1) * 512], lhsT=hT[:, kc, :],
                                                                      rhs=w_out[:, kc, g * 512:(g + 1) * 512],
                                                                      start=(kc == 0), stop=(kc == 7)),
                                 reads=["hT", "w_out"], writes=[pyk], chain=True)
                    for g in range(2):
                        P.op("dve", lambda e, g=g: e.tensor_tensor(out=y1[:, g * 512:(g + 1) * 512],
                                                                   in0=py[:, g * 512:(g + 1) * 512],
                                                                   in1=xt[:, g * 512:(g + 1) * 512], op=ALU.add),
                             reads=[pyk, xk], writes=[xk])
                    P.dma("sp", x1_d[it * 128:(it + 1) * 128, :], y1, reads=[xk], writes=["x1s%d" % it], chan="c:x1s%d" % slot)
            return mix_tile

        tile_fns = [make_slot(0), make_slot(1)]
        tilesA = []
        for b in range(NS):
            for c in range(NCH):
                it = b * NCH + c
                P.record()
                P.keymap = (SLOTKA, "_%d" % (it % 2))
                tile_fns[it % 2](b, c)
                P.keymap = None
                suf = P.stop()
                tilesA.append((G.pre, G.RM[0], G.RM[1], suf))
        import os as _os2
        _sa = _os2.environ.get('SCHEDA', '1')
        if _sa == '2':
            P.schedule([l for tl in tilesA for l in tl], win=10)
        elif _sa == '1':
            P.schedule([tl[0] + P.merge_list([tl[1], tl[2]]) + tl[3] for tl in tilesA], win=3)
        else:
            P.pipeline([tl[0] + P.merge_list([tl[1], tl[2]]) + tl[3] for tl in tilesA],
                       frac=float(_os2.environ.get('FRACA', '0.5')))
        ppset[0] = (0, 1, 2, 3)
        P.barrier()
        sb.off = mark0


    NB_ = NEXP * CAP

    class G:
        reg = None

    def breg(e):
        if G.reg is None:
            G.reg = nc.gpsimd.to_reg(NB_ - 1)
        return G.reg

    if "mix" in phases:
        phase_mix()

    def phase_xa():
        wq = sb.a([8, D], BF16)
        wo = sb.a([8, D], BF16)
        wkv = sb.a([8, 2 * D], BF16)
        for (wt, wd_, nm, ncol) in ((wq, wq_d, "wq", D), (wo, wo_d, "wo", D), (wkv, wkv_d, "wkv", 2 * D)):
            wv = wd_.rearrange("(kc p) n -> p kc n", p=128)
            for kc in range(8):
                for c0 in range(0, ncol, 1024):
                    P.dma("pool", wt[:, kc, c0:c0 + 1024], wv[:, kc, c0:c0 + 1024], writes=[nm])
        gxa = load_gain(1, "gxa")
        gmem = load_gain(2, "gmem")
        gmoe = load_gain(3, "gmoe")
        identf = sb.a([128], F32)
        P.dma("sp", identf, cd["identb"], writes=["identf"])
        triu = load_const("triu", BF16)
        ecap = load_const("ecap")
        onesb = sb.a([128], BF16)
        P.op("pool", lambda e: e.memset(onesb, 1.0), writes=["onesb"])
        epsc = sb.a([1], F32)
        P.op("pool", lambda e: e.memset(epsc, EPS), writes=["epsc"])
        wr = sb.a([8, 36], F32)
        P.dma("sp", wr, wr_d.rearrange("(kc p) n -> p kc n", p=128), writes=["wr"])
        rbb = sb.a([36], F32)
        P.dma("sp", rbb, rb_d, writes=["rb"])
        desti = sb.a([NT, 2], I32)
        wts = sb.a([NT, 2], F32)
        G.desti, G.wts = desti, wts
        G.mark_b = sb.off
        base = sb.a([NEXP], F32)
        P.op("pool", lambda e: e.memset(base, 0.0), writes=["base"])
        zt = sb.a([8 * D], BF16)
        P.op("pool", lambda e: e.memset(zt, 0.0), writes=["zt"])
        xsv = xs_d.rearrange("(n p r) d -> n p (r d)", p=128, r=8)
        for n_ in range(NB_ // 1024):
            P.dma("sp", xsv[n_], zt, reads=["zt"], writes=["xs_scr"], chan="c:xszero")
        SLOTK = {"xa", "junk", "ss", "rstd", "hb", "hT", "qxT", "mx4", "pe", "ssum", "pn", "pT", "oT", "x2t", "h2f", "h2b",
                 "h2T", "lg", "gmax", "goh", "ngmax", "gex", "gs", "pen", "lm", "m1", "oh0", "lm2", "m2", "oh1", "d12",
                 "cntb", "pos", "ovf", "tmp32", "destf"}

        def mkset(full):
            W = {}
            W["junk"] = sb.a([D], BF16)
            W["ss"] = sb.a([1], F32)
            W["rstd"] = sb.a([1], F32)
            W["hb"] = sb.a([D], BF16)
            W["xa"] = sb.a([D], F32)
            if not full:
                return W
            W["hT"] = sb.a([8, 128], BF16)
            W["qxT"] = sb.a([8, 128], BF16)
            W["mx4"] = sb.a([4], F32)
            W["pe"] = sb.a([4, 256], F32)
            W["ssum"] = sb.a([4], F32)
            W["pn"] = sb.a([4, 256], BF16)
            W["pT"] = sb.a([8, 128], BF16)
            W["oT"] = sb.a([8, 128], BF16)
            W["x2t"] = sb.a([D], F32)
            W["h2f"] = sb.a([D], F32)
            W["h2b"] = sb.a([D], BF16)
            W["h2T"] = sb.a([8, 128], F32)
            W["lg"] = sb.a([36], F32)
            W["r1"] = [sb.a([1], F32) for _ in range(8)]
            for nm_, n_ in (("goh", 4), ("gex", 4), ("pen", 4), ("lm2", 32), ("oh0", 32), ("oh1", 32), ("pos", 32),
                            ("ovf", 32), ("tmp32", 32), ("destf", 2)):
                W[nm_] = sb.a([n_], F32)
            W["lm"] = sb.a([4, 8], F32)
            W["cntb"] = sb.a([32], BF16)
            return W

        WS = [mkset(True), mkset(True), mkset(False)]
        memT = sb.a([8, 256], BF16)
        KTs = [sb.a([8, 256], BF16), sb.a([8, 256], BF16)]
        Vvs = [sb.a([2, D], BF16), sb.a([2, D], BF16)]

        def rmsnorm_b(W, src, srck, gain, gk, dst, dstk):
            junk, ss, rstd = W["junk"], W["ss"], W["rstd"]
            P.op("act", lambda e: e.activation(out=junk, in_=src, func=AF.Square, accum_out=ss),
                 reads=[srck], writes=["junk", "ss"])
            P.op("act", lambda e: e.activation(out=rstd, in_=ss, func=AF.Sqrt, bias=epsc, scale=1.0 / D),
                 reads=["ss", "epsc"], writes=["rstd"])
            P.op("dve", lambda e: e.reciprocal(out=rstd, in_=rstd), reads=["rstd"], writes=["rstd"])
            P.op("dve", lambda e: e.scalar_tensor_tensor(out=dst, in0=src, scalar=rstd, in1=gain,
                                                           op0=ALU.mult, op1=ALU.mult),
                 reads=[srck, "rstd", gk], writes=[dstk])

        def transpose8b(src, srck, dst, dstk):
            ptile, ptk = nextpt()
            for j in range(8):
                P.op("pe", lambda e, j=j: e.transpose(out=ptile[:, j, :], in_=src[:, j * 128:(j + 1) * 128],
                                                      identity=identb),
                     reads=[srck, "identb"], writes=[ptk], chain=True)
            P.op("act", lambda e: e.copy(out=dst, in_=ptile), reads=[ptk], writes=[dstk])

        def kv_seq(b):
            W = WS[2]
            KT, Vv = KTs[b % 2], Vvs[b % 2]
            ktk, vvk = "KT%d" % (b % 2), "Vv%d" % (b % 2)
            for mt in range(2):
                P.dma("sp", W["xa"], mem_d[b, mt * 128:(mt + 1) * 128, :], writes=["xa"])
                rmsnorm_b(W, W["xa"], "xa", gmem, "gmem", W["hb"], "hb")
                transpose8b(W["hb"], "hb", memT[:, :, mt * 128:(mt + 1) * 128], "memT")
            for half in range(2):
                pk, pkk = nextpp()
                for j4 in range(4):
                    jc = half * 4 + j4
                    for kc in range(8):
                        P.op("pe", lambda e, j4=j4, jc=jc, kc=kc, pk=pk: e.matmul(
                            pk[:, j4 * 256:(j4 + 1) * 256], lhsT=wkv[:, kc, jc * 128:(jc + 1) * 128],
                            rhs=memT[:, kc, :], start=(kc == 0), stop=(kc == 7)),
                            reads=["wkv", "memT"], writes=[pkk], chain=True)
                P.op("act", lambda e, half=half, pk=pk: e.copy(out=KT[:, half * 4:(half + 1) * 4, :].rearrange("p a b -> p (a b)"),
                                                               in_=pk[:, :]), reads=[pkk], writes=[ktk])
            for mt in range(2):
                pv, pvk = nextpp()
                for g in range(2):
                    for kc in range(8):
                        P.op("pe", lambda e, mt=mt, g=g, kc=kc, pv=pv: e.matmul(
                            pv[:, g * 512:(g + 1) * 512], lhsT=memT[:, kc, mt * 128:(mt + 1) * 128],
                            rhs=wkv[:, kc, D + g * 512:D + (g + 1) * 512], start=(kc == 0), stop=(kc == 7)),
                            reads=["wkv", "memT"], writes=[pvk], chain=True)
                P.op("act", lambda e, mt=mt, pv=pv: e.copy(out=Vv[:, mt, :], in_=pv[:, :]), reads=[pvk], writes=[vvk])

        def xa_tile(b, c):
            it = b * NCH + c
            W = WS[it % 2]
            KT, Vv = KTs[b % 2], Vvs[b % 2]
            ktk, vvk = "KT%d" % (b % 2), "Vv%d" % (b % 2)
            xt, hb, hT, qxT, mx4, pe_, ssum, pn, pT, oT = (W[k] for k in ("xa", "hb", "hT", "qxT", "mx4", "pe", "ssum", "pn", "pT", "oT"))
            x2t, h2f, h2b, h2T, lg = (W[k] for k in ("x2t", "h2f", "h2b", "h2T", "lg"))
            goh, gex, pen, lm, lm2, oh0, oh1, cntb, pos, ovf, tmp32, destf = (W[k] for k in (
                "goh", "gex", "pen", "lm", "lm2", "oh0", "oh1", "cntb", "pos", "ovf", "tmp32", "destf"))
            xk = "xa"
            P.dma("sp", xt, x1_d[it * 128:(it + 1) * 128, :], reads=["x1s%d" % it], writes=[xk])
            rmsnorm_b(W, xt, xk, gxa, "gxa", hb, "hb")
            transpose8b(hb, "hb", hT, "hT")
            pq, pqk_ = nextpp()
            for g in range(2):
                for kc in range(8):
                    P.op("pe", lambda e, g=g, kc=kc: e.matmul(pq[:, g * 512:(g + 1) * 512], lhsT=hT[:, kc, :],
                                                              rhs=wq[:, kc, g * 512:(g + 1) * 512],
                                                              start=(kc == 0), stop=(kc == 7)),
                         reads=["wq", "hT"], writes=[pqk_], chain=True)
            q_tok = W["junk"]
            P.op("act", lambda e: e.mul(out=q_tok, in_=pq[:, :], mul=1.0 / 16), reads=[pqk_], writes=["junk"])
            ptq_, ptqk_ = nextpt()
            for j in range(8):
                P.op("pe", lambda e, j=j: e.transpose(out=ptq_[:, j, :], in_=q_tok[:, j * 128:(j + 1) * 128], identity=identb),
                     reads=["junk", "identb"], writes=[ptqk_], chain=True)
            P.op("act", lambda e: e.copy(out=qxT, in_=ptq_), reads=[ptqk_], writes=["qxT"])
            pl, plk = nextpp()
            for hh in range(4):
                for i in range(2):
                    P.op("pe", lambda e, hh=hh, i=i: e.matmul(pl[:, hh * 256:(hh + 1) * 256], lhsT=qxT[:, 2 * hh + i, :],
                                                              rhs=KT[:, 2 * hh + i, :], start=(i == 0), stop=(i == 1)),
                         reads=["qxT", ktk], writes=[plk], chain=True)
            pl3 = pl[:, :].rearrange("p (a b) -> p a b", a=4)
            P.op("dve", lambda e: e.tensor_reduce(out=mx4, in_=pl3, axis=AX.X, op=ALU.max),
                 reads=[plk], writes=["mx4"])
            P.op("dve", lambda e: e.tensor_scalar(out=mx4, in0=mx4, scalar1=-1.0, scalar2=None, op0=ALU.mult),
                 reads=["mx4"], writes=["mx4"])
            for hh in range(4):
                P.op("act", lambda e, hh=hh: e.activation(out=pe_[:, hh, :], in_=pl3[:, hh, :], func=AF.Exp,
                                                          bias=mx4[:, hh:hh + 1], accum_out=ssum[:, hh:hh + 1]),
                     reads=[plk, "mx4"], writes=["pe", "ssum"])
            P.op("dve", lambda e: e.reciprocal(out=ssum, in_=ssum), reads=["ssum"], writes=["ssum"])
            P.op("dve", lambda e: e.tensor_tensor(out=pn, in0=pe_, in1=bc(ssum.unsqueeze(2), [128, 4, 256]), op=ALU.mult),
                 reads=["pe", "ssum"], writes=["pn"])
            pn2 = pn.rearrange("p a b -> p (a b)")
            ptp, ptpk = nextpt()
            for j in range(8):
                P.op("pe", lambda e, j=j: e.transpose(out=ptp[:, j, :], in_=pn2[:, j * 128:(j + 1) * 128], identity=identb),
                     reads=["pn", "identb"], writes=[ptpk], chain=True)
            P.op("act", lambda e: e.copy(out=pT, in_=ptp), reads=[ptpk], writes=["pT"])
            po, pok = nextpp()
            for hh in range(4):
                for dcc_ in range(2):
                    j = hh * 2 + dcc_
                    for mc in range(2):
                        P.op("pe", lambda e, hh=hh, dcc_=dcc_, j=j, mc=mc: e.matmul(
                            po[:, j * 128:(j + 1) * 128], lhsT=Vv[:, mc, hh * 256 + dcc_ * 128:hh * 256 + (dcc_ + 1) * 128],
                            rhs=pT[:, hh * 2 + mc, :], start=(mc == 0), stop=(mc == 1)),
                            reads=[vvk, "pT"], writes=[pok], chain=True)
            P.op("act", lambda e: e.copy(out=oT.rearrange("p a b -> p (a b)"), in_=po[:, :]), reads=[pok], writes=["oT"])
            py, pyk = nextpp()
            for g in range(2):
                for kc in range(8):
                    P.op("pe", lambda e, g=g, kc=kc: e.matmul(py[:, g * 512:(g + 1) * 512], lhsT=oT[:, kc, :],
                                                              rhs=wo[:, kc, g * 512:(g + 1) * 512],
                                                              start=(kc == 0), stop=(kc == 7)),
                         reads=["oT", "wo"], writes=[pyk], chain=True)
            for g in range(2):
                P.op("dve", lambda e, g=g: e.tensor_tensor(out=x2t[:, g * 512:(g + 1) * 512], in0=py[:, g * 512:(g + 1) * 512],
                                                           in1=xt[:, g * 512:(g + 1) * 512], op=ALU.add),
                     reads=[pyk, xk], writes=["x2t"])
            P.dma("sp", x2_d[it * 128:(it + 1) * 128, :], x2t, reads=["x2t"], writes=["x2s%d" % it], chan="c:x2t")
            if "moe" not in phases:
                return
            rmsnorm_b(W, x2t, "x2t", gmoe, "gmoe", h2f, "h2f")
            P.op("pool", lambda e: e.tensor_copy(out=h2b, in_=h2f), reads=["h2f"], writes=["h2b"])
            ph, phk = nextpp()
            for j in range(8):
                P.op("pe", lambda e, j=j: e.transpose(out=ph[:, j * 128:(j + 1) * 128], in_=h2f[:, j * 128:(j + 1) * 128],
                                                      identity=identf), reads=["h2f", "identf"], writes=[phk], f32=True)
            P.op("act", lambda e: e.copy(out=h2T.rearrange("p a b -> p (a b)"), in_=ph[:, :]), reads=[phk], writes=["h2T"])
            pr, prk = nextpp()
            for kc in range(8):
                P.op("pe", lambda e, kc=kc: e.matmul(pr[:, 0:36], lhsT=h2T[:, kc, :], rhs=wr[:, kc, :],
                                                     start=(kc == 0), stop=(kc == 7)), reads=["h2T", "wr"], writes=[prk], f32=True)
            P.op("dve", lambda e: e.tensor_tensor(out=lg, in0=pr[:, 0:36], in1=rbb, op=ALU.add),
                 reads=[prk, "rb"], writes=["lg"])
            gmax, ngmax, gs, m1, m2, d12, w0t, _ = W["r1"]
            V_ = lambda fn, r, w: P.op("dve", fn, reads=r, writes=w)
            V_(lambda e: e.tensor_reduce(out=gmax, in_=lg[:, 0:4], axis=AX.X, op=ALU.max), ["lg"], ["gmax"])
            V_(lambda e: e.tensor_scalar(out=goh, in0=lg[:, 0:4], scalar1=gmax, scalar2=None, op0=ALU.is_equal),
               ["lg", "gmax"], ["goh"])
            V_(lambda e: e.tensor_scalar(out=ngmax, in0=gmax, scalar1=-1.0, scalar2=None, op0=ALU.mult), ["gmax"], ["ngmax"])
            P.op("act", lambda e: e.activation(out=gex, in_=lg[:, 0:4], func=AF.Exp, bias=ngmax, accum_out=gs),
                 reads=["lg", "ngmax"], writes=["gex", "gs"])
            V_(lambda e: e.reciprocal(out=gs, in_=gs), ["gs"], ["gs"])
            V_(lambda e: e.tensor_scalar(out=pen, in0=goh, scalar1=-1.0, scalar2=1e9, op0=ALU.add, op1=ALU.mult),
               ["goh"], ["pen"])
            V_(lambda e: e.tensor_tensor(out=lm, in0=lg[:, 4:36].rearrange("p (a b) -> p a b", a=4),
                                         in1=bc(pen.unsqueeze(2), [128, 4, 8]), op=ALU.add), ["lg", "pen"], ["lm"])
            lmf = lm.rearrange("p a b -> p (a b)")
            V_(lambda e: e.tensor_reduce(out=m1, in_=lmf, axis=AX.X, op=ALU.max), ["lm"], ["m1"])
            V_(lambda e: e.tensor_scalar(out=oh0, in0=lmf, scalar1=m1, scalar2=None, op0=ALU.is_equal), ["lm", "m1"], ["oh0"])
            V_(lambda e: e.scalar_tensor_tensor(out=lm2, in0=oh0, scalar=-2e9, in1=lmf, op0=ALU.mult, op1=ALU.add),
               ["oh0", "lm"], ["lm2"])
            V_(lambda e: e.tensor_reduce(out=m2, in_=lm2, axis=AX.X, op=ALU.max), ["lm2"], ["m2"])
            V_(lambda e: e.tensor_scalar(out=oh1, in0=lm2, scalar1=m2, scalar2=None, op0=ALU.is_equal), ["lm2", "m2"], ["oh1"])
            V_(lambda e: e.tensor_tensor(out=d12, in0=m2, in1=m1, op=ALU.subtract), ["m1", "m2"], ["d12"])
            P.op("act", lambda e: e.activation(out=d12, in_=d12, func=AF.Exp), reads=["d12"], writes=["d12"])
            V_(lambda e: e.tensor_scalar(out=d12, in0=d12, scalar1=1.0, scalar2=None, op0=ALU.add), ["d12"], ["d12"])
            V_(lambda e: e.reciprocal(out=d12, in_=d12), ["d12"], ["d12"])
            V_(lambda e: e.tensor_tensor(out=wts[:, it, 0:1], in0=d12, in1=gs, op=ALU.mult), ["d12", "gs"], ["wts%d" % it])
            V_(lambda e: e.tensor_tensor(out=wts[:, it, 1:2], in0=gs, in1=wts[:, it, 0:1], op=ALU.subtract),
               ["gs", "wts%d" % it], ["wts%d" % it])
            V_(lambda e: e.tensor_tensor(out=cntb, in0=oh0, in1=oh1, op=ALU.add), ["oh0", "oh1"], ["cntb"])
            pp_, ppk = nextpp()
            P.op("pe", lambda e: e.matmul(pp_[:, 0:32], lhsT=triu, rhs=cntb, start=True, stop=True),
                 reads=["triu", "cntb"], writes=[ppk], chain=True)
            P.op("pe", lambda e: e.matmul(pp_[:, 32:64], lhsT=onesb, rhs=cntb, start=True, stop=True),
                 reads=["onesb", "cntb"], writes=[ppk], chain=True)
            V_(lambda e: e.tensor_tensor(out=pos, in0=pp_[:, 0:32], in1=base, op=ALU.add), [ppk, "base"], ["pos"])
            V_(lambda e: e.tensor_tensor(out=base, in0=pp_[:, 32:64], in1=base, op=ALU.add), [ppk, "base"], ["base"])
            V_(lambda e: e.tensor_scalar(out=ovf, in0=pos, scalar1=float(CAP), scalar2=1e6, op0=ALU.is_ge, op1=ALU.mult),
               ["pos"], ["ovf"])
            V_(lambda e: e.tensor_tensor(out=pos, in0=pos, in1=ecap, op=ALU.add), ["pos", "ecap"], ["pos"])
            V_(lambda e: e.tensor_tensor(out=pos, in0=pos, in1=ovf, op=ALU.add), ["pos", "ovf"], ["pos"])
            V_(lambda e: e.scalar_tensor_tensor(out=tmp32, in0=oh0, scalar=1.0, in1=pos, op0=ALU.mult, op1=ALU.mult,
                                                accum_out=destf[:, 0:1]), ["oh0", "pos"], ["tmp32", "destf"])
            V_(lambda e: e.scalar_tensor_tensor(out=tmp32, in0=oh1, scalar=1.0, in1=pos, op0=ALU.mult, op1=ALU.mult,
                                                accum_out=destf[:, 1:2]), ["oh1", "pos"], ["tmp32", "destf"])
            V_(lambda e: e.tensor_copy(out=desti[:, it, :], in_=destf), ["destf"], ["desti%d" % it])
            for k in range(2):
                P.dma_fn("pool", lambda e, k=k: nc.gpsimd.indirect_dma_start(
                    out=xs_d, out_offset=bass.IndirectOffsetOnAxis(ap=desti[:, it, k:k + 1], axis=0),
                    in_=h2b, in_offset=None, bounds_check=breg(e), oob_is_err=False),
                    reads=["h2b", "desti%d" % it, "xs_scr"], writes=["xsd%d_%d" % (it, k)], chan="c:xsd")

        tiles = []
        for b in range(NS):
            for c in range(NCH):
                it = b * NCH + c
                P.record()
                if c == 0:
                    P.keymap = (SLOTK, "_2")
                    ppset[0] = (0, 1) if it % 2 == 0 else (2, 3)
                    kv_seq(b)
                P.keymap = (SLOTK, "_%d" % (it % 2))
                ppset[0] = (0, 1) if it % 2 == 0 else (2, 3)
                xa_tile(b, c)
                P.keymap = None
                tiles.append(P.stop())
        import os as _os3
        if _os3.environ.get('SCHED', '1') == '1':
            P.schedule(tiles)
        else:
            P.pipeline(tiles, frac=float(_os3.environ.get('FRACB', '0.5')))
        ppset[0] = (0, 1, 2, 3)
        P.barrier()
        sb.off = G.mark_b

    if "xa" in phases:
        phase_xa()

    def phase_moe():
        desti, wts = G.desti, G.wts
        GS = 256 if CAP % 256 == 0 else 128
        wbuf = [(sb.a([8, 512], BF16), sb.a([8, 512], BF16), sb.a([4, D], BF16)) for _ in range(2)]
        NTB = GS // 128
        xb = [[sb.a([D], BF16) for _ in range(NTB)] for _ in range(2)]
        xbT_ = [sb.a([8, GS], BF16) for _ in range(2)]
        sgl_ = [sb.a([4 * GS], F32) for _ in range(2)]
        hid_ = [sb.a([4, GS], BF16) for _ in range(2)]
        ysb = [sb.a([D], BF16), sb.a([D], BF16)]
        NG = CAP // GS

        def load_w(e_):
            wg_t, wu_t, wd_t = wbuf[e_ % 2]
            k = "wb%d" % (e_ % 2)
            P.dma("pool", wg_t, wg_d[e_].rearrange("(kc p) n -> p kc n", p=128), writes=[k])
            P.dma("pool", wu_t, wu_d[e_].rearrange("(kc p) n -> p kc n", p=128), writes=[k])
            P.dma("pool", wd_t, wd_d[e_].rearrange("(kc p) n -> p kc n", p=128), writes=[k])

        def load_x(gi):
            e_, grp = divmod(gi, NG)
            r0 = e_ * CAP + grp * GS
            for tb in range(NTB):
                P.dma("sp", xb[gi % 2][tb], xs_d[r0 + tb * 128:r0 + (tb + 1) * 128, :], reads=["xs_scr"],
                      writes=["xb%d_%d" % (gi % 2, tb)])

        def group(gi):
            e_, grp = divmod(gi, NG)
            wg_t, wu_t, wd_t = wbuf[e_ % 2]
            wk = "wb%d" % (e_ % 2)
            r0 = e_ * CAP + grp * GS
            xbT, sgl, hid = xbT_[gi % 2], sgl_[gi % 2], hid_[gi % 2]
            xbTk, sglk, hidk = "xbT%d" % (gi % 2), "sgl%d" % (gi % 2), "hid%d" % (gi % 2)
            for tb in range(NTB):
                xt_ = xb[gi % 2][tb]
                xk_ = "xb%d_%d" % (gi % 2, tb)
                ptx, ptxk = nextpt()
                for j in range(8):
                    P.op("pe", lambda e, j=j, xt_=xt_, ptx=ptx: e.transpose(out=ptx[:, j, :],
                                                                            in_=xt_[:, j * 128:(j + 1) * 128], identity=identb),
                         reads=[xk_, "identb"], writes=[ptxk], chain=True)
                P.op("act", lambda e, tb=tb, ptx=ptx: e.copy(out=xbT[:, :, tb * 128:(tb + 1) * 128], in_=ptx),
                     reads=[ptxk], writes=[xbTk])
            pa, pak = nextpp()
            pb_, pbk = nextpp()
            for (pdst, pdk, wt_) in ((pa, pak, wg_t), (pb_, pbk, wu_t)):
                for fc in range(4):
                    for kc in range(8):
                        P.op("pe", lambda e, pdst=pdst, wt_=wt_, fc=fc, kc=kc: e.matmul(
                            pdst[:, fc * GS:(fc + 1) * GS], lhsT=wt_[:, kc, fc * 128:(fc + 1) * 128], rhs=xbT[:, kc, :],
                            start=(kc == 0), stop=(kc == 7)), reads=[wk, xbTk], writes=[pdk], chain=True)
            P.op("act", lambda e, pa=pa: e.activation(out=sgl, in_=pa[:, 0:4 * GS], func=AF.Silu), reads=[pak], writes=[sglk])
            P.op("dve", lambda e, pb_=pb_: e.tensor_tensor(out=hid.rearrange("p a b -> p (a b)"), in0=pb_[:, 0:4 * GS], in1=sgl,
                                                           op=ALU.mult), reads=[pbk, sglk], writes=[hidk])
            for tb in range(NTB):
                pc_, pck_ = nextpp()
                for g in range(2):
                    for fc in range(4):
                        P.op("pe", lambda e, g=g, fc=fc, tb=tb, pc_=pc_: e.matmul(
                            pc_[:, g * 512:(g + 1) * 512], lhsT=hid[:, fc, tb * 128:(tb + 1) * 128],
                            rhs=wd_t[:, fc, g * 512:(g + 1) * 512], start=(fc == 0), stop=(fc == 3)),
                            reads=[hidk, wk], writes=[pck_], chain=True)
                yi = tb % 2
                if tb % 2 == 0:
                    P.op("act", lambda e, yi=yi, pc_=pc_: e.copy(out=ysb[yi], in_=pc_[:, :]), reads=[pck_], writes=["ysb%d" % yi])
                else:
                    P.op("dve", lambda e, yi=yi, pc_=pc_: e.tensor_copy(out=ysb[yi], in_=pc_[:, :]), reads=[pck_],
                         writes=["ysb%d" % yi])
                P.dma("pool", ys_d[r0 + tb * 128:r0 + (tb + 1) * 128, :], ysb[yi], reads=["ysb%d" % yi],
                      writes=["ys_scr"], chan="c:ysb%d" % yi)

        load_w(0)
        glists = []
        for gi in range(NEXP * NG):
            e_, grp = divmod(gi, NG)
            P.record()
            if grp == 0 and e_ + 1 < NEXP:
                load_w(e_ + 1)
            load_x(gi)
            ppset[0] = (0, 1) if gi % 2 == 0 else (2, 3)
            group(gi)
            glists.append(P.stop())
        ppset[0] = (0, 1, 2, 3)
        P.schedule(glists, win=3)
        P.barrier()
        sb.off = G.mark_b
        gfin = load_gain(4, "gfin")
        epsd = sb.a([1], F32)
        P.op("pool", lambda e: e.memset(epsd, EPS), writes=["epsd"])
        xd = [sb.a([D], F32), sb.a([D], F32)]
        yk = [[sb.a([D], BF16), sb.a([D], BF16)] for _ in range(2)]
        zt2 = [sb.a([D], F32), sb.a([D], F32)]
        junkd2 = [sb.a([D], BF16), sb.a([D], BF16)]
        ssd2 = [sb.a([1], F32), sb.a([1], F32)]
        rsd2 = [sb.a([1], F32), sb.a([1], F32)]
        ot = [sb.a([D], F32), sb.a([D], F32)]
        for par in range(2):
            for k in range(2):
                P.op("pool", lambda e, par=par, k=k: e.memset(yk[par][k], 0.0), writes=["yk%d%d" % (par, k)])

        def fin_load(it):
            par = it % 2
            P.dma("sp", xd[par], x2_d[it * 128:(it + 1) * 128, :], reads=["x2s%d" % it], writes=["xd%d" % par])
            for k in range(2):
                P.dma_fn("pool", lambda e, k=k: nc.gpsimd.indirect_dma_start(
                    out=yk[par][k], out_offset=None, in_=ys_d,
                    in_offset=bass.IndirectOffsetOnAxis(ap=desti[:, it, k:k + 1], axis=0),
                    bounds_check=breg(e), oob_is_err=False),
                    reads=["ys_scr"], writes=["yk%d%d" % (par, k)])

        def fin_tile(it):
            b, c = divmod(it, NCH)
            par = it % 2
            zt_, junkd, ssd, rsd = zt2[par], junkd2[par], ssd2[par], rsd2[par]
            P.keymap = ({"zt_", "junkd", "ssd", "rsd"}, str(par))
            try:
                fin_tile_(it, b, c, par, zt_, junkd, ssd, rsd)
            finally:
                P.keymap = None

        def fin_tile_(it, b, c, par, zt_, junkd, ssd, rsd):
            P.op("dve", lambda e: e.scalar_tensor_tensor(out=zt_, in0=yk[par][0], scalar=wts[:, it, 0:1], in1=xd[par],
                                                         op0=ALU.mult, op1=ALU.add),
                 reads=["yk%d0" % par, "xd%d" % par], writes=["zt_"])
            P.op("dve", lambda e: e.scalar_tensor_tensor(out=zt_, in0=yk[par][1], scalar=wts[:, it, 1:2], in1=zt_,
                                                          op0=ALU.mult, op1=ALU.add),
                 reads=["yk%d1" % par, "zt_"], writes=["zt_"])
            P.op("act", lambda e: e.activation(out=junkd, in_=zt_, func=AF.Square, accum_out=ssd),
                 reads=["zt_"], writes=["junkd", "ssd"])
            P.op("act", lambda e: e.activation(out=rsd, in_=ssd, func=AF.Sqrt, bias=epsd, scale=1.0 / D),
                 reads=["ssd", "epsd"], writes=["rsd"])
            P.op("dve", lambda e: e.reciprocal(out=rsd, in_=rsd), reads=["rsd"], writes=["rsd"])
            P.op("dve", lambda e: e.scalar_tensor_tensor(out=ot[par], in0=zt_, scalar=rsd, in1=gfin, op0=ALU.mult,
                                                         op1=ALU.mult), reads=["zt_", "rsd", "gfin"], writes=["ot%d" % par])
            P.dma("sp", out_d[b, c * 128:(c + 1) * 128, :], ot[par], reads=["ot%d" % par], writes=["out%d" % it],
                  chan="c:ot%d" % par)

        flists = []
        for it in range(NT):
            P.record()
            fin_load(it)
            fin_tile(it)
            flists.append(P.stop())
        P.schedule(flists, win=3)
        P.wait_for("sp", ["out%d" % it for it in range(NT)])

    if "moe" in phases:
        phase_moe()
    else:
        src_d, skey = (x2_d, "x2s%d") if "xa" in phases else (x1_d, "x1s%d")
        t = sb.a([D], F32)
        for it in range(NT):
            b, c = divmod(it, NCH)
            P.dma("sp", t, src_d[it * 128:(it + 1) * 128, :], reads=[skey % it], writes=["dbgt"])
            P.dma("sp", out_d[b, c * 128:(c + 1) * 128, :], t, reads=["dbgt"], writes=["out%d" % it], chan="c:out")
        P.wait_for("sp", ["out%d" % it for it in range(NT)])

    P.emit(st)
    st.close()
    return nc, P, sb


_CACHE = {}


def _host_layout(inp):
    f = lambda a: np.ascontiguousarray(np.asarray(a, dtype=np.float32))
    m = {}
    m["w_in"] = f(inp["w_in"][0])
    m["w_out"] = f(inp["w_out"][0])
    m["xa_wq"] = f(inp["xa_wq"][0])
    m["xa_wkv"] = f(inp["xa_wkv"][0])
    m["xa_wo"] = f(inp["xa_wo"][0])
    m["moe_w_gate"] = f(inp["moe_w_gate"][0])
    m["moe_w_up"] = f(inp["moe_w_up"][0])
    m["moe_w_down"] = f(inp["moe_w_down"][0])
    hn = np.concatenate([np.asarray(inp["ret_norm_w"][0]), np.asarray(inp["ml_norm_w"][0])])
    g = np.stack([np.asarray(inp["norm_mix_w"][0]), np.asarray(inp["norm_xa_w"][0]), np.asarray(inp["norm_mem_w"][0]),
                  np.asarray(inp["norm_moe_w"][0]), np.asarray(inp["norm_final_w"]), hn])
    m["gains"] = f(np.broadcast_to(g[:, None, :], (6, 128, D)))
    cw = np.concatenate([np.asarray(inp["ml_conv_w"][0]), np.asarray(inp["ml_conv_b"][0])[None]], 0)
    m["convw"] = f(cw.reshape(5, 8, 128).transpose(2, 1, 0))
    m["gateb"] = f(np.asarray(inp["ml_gate_b"][0]).reshape(2, 4).T)
    m["wr"] = f(np.concatenate([np.asarray(inp["moe_w_group"][0]), np.asarray(inp["moe_w_router"][0])], 1))
    rb = np.concatenate([np.asarray(inp["moe_b_group"][0]), np.asarray(inp["moe_b_router"][0])])
    m["rb"] = f(np.broadcast_to(rb[None], (128, 36)))
    for k, v in host_consts().items():
        m["c_" + k] = v
    return m


def kernel(**inputs):
    ncores = 8
    x = np.asarray(inputs["x"], dtype=np.float32)
    mem = np.asarray(inputs["mem"], dtype=np.float32)
    B = x.shape[0]
    NS = B // ncores
    NCH = x.shape[1] // 128
    key = (NS, NCH)
    if key not in _CACHE:
        _CACHE[key] = build_program(NS, NCH)[0]
    nc = _CACHE[key]
    shared = _host_layout(inputs)
    in_maps = []
    for c in range(ncores):
        m = dict(shared)
        m["x"] = np.ascontiguousarray(x[c * NS:(c + 1) * NS])
        m["mem"] = np.ascontiguousarray(mem[c * NS:(c + 1) * NS])
        in_maps.append(m)
    res = run_bass_kernel_spmd(nc, in_maps, core_ids=list(range(ncores)))
    return np.concatenate([r["out"] for r in res.results], axis=0).astype(np.float32)
```

```python
from contextlib import ExitStack
import numpy as np
import concourse.bass as bass
import concourse.mybir as mybir
from concourse.bass_utils import run_bass_kernel_spmd

F32 = mybir.dt.float32
BF16 = mybir.dt.bfloat16
I32 = mybir.dt.int32
U8 = mybir.dt.uint8
ALU = mybir.AluOpType
AF = mybir.ActivationFunctionType
AX = mybir.AxisListType

ENGS = ("pe", "act", "dve", "pool", "sp")
D = 1024
S = 2048
CAP = 768
NEXP = 32
EPS = 1e-6


class _Op:
    __slots__ = ("eng", "fn", "deps", "is_dma", "chan", "sig", "val", "waits", "bar", "f32", "chain")


class Prog:
    def __init__(self, nc):
        self.nc = nc
        self.ops = []
        self.last_w = {}
        self.readers = {}
        self._rec = None
        self._stack = []
        self.keymap = None

    def record(self):
        self._stack.append(self._rec)
        self._rec = []

    def stop(self):
        r = self._rec
        self._rec = self._stack.pop()
        return r

    def replay(self, items):
        for it in items:
            self._add(*it)

    @staticmethod
    def _block_heads(l):
        heads = []
        for j, it in enumerate(l):
            if j > 0 and it[0] == "pe" and l[j - 1][0] == "pe" and it[3] and l[j - 1][3] and it[3][0] == l[j - 1][3][0]:
                heads.append(heads[-1])
            else:
                heads.append(j)
        return heads

    def merge_list(self, lists):
        allops = []
        for li, l in enumerate(lists):
            n = max(len(l), 1)
            hd = self._block_heads(l)
            for j, it in enumerate(l):
                allops.append(((hd[j] + 0.5) / n, li, j, it))
        allops.sort(key=lambda t: (t[0], t[1], t[2]))
        return [t[3] for t in allops]

    def merge(self, lists):
        allops = []
        for li, l in enumerate(lists):
            n = max(len(l), 1)
            hd = self._block_heads(l)
            for j, it in enumerate(l):
                allops.append(((hd[j] + 0.5) / n, li, j, it))
        allops.sort(key=lambda t: (t[0], t[1], t[2]))
        self.replay([t[3] for t in allops])

    def schedule(self, lists, win=3):
        from collections import defaultdict
        COST = {"pe": 0.16, "act": 0.45, "dve": 0.35, "pool": 0.9, "sp": 0.05}
        lists = [[(it[0], it[1], self._expand(it[2]), self._expand(it[3]), it[4], it[5]) for it in l] for l in lists]
        written = set()
        for l in lists:
            for it in l:
                written.update(it[3])
        cnt = defaultdict(lambda: defaultdict(int))
        opkeys = []
        for t, l in enumerate(lists):
            ok = []
            for it in l:
                ks = [k for k in set(it[2]) | set(it[3]) if k in written]
                ok.append(ks)
                for k in ks:
                    cnt[k][t] += 1
            opkeys.append(ok)
        tiles_of = {k: sorted(d) for k, d in cnt.items()}
        fptr = {k: 0 for k in cnt}

        def front(k):
            arr = tiles_of[k]
            p = fptr[k]
            while p < len(arr) and cnt[k][arr[p]] == 0:
                p += 1
            fptr[k] = p
            return arr[p] if p < len(arr) else 1 << 30

        heads = [self._block_heads(l) for l in lists]
        pos = [0] * len(lists)
        eng_free = defaultdict(float)
        kw = defaultdict(float)
        kr = defaultdict(float)
        order = []
        lo = 0
        n = len(lists)
        while lo < n:
            while lo < n and pos[lo] >= len(lists[lo]):
                lo += 1
            if lo >= n:
                break
            best = None
            for t in range(lo, min(n, lo + win)):
                if pos[t] >= len(lists[t]):
                    continue
                j = pos[t]
                blocked = False
                jj = j
                while True:
                    if any(front(k) < t for k in opkeys[t][jj]):
                        blocked = True
                        break
                    jj += 1
                    if jj >= len(lists[t]) or heads[t][jj] != heads[t][j]:
                        break
                if blocked:
                    continue
                it = lists[t][j]
                st_ = eng_free[it[0]]
                for r in it[2]:
                    st_ = max(st_, kw[r])
                for w in it[3]:
                    st_ = max(st_, kw[w], kr[w])
                if best is None or st_ < best[0]:
                    best = (st_, t)
            t = best[1]
            j = pos[t]
            hd = heads[t][j]
            while True:
                it = lists[t][j]
                eng = it[0]
                st_ = eng_free[eng]
                for r in it[2]:
                    st_ = max(st_, kw[r])
                for w in it[3]:
                    st_ = max(st_, kw[w], kr[w])
                if it[4]:
                    eng_free[eng] = st_ + 0.05
                    fin = st_ + 2.5
                else:
                    fin = st_ + COST.get(eng, 0.3)
                    eng_free[eng] = fin
                for r in it[2]:
                    kr[r] = max(kr[r], fin + 0.1)
                for w in it[3]:
                    kw[w] = fin + 0.1
                    kr[w] = 0.0
                for k in opkeys[t][j]:
                    cnt[k][t] -= 1
                order.append(it)
                j += 1
                if j >= len(lists[t]) or heads[t][j] != hd:
                    break
            pos[t] = j
        self.replay(order)

    def pipeline(self, lists, frac=0.5):
        allops = []
        L = max(len(l) for l in lists)
        stride = L * frac
        for t, l in enumerate(lists):
            hd = self._block_heads(l)
            for j, it in enumerate(l):
                allops.append((t * stride + hd[j], t, j, it))
        allops.sort(key=lambda t: (t[0], t[1], t[2]))
        self.replay([t[3] for t in allops])

    def _add(self, eng, fn, reads, writes, is_dma=False, chan=None):
        if self.keymap is not None:
            ks, sfx = self.keymap
            reads = [k + sfx if k in ks else k for k in reads]
            writes = [k + sfx if k in ks else k for k in writes]
            if chan is not None and chan != "f32" and not chan.startswith("chain") and chan[2:] in ks:
                chan = chan + sfx
        if self._rec is not None:
            km, self.keymap = self.keymap, None
            self._rec.append((eng, fn, reads, writes, is_dma, chan))
            self.keymap = km
            return None
        km, self.keymap = self.keymap, None
        try:
            return self._add2(eng, fn, reads, writes, is_dma, chan)
        finally:
            self.keymap = km

    @staticmethod
    def _expand(keys):
        out = []
        for k in keys:
            if len(k) == 3 and k[:2] == "pp" and k[2].isdigit():
                out.append(k + "a")
                out.append(k + "b")
            else:
                out.append(k)
        return out

    def _add2(self, eng, fn, reads, writes, is_dma=False, chan=None):
        reads = self._expand(reads)
        writes = self._expand(writes)
        o = _Op()
        o.eng, o.fn, o.is_dma, o.chan = eng, fn, is_dma, chan
        o.f32 = (not is_dma) and chan == "f32"
        o.chain = chan[5:] if ((not is_dma) and chan is not None and chan.startswith("chain")) else ""
        if o.f32 and eng == "pe":
            o.chain = "A"
        o.sig = False
        o.val = None
        if eng == "pe" and not is_dma:
            lp = getattr(self, "_last_pe", None)
            self._last_pe = o
        else:
            lp = None
        deps = []
        for k in reads:
            w = self.last_w.get(k)
            if w is not None:
                deps.append(w)
        keep_readers = set()
        for k in writes:
            w = self.last_w.get(k)
            if w is not None:
                if is_dma and w.is_dma and w.chan == chan:
                    keep_readers.add(k)
                else:
                    deps.append(w)
            deps.extend(self.readers.get(k, ()))
        seen = set()
        o.deps = []
        for d in deps:
            if d is o or id(d) in seen:
                continue
            if o.chain and getattr(d, "chain", "") == o.chain:
                continue
            seen.add(id(d))
            o.deps.append(d)
        for k in writes:
            self.last_w[k] = o
            if k not in keep_readers:
                self.readers[k] = []
        for k in reads:
            if k not in writes:
                self.readers.setdefault(k, []).append(o)
        self.ops.append(o)
        return o

    def op(self, eng, fn, reads=(), writes=(), f32=False, chain=False):
        if chain is True:
            chain = "A"
        return self._add(eng, fn, list(reads), list(writes), False, "f32" if f32 else (("chain" + chain) if chain else None))

    def dma(self, eng, out, in_, reads=(), writes=(), chan=None, **kw):
        reads, writes = list(reads), list(writes)
        if chan is None:
            chan = "c:" + (writes[0] if writes else reads[0])
        nc = self.nc
        q = {"pool": nc.gpsimd, "sp": nc.sync, "act": nc.scalar}[eng]
        fn = lambda e: q.dma_start(out=out, in_=in_, **kw)
        return self._add(eng, fn, reads, writes, is_dma=True, chan=chan)

    def dma_fn(self, eng, fn, reads=(), writes=(), chan=None):
        reads, writes = list(reads), list(writes)
        if chan is None:
            chan = "c:" + (writes[0] if writes else reads[0])
        return self._add(eng, fn, reads, writes, is_dma=True, chan=chan)

    def wait_for(self, eng, keys):
        return self._add(eng, None, list(keys), [])

    def barrier(self):
        last = {}
        for o in self.ops:
            if o.fn is None:
                continue
            last[(o.chan if o.is_dma else o.eng)] = o
        deps = list(last.values())
        for e in ENGS:
            o = _Op()
            o.eng, o.fn, o.is_dma, o.chan, o.sig, o.val = e, None, False, None, False, None
            o.deps = deps
            o.f32 = False
            o.chain = ""
            o.waits = None
            self.ops.append(o)

    def emit(self, stack):
        nc = self.nc
        for o in self.ops:
            for d in o.deps:
                d.sig = True
        cnt = {e: 0 for e in ENGS}
        chan_cnt = {}
        for o in self.ops:
            if o.fn is None:
                continue
            if o.is_dma:
                chan_cnt[o.chan] = chan_cnt.get(o.chan, 0) + 16
                o.val = chan_cnt[o.chan]
            elif o.sig:
                cnt[o.eng] += 1
                o.val = cnt[o.eng]
        sems = {}
        for e in ENGS:
            sems["e:" + e] = stack.enter_context(nc.semaphore("sem_" + e))
        for i, c in enumerate(sorted(chan_cnt)):
            sems[c] = stack.enter_context(nc.semaphore("semc_%d" % i))
        self.n_sems = len(sems)
        waited = {e: {} for e in ENGS}
        per_eng = {e: [] for e in ENGS}
        for o in self.ops:
            ws = {}
            for d in o.deps:
                if d.val is None:
                    continue
                s = d.chan if d.is_dma else "e:" + d.eng
                if ws.get(s, 0) < d.val:
                    ws[s] = d.val
            o.waits = []
            isbar = (o.fn is None and len(o.deps) > 8)
            for s, v in ws.items():
                if isbar or waited[o.eng].get(s, 0) < v:
                    waited[o.eng][s] = v
                    o.waits.append((s, v))
            per_eng[o.eng].append(o)
        block = stack.enter_context(nc.Block())
        self.counts = {e: len(per_eng[e]) for e in ENGS}

        def run(e, eng):
            for o in per_eng[e]:
                for s, v in o.waits:
                    eng.wait_ge(sems[s], v)
                if o.fn is None:
                    continue
                ins = o.fn(eng)
                if o.is_dma:
                    ins.then_inc(sems[o.chan], 16)
                elif o.sig:
                    ins.then_inc(sems["e:" + e], 1)

        @block.tensor
        def _(eng):
            run("pe", eng)

        @block.scalar
        def _(eng):
            run("act", eng)

        @block.vector
        def _(eng):
            run("dve", eng)

        @block.gpsimd
        def _(eng):
            run("pool", eng)

        @block.sync
        def _(eng):
            run("sp", eng)


_DTSZ = {F32: 4, BF16: 2, I32: 4}


class SB:
    def __init__(self, nc, nbytes):
        self.t = nc.alloc_sbuf_tensor("sbuf_all", [128, nbytes], U8)
        self.off = 0
        self.cap = nbytes
        self.hi = 0

    def a(self, free, dt, parts=128):
        if isinstance(free, int):
            free = [free]
        n = int(np.prod(free)) * _DTSZ[dt]
        off = (self.off + 31) // 32 * 32
        self.off = off + n
        self.hi = max(self.hi, self.off)
        assert self.off <= self.cap, ("SBUF overflow", self.off, self.cap)
        ap = self.t[0:parts, off:off + n].bitcast(dt)
        if len(free) == 2:
            ap = ap.rearrange("p (a b) -> p a b", a=free[0], b=free[1])
        elif len(free) == 3:
            ap = ap.rearrange("p (a b c) -> p a b c", a=free[0], b=free[1], c=free[2])
        return ap


def bc(ap, shape):
    return ap.to_broadcast(list(shape))


def host_consts():
    c = {}
    c["identb"] = np.eye(128, dtype=np.float32)
    log_g = np.log1p(-np.exp2(-5.0 - np.arange(4, dtype=np.float64)))
    n = np.arange(128, dtype=np.float64)
    diff = n[:, None] - n[None, :]
    dmat = np.where(diff[None] >= 0, np.exp(log_g[:, None, None] * np.maximum(diff, 0.0)[None]), 0.0)
    c["dmatT"] = np.ascontiguousarray(dmat.transpose(2, 0, 1)).astype(np.float32)
    gq = np.exp((n[None, :] + 1) * log_g[:, None])
    gqT = np.zeros((128, 2, 128))
    for p in range(2):
        gqT[:64, p, :] = gq[2 * p][None, :]
        gqT[64:, p, :] = gq[2 * p + 1][None, :]
    c["gqT"] = gqT.astype(np.float32)
    c["gkc"] = (np.exp((127 - n)[:, None] * log_g[None, :]) * 0.125).astype(np.float32)
    dc = np.zeros((128, 2))
    for p in range(2):
        dc[:64, p] = np.exp(128 * log_g[2 * p])
        dc[64:, p] = np.exp(128 * log_g[2 * p + 1])
    c["dc"] = dc.astype(np.float32)
    half = 32
    inv = 10000.0 ** (-np.arange(half, dtype=np.float64) / half)
    ang = np.arange(S, dtype=np.float64)[:, None] * inv[None, :]
    ang = ang.astype(np.float32).astype(np.float64)
    c["cos"] = np.ascontiguousarray(np.cos(ang).reshape(16, 128, 32).transpose(1, 0, 2)).astype(np.float32)
    c["sin"] = np.ascontiguousarray(np.sin(ang).reshape(16, 128, 32).transpose(1, 0, 2)).astype(np.float32)
    s_idx = np.arange(128)
    c["maskneg"] = np.where(s_idx[:, None] <= s_idx[None, :], 0.0, -30000.0).astype(np.float32)
    eh = np.zeros((4, 4, 128), np.float32)
    for h in range(4):
        eh[h, h, :] = 1.0
    c["eh"] = eh
    c["ident4"] = np.eye(4, dtype=np.float32)
    c["triu"] = (s_idx[:, None] < s_idx[None, :]).astype(np.float32)
    c["ecap"] = np.broadcast_to((np.arange(NEXP) * CAP).astype(np.float32)[None, :], (128, NEXP)).copy()
    return c


CONST_SHAPES = {
    "identb": [128, 128], "dmatT": [128, 4, 128], "gqT": [128, 2, 128], "gkc": [128, 4], "dc": [128, 2],
    "cos": [128, 16, 32], "sin": [128, 16, 32], "maskneg": [128, 128], "eh": [4, 4, 128], "ident4": [4, 4],
    "triu": [128, 128], "ecap": [128, NEXP],
}


def build_program(NS=4, NCH=16, phases=("mix", "xa", "moe"), dbg=False):
    SL = NCH * 128
    NT = NS * NCH
    NTOK = NT * 128
    nc = bass.Bass("TRN2", target_bir_lowering=False)

    def din(name, shape, dt=F32):
        return nc.dram_tensor(name, list(shape), dt, kind="ExternalInput").ap()

    x_d = din("x", [NS, SL, D])
    mem_d = din("mem", [NS, 256, D])
    w_in_d = din("w_in", [D, 3592])
    w_out_d = din("w_out", [D, D])
    wq_d = din("xa_wq", [D, D])
    wkv_d = din("xa_wkv", [D, 2 * D])
    wo_d = din("xa_wo", [D, D])
    wg_d = din("moe_w_gate", [NEXP, D, 512])
    wu_d = din("moe_w_up", [NEXP, D, 512])
    wd_d = din("moe_w_down", [NEXP, 512, D])
    gains_d = din("gains", [6, 128, D])
    convw_d = din("convw", [128, 8, 5])
    gateb_d = din("gateb", [4, 2])
    wr_d = din("wr", [D, 36])
    rb_d = din("rb", [128, 36])
    cd = {k: din("c_" + k, v) for k, v in CONST_SHAPES.items()}
    out_d = nc.dram_tensor("out", [NS, SL, D], F32, kind="ExternalOutput").ap()
    x1_d = nc.dram_tensor("x1_scr", [NTOK, D], F32, kind="Internal").ap()
    x2_d = nc.dram_tensor("x2_scr", [NTOK, D], F32, kind="Internal").ap()
    xs_d = nc.dram_tensor("xs_scr", [NEXP * CAP, D], BF16, kind="Internal").ap()
    ys_d = nc.dram_tensor("ys_scr", [NEXP * CAP, D], BF16, kind="Internal").ap()

    st = ExitStack()
    P = Prog(nc)
    sb = SB(nc, 212800)
    pp = [nc.alloc_psum_tensor("pp%d" % i, [128, 1024], F32) for i in range(4)]
    ppb = [t[:, 0:512].bitcast(BF16).rearrange("p (a b) -> p a b", a=8, b=128) for t in pp]
    ppb2 = [t[:, 512:1024].bitcast(BF16).rearrange("p (a b) -> p a b", a=8, b=128) for t in pp]
    rot = {}
    ppset = [(0, 1, 2, 3)]

    def nextpp(full=True):
        sset = ppset[0]
        if len(sset) == 1 and not full:
            r = rot.get((sset, "h"), 0)
            rot[(sset, "h")] = r + 1
            i = sset[0]
            if r % 2 == 0:
                return pp[i][:, 0:512], "pp%da" % i
            return pp[i][:, 512:1024], "pp%db" % i
        r = rot.get(sset, 0)
        rot[sset] = r + 1
        i = sset[r % len(sset)]
        return pp[i], "pp%d" % i

    def nextpt():
        sset = ppset[0]
        if len(sset) == 1:
            r = rot.get((sset, "h"), 0)
            rot[(sset, "h")] = r + 1
            i = sset[0]
            if r % 2 == 0:
                return ppb[i], "pp%da" % i
            return ppb2[i], "pp%db" % i
        r = rot.get(sset, 0)
        rot[sset] = r + 1
        i = sset[r % len(sset)]
        return ppb[i], "pp%d" % i

    def load_const(name, dt=F32, parts=128, eng="sp"):
        shp = CONST_SHAPES[name]
        t = sb.a(shp[1:], dt, parts=shp[0])
        if dt == F32:
            P.dma(eng, t, cd[name], writes=[name])
        else:
            P.dma("pool", t, cd[name], writes=[name])
        return t

    identb = load_const("identb", BF16)
    gains = {}

    def load_gain(i, name):
        t = sb.a([D], F32)
        P.dma("sp", t, gains_d[i], writes=[name])
        gains[name] = t
        return t

    dbg_out = {}
    mark0 = sb.off

    def phase_mix():
        w_in = sb.a([8, 3592], BF16)
        w_out = sb.a([8, D], BF16)
        wv = w_in_d.rearrange("(kc p) n -> p kc n", p=128)
        for kc in range(8):
            P.dma("pool", w_in[:, kc, 0:2048], wv[:, kc, 0:2048], writes=["w_in"])
            P.dma("pool", w_in[:, kc, 2048:3592], wv[:, kc, 2048:3592], writes=["w_in"])
        wv = w_out_d.rearrange("(kc p) n -> p kc n", p=128)
        for kc in range(8):
            P.dma("pool", w_out[:, kc, :], wv[:, kc, :], writes=["w_out"])
        gmix = load_gain(0, "gmix")
        ghn = load_gain(5, "ghn")
        dmatT = load_const("dmatT")
        gqT = load_const("gqT")
        gkc = load_const("gkc")
        dcc = load_const("dc")
        cosT = load_const("cos")
        sinT = load_const("sin")
        maskneg = load_const("maskneg", BF16)
        eh = load_const("eh")
        ident4 = load_const("ident4")
        convw = sb.a([8, 5], F32)
        P.dma("sp", convw, convw_d, writes=["convw"])
        gateb = sb.a([2], F32, parts=4)
        P.dma("sp", gateb, gateb_d, writes=["gateb"])
        ones4 = sb.a([128], F32, parts=4)
        P.op("pool", lambda e: e.memset(ones4, 1.0), writes=["ones4"])
        epsc = sb.a([1], F32)
        P.op("pool", lambda e: e.memset(epsc, EPS), writes=["epsc"])

        SLOTKA = {"xa", "ss", "rstd", "hb", "hT", "qk_f", "rt0", "rt1", "qkr", "qT", "kT", "qdT", "kdec", "rv", "sc_bf", "ro",
                  "cenR", "sqR", "st4R", "nm4R", "rs4R", "cenM", "sqM", "st4M", "nm4M", "rs4M", "sg", "mix", "cb", "acc", "tmpc",
                  "mqk_tok", "qkT", "mv", "so", "g_ig", "g_fp", "g_t1", "g_t2", "g_b", "g_u", "g_cu", "g_M", "g_nM", "g_in",
                  "g_fp", "g_ig", "g_t1", "s4", "dg", "cols", "sbcs", "DT", "Pm", "qTs", "kw", "hm", "dn4", "tmpC"}
        ST = [(sb.a([2, 128], F32), sb.a([2, 128], BF16), sb.a([4, 129], F32), sb.a([4, 129], BF16), sb.a([1], F32, parts=4))
              for _ in range(2)]
        CBS = [None, None]

        def make_slot(slot):
            xt = sb.a([D], F32)
            ss = sb.a([1], F32)
            rstd = sb.a([1], F32)
            hb = sb.a([D], BF16)
            hT = sb.a([8, 128], BF16)
            qk_f = sb.a([8, 2, 32], BF16)
            rt = [sb.a([8, 32], BF16) for _ in range(2)]
            qkr = sb.a([8, 2, 32], BF16)
            qT = sb.a([4, 128], BF16)
            kT = sb.a([2, 128], BF16)
            qdT = sb.a([4, 128], BF16)
            kdec = sb.a([4, 64], BF16)
            rv = sb.a([512], BF16)
            sc_bf = sb.a([4, 128], BF16)
            ro = sb.a([4, 128], F32)
            cen = sb.a([4, 128], F32)
            sq = sb.a([4, 128], BF16)
            st4 = sb.a([4], F32)
            nm4 = sb.a([4], F32)
            rs4 = sb.a([4], F32)
            sg = sb.a([512], BF16)
            mix = sb.a([D], BF16)
            cb = sb.a([8, 131], BF16)
            CBS[slot] = cb
            acc = sb.a([8, 128], F32)
            tmpc = sb.a([8, 128], BF16)
            mqk_tok = sb.a([D], BF16)
            junk = mqk_tok
            qkT = sb.a([8, 128], BF16)
            mv = sb.a([4, 129], BF16)
            so = sb.a([512], BF16)
            g_ig = sb.a([128], F32, parts=4)
            g_fp = sb.a([128], F32, parts=4)
            g_t1 = sb.a([128], F32, parts=4)
            g_t2 = sb.a([128], F32, parts=4)
            g_b = sb.a([128], F32, parts=4)
            g_u = sb.a([128], F32, parts=4)
            g_cu = sb.a([128], F32, parts=4)
            g_M = sb.a([128], F32, parts=4)
            g_nM = sb.a([128], F32, parts=4)
            g_in = sb.a([128], F32, parts=4)
            g_em = g_fp
            g_wa = g_ig
            g_mb = g_t1
            s4 = sb.a([4], F32, parts=4)
            dg = sb.a([2, 4], F32, parts=4)
            cols = sb.a([3, 4], F32)
            sbcs = sb.a([2, 4], F32)
            DT = sb.a([4, 128], BF16)
            Pm = sb.a([4, 128], BF16)
            qTs = sb.a([4, 128], BF16)
            kw = sb.a([4, 128], BF16)
            hm = sb.a([4, 128], F32)
            dn4 = sb.a([4], F32)
            tmpC = sb.a([4, 129], BF16)
            y1 = xt

            P.keymap = (SLOTKA, "_%d" % slot)
            P.op("pool", lambda e: e.memset(mv, 1.0), writes=["mv"])
            P.op("pool", lambda e: e.memset(qT, 0.0), writes=["qT"])
            P.op("pool", lambda e: e.memset(qdT, 0.0), writes=["qdT"])
            P.keymap = None
            DKS = 128.0 ** -0.5

            def rmsnorm_to_hb(src, srck, gain, gk):
                P.op("act", lambda e: e.activation(out=junk, in_=src, func=AF.Square, accum_out=ss),
                     reads=[srck], writes=["mqk_tok", "ss"])
                P.op("act", lambda e: e.activation(out=rstd, in_=ss, func=AF.Sqrt, bias=epsc, scale=1.0 / D),
                     reads=["ss", "epsc"], writes=["rstd"])
                P.op("dve", lambda e: e.reciprocal(out=rstd, in_=rstd), reads=["rstd"], writes=["rstd"])
                P.op("dve", lambda e: e.scalar_tensor_tensor(out=hb, in0=src, scalar=rstd, in1=gain,
                                                               op0=ALU.mult, op1=ALU.mult),
                     reads=[srck, "rstd", gk], writes=["hb"])

            def transpose8(src, srck, dst, dstk):
                ptile, ptk = nextpt()
                for j in range(8):
                    P.op("pe", lambda e, j=j: e.transpose(out=ptile[:, j, :], in_=src[:, j * 128:(j + 1) * 128],
                                                          identity=identb),
                         reads=[srck, "identb"], writes=[ptk], chain=True)
                P.op("act", lambda e: e.copy(out=dst, in_=ptile), reads=[ptk], writes=[dstk])

            hn_tmp = {"R": (st4, nm4, rs4, cen, sq),
                      "M": (sb.a([4], F32), sb.a([4], F32), sb.a([4], F32), sb.a([4, 128], F32), sb.a([4, 128], BF16))}

            def head_norm(src, srck, gain_sl, gate, gatek, dst_sl, br="R"):
                st4, nm4, rs4, cen, sq = hn_tmp[br]
                return head_norm_(src, srck, gain_sl, gate, gatek, dst_sl, st4, nm4, rs4, cen, sq, br)

            def head_norm_(src, srck, gain_sl, gate, gatek, dst_sl, st4, nm4, rs4, cen, sq, br):
                head_norm__(src, srck, gain_sl, gate, gatek, dst_sl, st4, nm4, rs4, cen, sq, br)

            def head_norm__(src, srck, gain_sl, gate, gatek, dst_sl, st4, nm4, rs4, cen, sq, br):
                P.op("dve", lambda e: e.tensor_reduce(out=st4, in_=src, axis=AX.X, op=ALU.add),
                     reads=[srck], writes=["st4" + br])
                P.op("dve", lambda e: e.tensor_scalar(out=nm4, in0=st4, scalar1=-1.0 / 128, scalar2=None, op0=ALU.mult),
                     reads=["st4" + br], writes=["nm4" + br])
                P.op("dve", lambda e: e.tensor_tensor(out=cen, in0=src, in1=bc(nm4.unsqueeze(2), [128, 4, 128]),
                                                      op=ALU.add), reads=[srck, "nm4" + br], writes=["cen" + br])
                P.op("pool", lambda e: e.tensor_tensor(out=sq, in0=cen, in1=cen, op=ALU.mult),
                     reads=["cen" + br], writes=["sq" + br])
                P.op("dve", lambda e: e.tensor_reduce(out=st4, in_=sq, axis=AX.X, op=ALU.add),
                     reads=["sq" + br], writes=["st4" + br])
                P.op("act", lambda e: e.activation(out=rs4, in_=st4, func=AF.Sqrt, bias=epsc, scale=1.0 / 128),
                     reads=["st4" + br, "epsc"], writes=["rs4" + br])
                P.op("dve", lambda e: e.reciprocal(out=rs4, in_=rs4), reads=["rs4" + br], writes=["rs4" + br])
                P.op("dve", lambda e: e.tensor_tensor(out=cen, in0=cen, in1=bc(rs4.unsqueeze(2), [128, 4, 128]),
                                                      op=ALU.mult), reads=["cen" + br, "rs4" + br], writes=["cen" + br])
                P.op("pool", lambda e: e.tensor_tensor(out=cen, in0=cen, in1=gain_sl, op=ALU.mult),
                     reads=["cen" + br, "ghn"], writes=["cen" + br])
                P.op("dve", lambda e: e.tensor_tensor(out=dst_sl, in0=cen, in1=gate, op=ALU.mult),
                     reads=["cen" + br, gatek], writes=["mix"])

            def mix_tile(b, c):
                if True:
                    it = b * NCH + c
                    Rf, Rbf, Cf, Cbf, mprev = ST[b % 2]
                    sk = lambda n: n + str(b % 2)
                    xk = "xa"
                    PA, PB = (2 * slot,), (2 * slot + 1,)
                    if c == 0:
                        P.op("pool", lambda e: e.memset(Rf, 0.0), writes=[sk("Rf")])
                        P.op("pool", lambda e: e.memset(Rbf, 0.0), writes=[sk("Rbf")])
                        P.op("pool", lambda e: e.memset(Cf, 0.0), writes=[sk("Cf")])
                        P.op("pool", lambda e: e.memset(Cbf, 0.0), writes=[sk("Cbf")])
                        P.op("pool", lambda e: e.memset(mprev, 0.0), writes=[sk("mprev")])
                        P.op("pool", lambda e: e.memset(cb[:, :, 0:3], 0.0), writes=["cbh%d" % slot])
                    P.dma("sp", xt, x_d[b, c * 128:(c + 1) * 128, :], writes=[xk])
                    ppset[0] = PA
                    rmsnorm_to_hb(xt, xk, gmix, "gmix")
                    transpose8(hb, "hb", hT, "hT")

                    def proj_tok(lo):
                        pst, psk = nextpp(full=False)
                        for kc in range(8):
                            P.op("pe", lambda e, kc=kc: e.matmul(pst[:, 0:512], lhsT=hT[:, kc, :],
                                                                 rhs=w_in[:, kc, lo:lo + 512],
                                                                 start=(kc == 0), stop=(kc == 7)),
                                 reads=["hT", "w_in"], writes=[psk], chain=True)
                        return pst, psk

                    G.pre = P.stop()
                    P.record()
                    ppset[0] = PA
                    pqk, pqkk = proj_tok(0)
                    P.op("act", lambda e: e.copy(out=qk_f.rearrange("p a b c -> p (a b c)"), in_=pqk[:, 0:512]),
                         reads=[pqkk], writes=["qk_f"])
                    cs = bc(cosT[:, c, :].unsqueeze(1), [128, 8, 32])
                    sn = bc(sinT[:, c, :].unsqueeze(1), [128, 8, 32])
                    x1v, x2v = qk_f[:, :, 0, :], qk_f[:, :, 1, :]
                    P.op("dve", lambda e: e.tensor_tensor(out=rt[0], in0=x1v, in1=cs, op=ALU.mult),
                         reads=["qk_f", "cos"], writes=["rt0"])
                    P.op("pool", lambda e: e.tensor_tensor(out=rt[1], in0=x2v, in1=sn, op=ALU.mult),
                         reads=["qk_f", "sin"], writes=["rt1"])
                    P.op("dve", lambda e: e.tensor_tensor(out=qkr[:, :, 0, :], in0=rt[0], in1=rt[1], op=ALU.subtract),
                         reads=["rt0", "rt1"], writes=["qkr"])
                    P.op("pool", lambda e: e.tensor_tensor(out=rt[0], in0=x1v, in1=sn, op=ALU.mult),
                         reads=["qk_f", "sin"], writes=["rt0"])
                    P.op("dve", lambda e: e.tensor_tensor(out=rt[1], in0=x2v, in1=cs, op=ALU.mult),
                         reads=["qk_f", "cos"], writes=["rt1"])
                    P.op("dve", lambda e: e.tensor_tensor(out=qkr[:, :, 1, :], in0=rt[0], in1=rt[1], op=ALU.add),
                         reads=["rt0", "rt1"], writes=["qkr"])
                    qkr2 = qkr.rearrange("p a b c -> p (a b c)")
                    ptq, ptqk = nextpt()
                    for j in range(4):
                        P.op("pe", lambda e, j=j: e.transpose(out=ptq[:, j, :], in_=qkr2[:, j * 128:(j + 1) * 128],
                                                              identity=identb),
                             reads=["qkr", "identb"], writes=[ptqk], chain=True)
                    P.op("act", lambda e: e.copy(out=qT[0:64, 0:4:2, :], in_=ptq[0:64, 0:2, :]), reads=[ptqk], writes=["qT"])
                    P.op("act", lambda e: e.copy(out=qT[64:128, 1:4:2, :], in_=ptq[64:128, 0:2, :]), reads=[ptqk], writes=["qT"])
                    import os as _os
                    _v = int(_os.environ.get("DBGV", "0"))
                    if _v != 1:
                        P.op("act", lambda e: e.mul(out=kT, in_=ptq[:, 2:4, :], mul=0.125), reads=[ptqk], writes=["kT"])
                    P.op("dve", lambda e: e.tensor_tensor(out=qdT[0:64, 0:4:2, :], in0=qT[0:64, 0:4:2, :], in1=gqT[0:64, :, :],
                                                          op=ALU.mult), reads=["qT", "gqT"], writes=["qdT"])
                    P.op("dve", lambda e: e.tensor_tensor(out=qdT[64:128, 1:4:2, :], in0=qT[64:128, 1:4:2, :],
                                                          in1=gqT[64:128, :, :], op=ALU.mult), reads=["qT", "gqT"], writes=["qdT"])
                    P.op("dve", lambda e: e.tensor_tensor(out=kdec, in0=qkr2[:, 256:512].rearrange("p (h d) -> p h d", h=4),
                                                           in1=bc(gkc.unsqueeze(2), [128, 4, 64]), op=ALU.mult),
                         reads=["qkr", "gkc"], writes=["kdec"])
                    prv, prvk = proj_tok(512)
                    P.op("act", lambda e: e.copy(out=rv, in_=prv[:, 0:512]), reads=[prvk], writes=["rv"])
                    prg, prgk = proj_tok(1024)
                    P.op("act", lambda e: e.activation(out=sg, in_=prg[:, 0:512], func=AF.Silu),
                         reads=[prgk], writes=["sg"])
                    psc, psck = nextpp(full=False)
                    for h in range(4):
                        p_, off = h // 2, (h % 2) * 64
                        P.op("pe", lambda e, h=h, p_=p_, off=off: e.matmul(
                            psc[:, h * 128:(h + 1) * 128], lhsT=kT[:, p_, :], rhs=qT[:, h, :],
                            start=True, stop=True), reads=["kT", "qT"], writes=[psck], chain=True)
                    P.op("dve", lambda e: e.tensor_tensor(out=sc_bf.rearrange("p a b -> p (a b)"), in0=psc[:, 0:512],
                                                          in1=dmatT.rearrange("p a b -> p (a b)"), op=ALU.mult),
                         reads=[psck, "dmatT"], writes=["sc_bf"])
                    pro, prok = nextpp(full=False)
                    for h in range(4):
                        p_, off = h // 2, (h % 2) * 64
                        P.op("pe", lambda e, h=h: e.matmul(pro[:, h * 128:(h + 1) * 128], lhsT=sc_bf[:, h, :],
                                                           rhs=rv[:, h * 128:(h + 1) * 128], start=True, stop=False),
                             reads=["sc_bf", "rv"], writes=[prok], chain=True)
                        P.op("pe", lambda e, h=h, p_=p_, off=off: e.matmul(
                            pro[:, h * 128:(h + 1) * 128], lhsT=qdT[:, h, :], rhs=Rbf[:, p_, :],
                            start=False, stop=True), reads=["qdT", sk("Rbf")], writes=[prok], chain=True)
                    P.op("act", lambda e: e.copy(out=ro.rearrange("p a b -> p (a b)"), in_=pro[:, 0:512]),
                         reads=[prok], writes=["ro"])
                    pkv, pkvk = nextpp(full=False)
                    for h in range(4):
                        p_ = h // 2
                        P.op("pe", lambda e, h=h, p_=p_: e.matmul(
                            pkv[:, h * 128:(h + 1) * 128], lhsT=kdec[:, 2 * p_:2 * p_ + 2, :].rearrange("p a b -> p (a b)"),
                            rhs=rv[:, h * 128:(h + 1) * 128], start=True, stop=True),
                            reads=["kdec", "rv"], writes=[pkvk], chain=True)
                    for h in range(4):
                        p_, off = h // 2, (h % 2) * 64
                        P.op("dve", lambda e, h=h, p_=p_, off=off: e.scalar_tensor_tensor(
                            out=Rf[off:off + 64, p_, :], in0=Rf[off:off + 64, p_, :], scalar=dcc[off:off + 64, p_:p_ + 1],
                            in1=pkv[off:off + 64, h * 128:(h + 1) * 128], op0=ALU.mult, op1=ALU.add),
                            reads=[sk("Rf"), "dc", pkvk], writes=[sk("Rf")])
                    P.op("pool", lambda e: e.tensor_copy(out=Rbf, in_=Rf), reads=[sk("Rf")], writes=[sk("Rbf")])
                    head_norm(ro, "ro", ghn[:, 0:512].rearrange("p (a b) -> p a b", a=4),
                              sg.rearrange("p (a b) -> p a b", a=4), "sg",
                              mix[:, 0:512].rearrange("p (a b) -> p a b", a=4))

                    listR = P.stop()
                    P.record()
                    ppset[0] = PB
                    pg, pgk = nextpp(full=False)
                    for gi in range(2):
                        for kc in range(8):
                            P.op("pe", lambda e, gi=gi, kc=kc: e.matmul(
                                pg[0:4, gi * 128:(gi + 1) * 128], lhsT=w_in[:, kc, 3584 + 4 * gi:3588 + 4 * gi],
                                rhs=hT[:, kc, :], start=(kc == 0), stop=(kc == 7)),
                                reads=["hT", "w_in"], writes=[pgk], chain=True)
                    P.op("act", lambda e: e.activation(out=g_ig, in_=pg[0:4, 0:128], func=AF.Identity, bias=gateb[:, 0:1]),
                         reads=[pgk, "gateb"], writes=["g_ig"])
                    P.op("act", lambda e: e.activation(out=g_fp, in_=pg[0:4, 128:256], func=AF.Identity, bias=gateb[:, 1:2]),
                         reads=[pgk, "gateb"], writes=["g_fp"])
                    P.op("act", lambda e: e.activation(out=g_t1, in_=g_fp, func=AF.Abs),
                         reads=["g_fp"], writes=["g_t1"])
                    P.op("act", lambda e: e.activation(out=g_t1, in_=g_t1, func=AF.Exp, scale=-1.0),
                         reads=["g_t1"], writes=["g_t1"])
                    P.op("act", lambda e: e.activation(out=g_t1, in_=g_t1, func=AF.Ln, bias=1.0),
                         reads=["g_t1"], writes=["g_t1"])
                    P.op("dve", lambda e: e.tensor_scalar(out=g_t2, in0=g_fp, scalar1=0.0, scalar2=None, op0=ALU.min),
                         reads=["g_fp"], writes=["g_t2"])
                    P.op("dve", lambda e: e.tensor_tensor(out=g_t2, in0=g_t2, in1=g_t1, op=ALU.subtract),
                         reads=["g_t2", "g_t1"], writes=["g_t2"])
                    P.op("dve", lambda e: e.tensor_tensor_scan(out=g_b, data0=ones4, data1=g_t2, initial=0.0,
                                                               op0=ALU.mult, op1=ALU.add),
                         reads=["ones4", "g_t2"], writes=["g_b"])
                    P.op("dve", lambda e: e.tensor_tensor(out=g_u, in0=g_ig, in1=g_b, op=ALU.subtract),
                         reads=["g_ig", "g_b"], writes=["g_u"])
                    P.op("dve", lambda e: e.tensor_tensor_scan(out=g_cu, data0=ones4, data1=g_u, initial=-1e30,
                                                               op0=ALU.mult, op1=ALU.max),
                         reads=["ones4", "g_u"], writes=["g_cu"])
                    P.op("dve", lambda e: e.tensor_scalar(out=g_M, in0=g_cu, scalar1=mprev, scalar2=None, op0=ALU.max),
                         reads=["g_cu", sk("mprev")], writes=["g_M"])
                    P.op("dve", lambda e: e.tensor_scalar(out=g_nM, in0=g_M, scalar1=-1.0, scalar2=None, op0=ALU.mult),
                         reads=["g_M"], writes=["g_nM"])
                    P.op("act", lambda e: e.activation(out=g_in, in_=g_M, func=AF.Exp, scale=-1.0, bias=mprev),
                         reads=["g_M", sk("mprev")], writes=["g_in"])
                    P.op("dve", lambda e: e.tensor_tensor(out=g_mb, in0=g_M, in1=g_b, op=ALU.add),
                         reads=["g_M", "g_b"], writes=["g_t1"])
                    P.op("act", lambda e: e.activation(out=g_em, in_=g_mb, func=AF.Exp, scale=-1.0),
                         reads=["g_t1"], writes=["g_fp"])
                    P.op("dve", lambda e: e.tensor_scalar(out=s4[:, 1:2], in0=g_cu[:, 127:128],
                                                          scalar1=-1.0, scalar2=None, op0=ALU.mult),
                         reads=["g_cu"], writes=["s4"])
                    P.op("dve", lambda e: e.tensor_scalar(out=s4[:, 2:3], in0=g_M[:, 127:128], scalar1=-1.0, scalar2=None,
                                                          op0=ALU.mult), reads=["g_M"], writes=["s4"])
                    P.op("act", lambda e: e.activation(out=g_wa, in_=g_u, func=AF.Exp, bias=s4[:, 1:2]),
                         reads=["g_u", "s4"], writes=["g_ig"])
                    P.op("act", lambda e: e.activation(out=s4[:, 3:4], in_=g_cu[:, 127:128], func=AF.Exp, bias=s4[:, 2:3]),
                         reads=["g_cu", "s4"], writes=["s4"])
                    P.op("dve", lambda e: e.tensor_scalar(out=dg[:, 0, :], in0=ident4, scalar1=g_in[:, 127:128], scalar2=None,
                                                          op0=ALU.mult), reads=["ident4", "g_in"], writes=["dg"])
                    P.op("dve", lambda e: e.tensor_scalar(out=dg[:, 1, :], in0=ident4, scalar1=s4[:, 3:4], scalar2=None,
                                                          op0=ALU.mult), reads=["ident4", "s4"], writes=["dg"])
                    P.op("dve", lambda e: e.tensor_copy(out=mprev, in_=g_mb[:, 127:128]),
                         reads=["g_t1", "g_in", "g_M"], writes=[sk("mprev")])
                    pc, pck = nextpp(full=False)
                    P.op("pe", lambda e: e.matmul(pc[:, 0:4], lhsT=g_wa, rhs=ident4, start=True, stop=True),
                         reads=["g_ig", "ident4"], writes=[pck], f32=True)
                    P.op("pe", lambda e: e.matmul(pc[:, 8:12], lhsT=g_em, rhs=ident4, start=True, stop=True),
                         reads=["g_fp", "ident4"], writes=[pck], f32=True)
                    P.op("pe", lambda e: e.matmul(pc[:, 16:20], lhsT=ones4, rhs=dg[:, 0, :], start=True, stop=True),
                         reads=["ones4", "dg"], writes=[pck], f32=True)
                    P.op("pe", lambda e: e.matmul(pc[:, 20:24], lhsT=ones4, rhs=dg[:, 1, :], start=True, stop=True),
                         reads=["ones4", "dg"], writes=[pck], f32=True)
                    P.op("dve", lambda e: e.tensor_scalar(out=cols[:, 0, :], in0=pc[:, 0:4], scalar1=DKS, scalar2=None,
                                                          op0=ALU.mult), reads=[pck], writes=["cols"])
                    P.op("dve", lambda e: e.tensor_copy(out=cols[:, 2, :], in_=pc[:, 8:12]), reads=[pck], writes=["cols"])
                    P.op("dve", lambda e: e.tensor_copy(out=sbcs.rearrange("p a b -> p (a b)"), in_=pc[:, 16:24]),
                         reads=[pck], writes=["sbcs"])

                    pm, pmk = nextpp()
                    for g in range(2):
                        for kc in range(8):
                            P.op("pe", lambda e, g=g, kc=kc: e.matmul(
                                pm[:, g * 512:(g + 1) * 512], lhsT=hT[:, kc, :],
                                rhs=w_in[:, kc, 1536 + g * 512:1536 + (g + 1) * 512], start=(kc == 0), stop=(kc == 7)),
                                reads=["hT", "w_in"], writes=[pmk], chain=True)
                    P.op("act", lambda e: e.copy(out=mqk_tok, in_=pm[:, :]), reads=[pmk], writes=["mqk_tok"])
                    ptm, ptmk = nextpt()
                    for j in range(8):
                        P.op("pe", lambda e, j=j: e.transpose(out=ptm[:, j, :], in_=mqk_tok[:, j * 128:(j + 1) * 128],
                                                              identity=identb),
                             reads=["mqk_tok", "identb"], writes=[ptmk], chain=True)
                    P.op("act", lambda e: e.copy(out=cb[:, :, 3:131], in_=ptm), reads=[ptmk], writes=["cb"])
                    if c + 1 < NCH:
                        P.op("pool", lambda e: e.tensor_copy(out=CBS[1 - slot][:, :, 0:3], in_=cb[:, :, 128:131]),
                             reads=["cb"], writes=["cbh%d" % (1 - slot)])
                    P.op("dve", lambda e: e.tensor_tensor(out=acc, in0=cb[:, :, 3:131],
                                                          in1=bc(convw[:, :, 3:4], [128, 8, 128]), op=ALU.mult),
                         reads=["cb", "cbh%d" % slot, "convw"], writes=["acc"])
                    for tap in range(3):
                        P.op("pool", lambda e, tap=tap: e.tensor_tensor(out=tmpc, in0=cb[:, :, tap:tap + 128],
                                                                        in1=bc(convw[:, :, tap:tap + 1], [128, 8, 128]),
                                                                        op=ALU.mult),
                             reads=["cb", "cbh%d" % slot, "convw"], writes=["tmpc"])
                        P.op("dve", lambda e: e.tensor_tensor(out=acc, in0=acc, in1=tmpc, op=ALU.add),
                             reads=["acc", "tmpc"], writes=["acc"])
                    for j in range(8):
                        P.op("act", lambda e, j=j: e.activation(out=qkT[:, j, :], in_=acc[:, j, :], func=AF.Silu,
                                                                bias=convw[:, j, 4:5]),
                             reads=["acc", "convw"], writes=["qkT"])
                    pmv, pmvk = proj_tok(2560)
                    P.op("act", lambda e: e.copy(out=mv[:, :, 0:128], in_=pmv[:, 0:512].rearrange("p (a b) -> p a b", a=4)),
                         reads=[pmvk], writes=["mv"])
                    pmo, pmok = proj_tok(3072)
                    P.op("act", lambda e: e.activation(out=so, in_=pmo[:, 0:512], func=AF.Sigmoid),
                         reads=[pmok], writes=["so"])
                    ptk_, ptkk = nextpt()
                    for h in range(4):
                        P.op("pe", lambda e, h=h: e.transpose(out=ptk_[:, h, :], in_=qkT[:, 4 + h, :], identity=identb),
                             reads=["qkT", "identb"], writes=[ptkk], chain=True)
                    P.op("act", lambda e: e.copy(out=kw, in_=ptk_[:, 0:4, :]), reads=[ptkk], writes=["kw"])
                    P.op("dve", lambda e: e.tensor_tensor(out=kw, in0=kw,
                                                          in1=bc(cols[:, 0, :].unsqueeze(2), [128, 4, 128]), op=ALU.mult),
                         reads=["kw", "cols"], writes=["kw"])
                    ps_, psk_ = nextpp()
                    for h in range(4):
                        P.op("pe", lambda e, h=h: e.matmul(ps_[:, h * 128:(h + 1) * 128], lhsT=qkT[:, 4 + h, :],
                                                           rhs=qkT[:, h, :], start=True, stop=True),
                             reads=["qkT"], writes=[psk_], chain=True)
                    for h in range(4):
                        P.op("pe", lambda e, h=h: e.matmul(ps_[:, 512 + h * 128:512 + (h + 1) * 128], lhsT=g_u,
                                                           rhs=eh[:, h, :], start=True, stop=False),
                             reads=["g_u", "eh"], writes=[psk_], f32=True)
                        P.op("pe", lambda e, h=h: e.matmul(ps_[:, 512 + h * 128:512 + (h + 1) * 128], lhsT=eh[:, h, :],
                                                           rhs=g_nM, start=False, stop=False),
                             reads=["g_nM", "eh"], writes=[psk_], f32=True)
                        P.op("pe", lambda e, h=h: e.matmul(ps_[:, 512 + h * 128:512 + (h + 1) * 128], lhsT=identb,
                                                           rhs=maskneg, start=False, stop=True),
                             reads=["identb", "maskneg"], writes=[psk_], chain=True)
                    P.op("act", lambda e: e.activation(out=DT.rearrange("p a b -> p (a b)"), in_=ps_[:, 512:1024], func=AF.Exp),
                         reads=[psk_], writes=["DT"])
                    P.op("dve", lambda e: e.scalar_tensor_tensor(out=Pm.rearrange("p a b -> p (a b)"), in0=ps_[:, 0:512],
                                                                 scalar=DKS, in1=DT.rearrange("p a b -> p (a b)"),
                                                                 op0=ALU.mult, op1=ALU.mult),
                         reads=[psk_, "DT"], writes=["Pm"])
                    pib, pibk = nextpp(full=False)
                    for h in range(4):
                        P.op("pe", lambda e, h=h: e.matmul(pib[:, h * 128:(h + 1) * 128], lhsT=eh[:, h, :], rhs=g_in,
                                                           start=True, stop=True), reads=["eh", "g_in"], writes=[pibk], f32=True)
                    P.op("dve", lambda e: e.tensor_tensor(out=qTs.rearrange("p a b -> p (a b)"),
                                                          in0=qkT[:, 0:4, :].rearrange("p a b -> p (a b)"),
                                                          in1=pib[:, 0:512], op=ALU.mult),
                         reads=["qkT", pibk], writes=["qTs"])
                    pn_, pnk = nextpp()
                    for h in range(4):
                        P.op("pe", lambda e, h=h: e.matmul(pn_[:, h * 256:h * 256 + 129], lhsT=Pm[:, h, :], rhs=mv[:, h, :],
                                                           start=True, stop=False), reads=["Pm", "mv"], writes=[pnk], chain=True)
                        P.op("pe", lambda e, h=h: e.matmul(pn_[:, h * 256:h * 256 + 129], lhsT=qTs[:, h, :], rhs=Cbf[:, h, :],
                                                           start=False, stop=True), reads=["qTs", sk("Cbf")], writes=[pnk], chain=True)
                    pn3 = pn_[:, :].rearrange("p (a b) -> p a b", a=4)
                    P.op("act", lambda e: e.activation(out=dn4, in_=pn3[:, :, 128], func=AF.Abs),
                         reads=[pnk], writes=["dn4"])
                    P.op("dve", lambda e: e.tensor_tensor(out=dn4, in0=dn4, in1=cols[:, 2, :], op=ALU.max),
                         reads=["dn4", "cols"], writes=["dn4"])
                    P.op("dve", lambda e: e.reciprocal(out=dn4, in_=dn4), reads=["dn4"], writes=["dn4"])
                    P.op("dve", lambda e: e.tensor_tensor(out=hm, in0=pn3[:, :, 0:128],
                                                          in1=bc(dn4.unsqueeze(2), [128, 4, 128]), op=ALU.mult),
                         reads=[pnk, "dn4"], writes=["hm"])
                    pkc, pkck = nextpp()
                    for h in range(4):
                        P.op("pe", lambda e, h=h: e.matmul(pkc[:, h * 256:h * 256 + 129], lhsT=kw[:, h, :], rhs=mv[:, h, :],
                                                           start=True, stop=True), reads=["kw", "mv"], writes=[pkck], chain=True)
                    pk3 = pkc[:, :].rearrange("p (a b) -> p a b", a=4)
                    P.op("pool", lambda e: e.tensor_tensor(out=Cf, in0=Cf, in1=bc(sbcs[:, 0, :].unsqueeze(2), [128, 4, 129]),
                                                           op=ALU.mult), reads=[sk("Cf"), "sbcs", sk("Cbf")], writes=[sk("Cf")])
                    P.op("dve", lambda e: e.tensor_tensor(out=tmpC, in0=pk3[:, :, 0:129],
                                                          in1=bc(sbcs[:, 1, :].unsqueeze(2), [128, 4, 129]), op=ALU.mult),
                         reads=[pkck, "sbcs"], writes=["tmpC"])
                    P.op("pool", lambda e: e.tensor_tensor(out=Cf, in0=Cf, in1=tmpC, op=ALU.add),
                         reads=[sk("Cf"), "tmpC"], writes=[sk("Cf")])
                    P.op("pool", lambda e: e.tensor_copy(out=Cbf, in_=Cf), reads=[sk("Cf")], writes=[sk("Cbf")])
                    head_norm(hm, "hm", ghn[:, 512:1024].rearrange("p (a b) -> p a b", a=4),
                              so.rearrange("p (a b) -> p a b", a=4), "so",
                              mix[:, 512:1024].rearrange("p (a b) -> p a b", a=4), br="M")
                    listM = P.stop()
                    ppset[0] = PA
                    G.RM = (listR, listM)
                    P.record()

                    transpose8(mix, "mix", hT, "hT")
                    py, pyk = nextpp()
                    for g in range(2):
                        for kc in range(8):
                            P.op("pe", lambda e, g=g, kc=kc: e.matmul(py[:, g * 512:(g + 1) * 512], lhsT=hT[:, kc, :],
                                                                      rhs=w_out[:, kc, g * 512:(g + 1) * 512],
                                                                      start=(kc == 0), stop=(kc == 7)),
                                 reads=["hT", "w_out"], writes=[pyk], chain=True)
                    for g in range(2):
                        P.op("dve", lambda e, g=g: e.tensor_tensor(out=y1[:, g * 512:(g + 1) * 512],
                                                                   in0=py[:, g * 512:(g + 1) * 512],
                                                                   in1=xt[:, g * 512:(g + 1) * 512], op=ALU.add),
                             reads=[pyk, xk], writes=[xk])
                    P.dma("sp", x1_d[it * 128:(it + 1) * 128, :], y1, reads=[xk], writes=["x1s%d" % it], chan="c:x1s%d" % slot)
            return mix_tile

        tile_fns = [make_slot(0), make_slot(1)]
        tilesA = []
        for b in range(NS):
            for c in range(NCH):
                it = b * NCH + c
                P.record()
                P.keymap = (SLOTKA, "_%d" % (it % 2))
                tile_fns[it % 2](b, c)
                P.keymap = None
                suf = P.stop()
                tilesA.append((G.pre, G.RM[0], G.RM[1], suf))
        import os as _os2
        _sa = _os2.environ.get('SCHEDA', '1')
        if _sa == '2':
            P.schedule([l for tl in tilesA for l in tl], win=10)
        elif _sa == '1':
            P.schedule([tl[0] + P.merge_list([tl[1], tl[2]]) + tl[3] for tl in tilesA], win=3)
        else:
            P.pipeline([tl[0] + P.merge_list([tl[1], tl[2]]) + tl[3] for tl in tilesA],
                       frac=float(_os2.environ.get('FRACA', '0.5')))
        ppset[0] = (0, 1, 2, 3)
        P.barrier()
        sb.off = mark0


    NB_ = NEXP * CAP

    class G:
        reg = None

    def breg(e):
        if G.reg is None:
            G.reg = nc.gpsimd.to_reg(NB_ - 1)
        return G.reg

    if "mix" in phases:
        phase_mix()

    def phase_xa():
        wq = sb.a([8, D], BF16)
        wo = sb.a([8, D], BF16)
        wkv = sb.a([8, 2 * D], BF16)
        for (wt, wd_, nm, ncol) in ((wq, wq_d, "wq", D), (wo, wo_d, "wo", D), (wkv, wkv_d, "wkv", 2 * D)):
            wv = wd_.rearrange("(kc p) n -> p kc n", p=128)
            for kc in range(8):
                for c0 in range(0, ncol, 1024):
                    P.dma("pool", wt[:, kc, c0:c0 + 1024], wv[:, kc, c0:c0 + 1024], writes=[nm])
        gxa = load_gain(1, "gxa")
        gmem = load_gain(2, "gmem")
        gmoe = load_gain(3, "gmoe")
        identf = sb.a([128], F32)
        P.dma("sp", identf, cd["identb"], writes=["identf"])
        triu = load_const("triu", BF16)
        ecap = load_const("ecap")
        onesb = sb.a([128], BF16)
        P.op("pool", lambda e: e.memset(onesb, 1.0), writes=["onesb"])
        epsc = sb.a([1], F32)
        P.op("pool", lambda e: e.memset(epsc, EPS), writes=["epsc"])
        wr = sb.a([8, 36], F32)
        P.dma("sp", wr, wr_d.rearrange("(kc p) n -> p kc n", p=128), writes=["wr"])
        rbb = sb.a([36], F32)
        P.dma("sp", rbb, rb_d, writes=["rb"])
        desti = sb.a([NT, 2], I32)
        wts = sb.a([NT, 2], F32)
        G.desti, G.wts = desti, wts
        G.mark_b = sb.off
        base = sb.a([NEXP], F32)
        P.op("pool", lambda e: e.memset(base, 0.0), writes=["base"])
        zt = sb.a([8 * D], BF16)
        P.op("pool", lambda e: e.memset(zt, 0.0), writes=["zt"])
        xsv = xs_d.rearrange("(n p r) d -> n p (r d)", p=128, r=8)
        for n_ in range(NB_ // 1024):
            P.dma("sp", xsv[n_], zt, reads=["zt"], writes=["xs_scr"], chan="c:xszero")
        SLOTK = {"xa", "junk", "ss", "rstd", "hb", "hT", "qxT", "mx4", "pe", "ssum", "pn", "pT", "oT", "x2t", "h2f", "h2b",
                 "h2T", "lg", "gmax", "goh", "ngmax", "gex", "gs", "pen", "lm", "m1", "oh0", "lm2", "m2", "oh1", "d12",
                 "cntb", "pos", "ovf", "tmp32", "destf"}

        def mkset(full):
            W = {}
            W["junk"] = sb.a([D], BF16)
            W["ss"] = sb.a([1], F32)
            W["rstd"] = sb.a([1], F32)
            W["hb"] = sb.a([D], BF16)
            W["xa"] = sb.a([D], F32)
            if not full:
                return W
            W["hT"] = sb.a([8, 128], BF16)
            W["qxT"] = sb.a([8, 128], BF16)
            W["mx4"] = sb.a([4], F32)
            W["pe"] = sb.a([4, 256], F32)
            W["ssum"] = sb.a([4], F32)
            W["pn"] = sb.a([4, 256], BF16)
            W["pT"] = sb.a([8, 128], BF16)
            W["oT"] = sb.a([8, 128], BF16)
            W["x2t"] = sb.a([D], F32)
            W["h2f"] = sb.a([D], F32)
            W["h2b"] = sb.a([D], BF16)
            W["h2T"] = sb.a([8, 128], F32)
            W["lg"] = sb.a([36], F32)
            W["r1"] = [sb.a([1], F32) for _ in range(8)]
            for nm_, n_ in (("goh", 4), ("gex", 4), ("pen", 4), ("lm2", 32), ("oh0", 32), ("oh1", 32), ("pos", 32),
                            ("ovf", 32), ("tmp32", 32), ("destf", 2)):
                W[nm_] = sb.a([n_], F32)
            W["lm"] = sb.a([4, 8], F32)
            W["cntb"] = sb.a([32], BF16)
            return W

        WS = [mkset(True), mkset(True), mkset(False)]
        memT = sb.a([8, 256], BF16)
        KTs = [sb.a([8, 256], BF16), sb.a([8, 256], BF16)]
        Vvs = [sb.a([2, D], BF16), sb.a([2, D], BF16)]

        def rmsnorm_b(W, src, srck, gain, gk, dst, dstk):
            junk, ss, rstd = W["junk"], W["ss"], W["rstd"]
            P.op("act", lambda e: e.activation(out=junk, in_=src, func=AF.Square, accum_out=ss),
                 reads=[srck], writes=["junk", "ss"])
            P.op("act", lambda e: e.activation(out=rstd, in_=ss, func=AF.Sqrt, bias=epsc, scale=1.0 / D),
                 reads=["ss", "epsc"], writes=["rstd"])
            P.op("dve", lambda e: e.reciprocal(out=rstd, in_=rstd), reads=["rstd"], writes=["rstd"])
            P.op("dve", lambda e: e.scalar_tensor_tensor(out=dst, in0=src, scalar=rstd, in1=gain,
                                                           op0=ALU.mult, op1=ALU.mult),
                 reads=[srck, "rstd", gk], writes=[dstk])

        def transpose8b(src, srck, dst, dstk):
            ptile, ptk = nextpt()
            for j in range(8):
                P.op("pe", lambda e, j=j: e.transpose(out=ptile[:, j, :], in_=src[:, j * 128:(j + 1) * 128],
                                                      identity=identb),
                     reads=[srck, "identb"], writes=[ptk], chain=True)
            P.op("act", lambda e: e.copy(out=dst, in_=ptile), reads=[ptk], writes=[dstk])

        def kv_seq(b):
            W = WS[2]
            KT, Vv = KTs[b % 2], Vvs[b % 2]
            ktk, vvk = "KT%d" % (b % 2), "Vv%d" % (b % 2)
            for mt in range(2):
                P.dma("sp", W["xa"], mem_d[b, mt * 128:(mt + 1) * 128, :], writes=["xa"])
                rmsnorm_b(W, W["xa"], "xa", gmem, "gmem", W["hb"], "hb")
                transpose8b(W["hb"], "hb", memT[:, :, mt * 128:(mt + 1) * 128], "memT")
            for half in range(2):
                pk, pkk = nextpp()
                for j4 in range(4):
                    jc = half * 4 + j4
                    for kc in range(8):
                        P.op("pe", lambda e, j4=j4, jc=jc, kc=kc, pk=pk: e.matmul(
                            pk[:, j4 * 256:(j4 + 1) * 256], lhsT=wkv[:, kc, jc * 128:(jc + 1) * 128],
                            rhs=memT[:, kc, :], start=(kc == 0), stop=(kc == 7)),
                            reads=["wkv", "memT"], writes=[pkk], chain=True)
                P.op("act", lambda e, half=half, pk=pk: e.copy(out=KT[:, half * 4:(half + 1) * 4, :].rearrange("p a b -> p (a b)"),
                                                               in_=pk[:, :]), reads=[pkk], writes=[ktk])
            for mt in range(2):
                pv, pvk = nextpp()
                for g in range(2):
                    for kc in range(8):
                        P.op("pe", lambda e, mt=mt, g=g, kc=kc, pv=pv: e.matmul(
                            pv[:, g * 512:(g + 1) * 512], lhsT=memT[:, kc, mt * 128:(mt + 1) * 128],
                            rhs=wkv[:, kc, D + g * 512:D + (g + 1) * 512], start=(kc == 0), stop=(kc == 7)),
                            reads=["wkv", "memT"], writes=[pvk], chain=True)
                P.op("act", lambda e, mt=mt, pv=pv: e.copy(out=Vv[:, mt, :], in_=pv[:, :]), reads=[pvk], writes=[vvk])

        def xa_tile(b, c):
            it = b * NCH + c
            W = WS[it % 2]
            KT, Vv = KTs[b % 2], Vvs[b % 2]
            ktk, vvk = "KT%d" % (b % 2), "Vv%d" % (b % 2)
            xt, hb, hT, qxT, mx4, pe_, ssum, pn, pT, oT = (W[k] for k in ("xa", "hb", "hT", "qxT", "mx4", "pe", "ssum", "pn", "pT", "oT"))
            x2t, h2f, h2b, h2T, lg = (W[k] for k in ("x2t", "h2f", "h2b", "h2T", "lg"))
            goh, gex, pen, lm, lm2, oh0, oh1, cntb, pos, ovf, tmp32, destf = (W[k] for k in (
                "goh", "gex", "pen", "lm", "lm2", "oh0", "oh1", "cntb", "pos", "ovf", "tmp32", "destf"))
            xk = "xa"
            P.dma("sp", xt, x1_d[it * 128:(it + 1) * 128, :], reads=["x1s%d" % it], writes=[xk])
            rmsnorm_b(W, xt, xk, gxa, "gxa", hb, "hb")
            transpose8b(hb, "hb", hT, "hT")
            pq, pqk_ = nextpp()
            for g in range(2):
                for kc in range(8):
                    P.op("pe", lambda e, g=g, kc=kc: e.matmul(pq[:, g * 512:(g + 1) * 512], lhsT=hT[:, kc, :],
                                                              rhs=wq[:, kc, g * 512:(g + 1) * 512],
                                                              start=(kc == 0), stop=(kc == 7)),
                         reads=["wq", "hT"], writes=[pqk_], chain=True)
            q_tok = W["junk"]
            P.op("act", lambda e: e.mul(out=q_tok, in_=pq[:, :], mul=1.0 / 16), reads=[pqk_], writes=["junk"])
            ptq_, ptqk_ = nextpt()
            for j in range(8):
                P.op("pe", lambda e, j=j: e.transpose(out=ptq_[:, j, :], in_=q_tok[:, j * 128:(j + 1) * 128], identity=identb),
                     reads=["junk", "identb"], writes=[ptqk_], chain=True)
            P.op("act", lambda e: e.copy(out=qxT, in_=ptq_), reads=[ptqk_], writes=["qxT"])
            pl, plk = nextpp()
            for hh in range(4):
                for i in range(2):
                    P.op("pe", lambda e, hh=hh, i=i: e.matmul(pl[:, hh * 256:(hh + 1) * 256], lhsT=qxT[:, 2 * hh + i, :],
                                                              rhs=KT[:, 2 * hh + i, :], start=(i == 0), stop=(i == 1)),
                         reads=["qxT", ktk], writes=[plk], chain=True)
            pl3 = pl[:, :].rearrange("p (a b) -> p a b", a=4)
            P.op("dve", lambda e: e.tensor_reduce(out=mx4, in_=pl3, axis=AX.X, op=ALU.max),
                 reads=[plk], writes=["mx4"])
            P.op("dve", lambda e: e.tensor_scalar(out=mx4, in0=mx4, scalar1=-1.0, scalar2=None, op0=ALU.mult),
                 reads=["mx4"], writes=["mx4"])
            for hh in range(4):
                P.op("act", lambda e, hh=hh: e.activation(out=pe_[:, hh, :], in_=pl3[:, hh, :], func=AF.Exp,
                                                          bias=mx4[:, hh:hh + 1], accum_out=ssum[:, hh:hh + 1]),
                     reads=[plk, "mx4"], writes=["pe", "ssum"])
            P.op("dve", lambda e: e.reciprocal(out=ssum, in_=ssum), reads=["ssum"], writes=["ssum"])
            P.op("dve", lambda e: e.tensor_tensor(out=pn, in0=pe_, in1=bc(ssum.unsqueeze(2), [128, 4, 256]), op=ALU.mult),
                 reads=["pe", "ssum"], writes=["pn"])
            pn2 = pn.rearrange("p a b -> p (a b)")
            ptp, ptpk = nextpt()
            for j in range(8):
                P.op("pe", lambda e, j=j: e.transpose(out=ptp[:, j, :], in_=pn2[:, j * 128:(j + 1) * 128], identity=identb),
                     reads=["pn", "identb"], writes=[ptpk], chain=True)
            P.op("act", lambda e: e.copy(out=pT, in_=ptp), reads=[ptpk], writes=["pT"])
            po, pok = nextpp()
            for hh in range(4):
                for dcc_ in range(2):
                    j = hh * 2 + dcc_
                    for mc in range(2):
                        P.op("pe", lambda e, hh=hh, dcc_=dcc_, j=j, mc=mc: e.matmul(
                            po[:, j * 128:(j + 1) * 128], lhsT=Vv[:, mc, hh * 256 + dcc_ * 128:hh * 256 + (dcc_ + 1) * 128],
                            rhs=pT[:, hh * 2 + mc, :], start=(mc == 0), stop=(mc == 1)),
                            reads=[vvk, "pT"], writes=[pok], chain=True)
            P.op("act", lambda e: e.copy(out=oT.rearrange("p a b -> p (a b)"), in_=po[:, :]), reads=[pok], writes=["oT"])
            py, pyk = nextpp()
            for g in range(2):
                for kc in range(8):
                    P.op("pe", lambda e, g=g, kc=kc: e.matmul(py[:, g * 512:(g + 1) * 512], lhsT=oT[:, kc, :],
                                                              rhs=wo[:, kc, g * 512:(g + 1) * 512],
                                                              start=(kc == 0), stop=(kc == 7)),
                         reads=["oT", "wo"], writes=[pyk], chain=True)
            for g in range(2):
                P.op("dve", lambda e, g=g: e.tensor_tensor(out=x2t[:, g * 512:(g + 1) * 512], in0=py[:, g * 512:(g + 1) * 512],
                                                           in1=xt[:, g * 512:(g + 1) * 512], op=ALU.add),
                     reads=[pyk, xk], writes=["x2t"])
            P.dma("sp", x2_d[it * 128:(it + 1) * 128, :], x2t, reads=["x2t"], writes=["x2s%d" % it], chan="c:x2t")
            if "moe" not in phases:
                return
            rmsnorm_b(W, x2t, "x2t", gmoe, "gmoe", h2f, "h2f")
            P.op("pool", lambda e: e.tensor_copy(out=h2b, in_=h2f), reads=["h2f"], writes=["h2b"])
            ph, phk = nextpp()
            for j in range(8):
                P.op("pe", lambda e, j=j: e.transpose(out=ph[:, j * 128:(j + 1) * 128], in_=h2f[:, j * 128:(j + 1) * 128],
                                                      identity=identf), reads=["h2f", "identf"], writes=[phk], f32=True)
            P.op("act", lambda e: e.copy(out=h2T.rearrange("p a b -> p (a b)"), in_=ph[:, :]), reads=[phk], writes=["h2T"])
            pr, prk = nextpp()
            for kc in range(8):
                P.op("pe", lambda e, kc=kc: e.matmul(pr[:, 0:36], lhsT=h2T[:, kc, :], rhs=wr[:, kc, :],
                                                     start=(kc == 0), stop=(kc == 7)), reads=["h2T", "wr"], writes=[prk], f32=True)
            P.op("dve", lambda e: e.tensor_tensor(out=lg, in0=pr[:, 0:36], in1=rbb, op=ALU.add),
                 reads=[prk, "rb"], writes=["lg"])
            gmax, ngmax, gs, m1, m2, d12, w0t, _ = W["r1"]
            V_ = lambda fn, r, w: P.op("dve", fn, reads=r, writes=w)
            V_(lambda e: e.tensor_reduce(out=gmax, in_=lg[:, 0:4], axis=AX.X, op=ALU.max), ["lg"], ["gmax"])
            V_(lambda e: e.tensor_scalar(out=goh, in0=lg[:, 0:4], scalar1=gmax, scalar2=None, op0=ALU.is_equal),
               ["lg", "gmax"], ["goh"])
            V_(lambda e: e.tensor_scalar(out=ngmax, in0=gmax, scalar1=-1.0, scalar2=None, op0=ALU.mult), ["gmax"], ["ngmax"])
            P.op("act", lambda e: e.activation(out=gex, in_=lg[:, 0:4], func=AF.Exp, bias=ngmax, accum_out=gs),
                 reads=["lg", "ngmax"], writes=["gex", "gs"])
            V_(lambda e: e.reciprocal(out=gs, in_=gs), ["gs"], ["gs"])
            V_(lambda e: e.tensor_scalar(out=pen, in0=goh, scalar1=-1.0, scalar2=1e9, op0=ALU.add, op1=ALU.mult),
               ["goh"], ["pen"])
            V_(lambda e: e.tensor_tensor(out=lm, in0=lg[:, 4:36].rearrange("p (a b) -> p a b", a=4),
                                         in1=bc(pen.unsqueeze(2), [128, 4, 8]), op=ALU.add), ["lg", "pen"], ["lm"])
            lmf = lm.rearrange("p a b -> p (a b)")
            V_(lambda e: e.tensor_reduce(out=m1, in_=lmf, axis=AX.X, op=ALU.max), ["lm"], ["m1"])
            V_(lambda e: e.tensor_scalar(out=oh0, in0=lmf, scalar1=m1, scalar2=None, op0=ALU.is_equal), ["lm", "m1"], ["oh0"])
            V_(lambda e: e.scalar_tensor_tensor(out=lm2, in0=oh0, scalar=-2e9, in1=lmf, op0=ALU.mult, op1=ALU.add),
               ["oh0", "lm"], ["lm2"])
            V_(lambda e: e.tensor_reduce(out=m2, in_=lm2, axis=AX.X, op=ALU.max), ["lm2"], ["m2"])
            V_(lambda e: e.tensor_scalar(out=oh1, in0=lm2, scalar1=m2, scalar2=None, op0=ALU.is_equal), ["lm2", "m2"], ["oh1"])
            V_(lambda e: e.tensor_tensor(out=d12, in0=m2, in1=m1, op=ALU.subtract), ["m1", "m2"], ["d12"])
            P.op("act", lambda e: e.activation(out=d12, in_=d12, func=AF.Exp), reads=["d12"], writes=["d12"])
            V_(lambda e: e.tensor_scalar(out=d12, in0=d12, scalar1=1.0, scalar2=None, op0=ALU.add), ["d12"], ["d12"])
            V_(lambda e: e.reciprocal(out=d12, in_=d12), ["d12"], ["d12"])
            V_(lambda e: e.tensor_tensor(out=wts[:, it, 0:1], in0=d12, in1=gs, op=ALU.mult), ["d12", "gs"], ["wts%d" % it])
            V_(lambda e: e.tensor_tensor(out=wts[:, it, 1:2], in0=gs, in1=wts[:, it, 0:1], op=ALU.subtract),
               ["gs", "wts%d" % it], ["wts%d" % it])
            V_(lambda e: e.tensor_tensor(out=cntb, in0=oh0, in1=oh1, op=ALU.add), ["oh0", "oh1"], ["cntb"])
            pp_, ppk = nextpp()
            P.op("pe", lambda e: e.matmul(pp_[:, 0:32], lhsT=triu, rhs=cntb, start=True, stop=True),
                 reads=["triu", "cntb"], writes=[ppk], chain=True)
            P.op("pe", lambda e: e.matmul(pp_[:, 32:64], lhsT=onesb, rhs=cntb, start=True, stop=True),
                 reads=["onesb", "cntb"], writes=[ppk], chain=True)
            V_(lambda e: e.tensor_tensor(out=pos, in0=pp_[:, 0:32], in1=base, op=ALU.add), [ppk, "base"], ["pos"])
            V_(lambda e: e.tensor_tensor(out=base, in0=pp_[:, 32:64], in1=base, op=ALU.add), [ppk, "base"], ["base"])
            V_(lambda e: e.tensor_scalar(out=ovf, in0=pos, scalar1=float(CAP), scalar2=1e6, op0=ALU.is_ge, op1=ALU.mult),
               ["pos"], ["ovf"])
            V_(lambda e: e.tensor_tensor(out=pos, in0=pos, in1=ecap, op=ALU.add), ["pos", "ecap"], ["pos"])
            V_(lambda e: e.tensor_tensor(out=pos, in0=pos, in1=ovf, op=ALU.add), ["pos", "ovf"], ["pos"])
            V_(lambda e: e.scalar_tensor_tensor(out=tmp32, in0=oh0, scalar=1.0, in1=pos, op0=ALU.mult, op1=ALU.mult,
                                                accum_out=destf[:, 0:1]), ["oh0", "pos"], ["tmp32", "destf"])
            V_(lambda e: e.scalar_tensor_tensor(out=tmp32, in0=oh1, scalar=1.0, in1=pos, op0=ALU.mult, op1=ALU.mult,
                                                accum_out=destf[:, 1:2]), ["oh1", "pos"], ["tmp32", "destf"])
            V_(lambda e: e.tensor_copy(out=desti[:, it, :], in_=destf), ["destf"], ["desti%d" % it])
            for k in range(2):
                P.dma_fn("pool", lambda e, k=k: nc.gpsimd.indirect_dma_start(
                    out=xs_d, out_offset=bass.IndirectOffsetOnAxis(ap=desti[:, it, k:k + 1], axis=0),
                    in_=h2b, in_offset=None, bounds_check=breg(e), oob_is_err=False),
                    reads=["h2b", "desti%d" % it, "xs_scr"], writes=["xsd%d_%d" % (it, k)], chan="c:xsd%d" % (it % 2))

        tiles = []
        for b in range(NS):
            for c in range(NCH):
                it = b * NCH + c
                P.record()
                if c == 0:
                    P.keymap = (SLOTK, "_2")
                    ppset[0] = (0, 1) if it % 2 == 0 else (2, 3)
                    kv_seq(b)
                P.keymap = (SLOTK, "_%d" % (it % 2))
                ppset[0] = (0, 1) if it % 2 == 0 else (2, 3)
                xa_tile(b, c)
                P.keymap = None
                tiles.append(P.stop())
        import os as _os3
        if _os3.environ.get('SCHED', '1') == '1':
            P.schedule(tiles)
        else:
            P.pipeline(tiles, frac=float(_os3.environ.get('FRACB', '0.5')))
        ppset[0] = (0, 1, 2, 3)
        P.barrier()
        sb.off = G.mark_b

    if "xa" in phases:
        phase_xa()

    def phase_moe():
        desti, wts = G.desti, G.wts
        GS = 256 if CAP % 256 == 0 else 128
        wbuf = [(sb.a([8, 512], BF16), sb.a([8, 512], BF16), sb.a([4, D], BF16)) for _ in range(2)]
        NTB = GS // 128
        xb = [[sb.a([D], BF16) for _ in range(NTB)] for _ in range(2)]
        xbT_ = [sb.a([8, GS], BF16) for _ in range(2)]
        sgl_ = [sb.a([4 * GS], F32) for _ in range(2)]
        hid_ = [sb.a([4, GS], BF16) for _ in range(2)]
        ysb = [sb.a([D], BF16), sb.a([D], BF16)]
        NG = CAP // GS

        def load_w(e_):
            wg_t, wu_t, wd_t = wbuf[e_ % 2]
            k = "wb%d" % (e_ % 2)
            P.dma("pool", wg_t, wg_d[e_].rearrange("(kc p) n -> p kc n", p=128), writes=[k])
            P.dma("pool", wu_t, wu_d[e_].rearrange("(kc p) n -> p kc n", p=128), writes=[k])
            P.dma("pool", wd_t, wd_d[e_].rearrange("(kc p) n -> p kc n", p=128), writes=[k])

        def load_x(gi):
            e_, grp = divmod(gi, NG)
            r0 = e_ * CAP + grp * GS
            for tb in range(NTB):
                P.dma("sp", xb[gi % 2][tb], xs_d[r0 + tb * 128:r0 + (tb + 1) * 128, :], reads=["xs_scr"],
                      writes=["xb%d_%d" % (gi % 2, tb)])

        def group(gi):
            e_, grp = divmod(gi, NG)
            wg_t, wu_t, wd_t = wbuf[e_ % 2]
            wk = "wb%d" % (e_ % 2)
            r0 = e_ * CAP + grp * GS
            xbT, sgl, hid = xbT_[gi % 2], sgl_[gi % 2], hid_[gi % 2]
            xbTk, sglk, hidk = "xbT%d" % (gi % 2), "sgl%d" % (gi % 2), "hid%d" % (gi % 2)
            for tb in range(NTB):
                xt_ = xb[gi % 2][tb]
                xk_ = "xb%d_%d" % (gi % 2, tb)
                ptx, ptxk = nextpt()
                for j in range(8):
                    P.op("pe", lambda e, j=j, xt_=xt_, ptx=ptx: e.transpose(out=ptx[:, j, :],
                                                                            in_=xt_[:, j * 128:(j + 1) * 128], identity=identb),
                         reads=[xk_, "identb"], writes=[ptxk], chain=True)
                P.op("act", lambda e, tb=tb, ptx=ptx: e.copy(out=xbT[:, :, tb * 128:(tb + 1) * 128], in_=ptx),
                     reads=[ptxk], writes=[xbTk])
            pa, pak = nextpp()
            pb_, pbk = nextpp()
            for (pdst, pdk, wt_) in ((pa, pak, wg_t), (pb_, pbk, wu_t)):
                for fc in range(4):
                    for kc in range(8):
                        P.op("pe", lambda e, pdst=pdst, wt_=wt_, fc=fc, kc=kc: e.matmul(
                            pdst[:, fc * GS:(fc + 1) * GS], lhsT=wt_[:, kc, fc * 128:(fc + 1) * 128], rhs=xbT[:, kc, :],
                            start=(kc == 0), stop=(kc == 7)), reads=[wk, xbTk], writes=[pdk], chain=True)
            P.op("act", lambda e, pa=pa: e.activation(out=sgl, in_=pa[:, 0:4 * GS], func=AF.Silu), reads=[pak], writes=[sglk])
            P.op("dve", lambda e, pb_=pb_: e.tensor_tensor(out=hid.rearrange("p a b -> p (a b)"), in0=pb_[:, 0:4 * GS], in1=sgl,
                                                           op=ALU.mult), reads=[pbk, sglk], writes=[hidk])
            for tb in range(NTB):
                pc_, pck_ = nextpp()
                for g in range(2):
                    for fc in range(4):
                        P.op("pe", lambda e, g=g, fc=fc, tb=tb, pc_=pc_: e.matmul(
                            pc_[:, g * 512:(g + 1) * 512], lhsT=hid[:, fc, tb * 128:(tb + 1) * 128],
                            rhs=wd_t[:, fc, g * 512:(g + 1) * 512], start=(fc == 0), stop=(fc == 3)),
                            reads=[hidk, wk], writes=[pck_], chain=True)
                yi = tb % 2
                if tb % 2 == 0:
                    P.op("act", lambda e, yi=yi, pc_=pc_: e.copy(out=ysb[yi], in_=pc_[:, :]), reads=[pck_], writes=["ysb%d" % yi])
                else:
                    P.op("dve", lambda e, yi=yi, pc_=pc_: e.tensor_copy(out=ysb[yi], in_=pc_[:, :]), reads=[pck_],
                         writes=["ysb%d" % yi])
                P.dma("pool", ys_d[r0 + tb * 128:r0 + (tb + 1) * 128, :], ysb[yi], reads=["ysb%d" % yi],
                      writes=["ys_scr"], chan="c:ysb%d" % yi)

        load_w(0)
        glists = []
        for gi in range(NEXP * NG):
            e_, grp = divmod(gi, NG)
            P.record()
            if grp == 0 and e_ + 1 < NEXP:
                load_w(e_ + 1)
            load_x(gi)
            ppset[0] = (0, 1) if gi % 2 == 0 else (2, 3)
            group(gi)
            glists.append(P.stop())
        ppset[0] = (0, 1, 2, 3)
        P.schedule(glists, win=3)
        P.barrier()
        sb.off = G.mark_b
        gfin = load_gain(4, "gfin")
        epsd = sb.a([1], F32)
        P.op("pool", lambda e: e.memset(epsd, EPS), writes=["epsd"])
        xd = [sb.a([D], F32), sb.a([D], F32)]
        yk = [[sb.a([D], BF16), sb.a([D], BF16)] for _ in range(2)]
        zt2 = [sb.a([D], F32), sb.a([D], F32)]
        junkd2 = [sb.a([D], BF16), sb.a([D], BF16)]
        ssd2 = [sb.a([1], F32), sb.a([1], F32)]
        rsd2 = [sb.a([1], F32), sb.a([1], F32)]
        ot = [sb.a([D], F32), sb.a([D], F32)]
        for par in range(2):
            for k in range(2):
                P.op("pool", lambda e, par=par, k=k: e.memset(yk[par][k], 0.0), writes=["yk%d%d" % (par, k)])

        def fin_load(it):
            par = it % 2
            P.dma("sp", xd[par], x2_d[it * 128:(it + 1) * 128, :], reads=["x2s%d" % it], writes=["xd%d" % par])
            for k in range(2):
                P.dma_fn("pool", lambda e, k=k: nc.gpsimd.indirect_dma_start(
                    out=yk[par][k], out_offset=None, in_=ys_d,
                    in_offset=bass.IndirectOffsetOnAxis(ap=desti[:, it, k:k + 1], axis=0),
                    bounds_check=breg(e), oob_is_err=False),
                    reads=["ys_scr"], writes=["yk%d%d" % (par, k)])

        def fin_tile(it):
            b, c = divmod(it, NCH)
            par = it % 2
            zt_, junkd, ssd, rsd = zt2[par], junkd2[par], ssd2[par], rsd2[par]
            P.keymap = ({"zt_", "junkd", "ssd", "rsd"}, str(par))
            try:
                fin_tile_(it, b, c, par, zt_, junkd, ssd, rsd)
            finally:
                P.keymap = None

        def fin_tile_(it, b, c, par, zt_, junkd, ssd, rsd):
            P.op("dve", lambda e: e.scalar_tensor_tensor(out=zt_, in0=yk[par][0], scalar=wts[:, it, 0:1], in1=xd[par],
                                                         op0=ALU.mult, op1=ALU.add),
                 reads=["yk%d0" % par, "xd%d" % par], writes=["zt_"])
            P.op("dve", lambda e: e.scalar_tensor_tensor(out=zt_, in0=yk[par][1], scalar=wts[:, it, 1:2], in1=zt_,
                                                          op0=ALU.mult, op1=ALU.add),
                 reads=["yk%d1" % par, "zt_"], writes=["zt_"])
            P.op("act", lambda e: e.activation(out=junkd, in_=zt_, func=AF.Square, accum_out=ssd),
                 reads=["zt_"], writes=["junkd", "ssd"])
            P.op("act", lambda e: e.activation(out=rsd, in_=ssd, func=AF.Sqrt, bias=epsd, scale=1.0 / D),
                 reads=["ssd", "epsd"], writes=["rsd"])
            P.op("dve", lambda e: e.reciprocal(out=rsd, in_=rsd), reads=["rsd"], writes=["rsd"])
            P.op("dve", lambda e: e.scalar_tensor_tensor(out=ot[par], in0=zt_, scalar=rsd, in1=gfin, op0=ALU.mult,
                                                         op1=ALU.mult), reads=["zt_", "rsd", "gfin"], writes=["ot%d" % par])
            P.dma("sp", out_d[b, c * 128:(c + 1) * 128, :], ot[par], reads=["ot%d" % par], writes=["out%d" % it],
                  chan="c:ot%d" % par)

        flists = []
        for it in range(NT):
            P.record()
            fin_load(it)
            fin_tile(it)
            flists.append(P.stop())
        P.schedule(flists, win=3)
        P.wait_for("sp", ["out%d" % it for it in range(NT)])

    if "moe" in phases:
        phase_moe()
    else:
        src_d, skey = (x2_d, "x2s%d") if "xa" in phases else (x1_d, "x1s%d")
        t = sb.a([D], F32)
        for it in range(NT):
            b, c = divmod(it, NCH)
            P.dma("sp", t, src_d[it * 128:(it + 1) * 128, :], reads=[skey % it], writes=["dbgt"])
            P.dma("sp", out_d[b, c * 128:(c + 1) * 128, :], t, reads=["dbgt"], writes=["out%d" % it], chan="c:out")
        P.wait_for("sp", ["out%d" % it for it in range(NT)])

    P.emit(st)
    st.close()
    return nc, P, sb


_CACHE = {}


def _host_layout(inp):
    f = lambda a: np.ascontiguousarray(np.asarray(a, dtype=np.float32))
    m = {}
    m["w_in"] = f(inp["w_in"][0])
    m["w_out"] = f(inp["w_out"][0])
    m["xa_wq"] = f(inp["xa_wq"][0])
    m["xa_wkv"] = f(inp["xa_wkv"][0])
    m["xa_wo"] = f(inp["xa_wo"][0])
    m["moe_w_gate"] = f(inp["moe_w_gate"][0])
    m["moe_w_up"] = f(inp["moe_w_up"][0])
    m["moe_w_down"] = f(inp["moe_w_down"][0])
    hn = np.concatenate([np.asarray(inp["ret_norm_w"][0]), np.asarray(inp["ml_norm_w"][0])])
    g = np.stack([np.asarray(inp["norm_mix_w"][0]), np.asarray(inp["norm_xa_w"][0]), np.asarray(inp["norm_mem_w"][0]),
                  np.asarray(inp["norm_moe_w"][0]), np.asarray(inp["norm_final_w"]), hn])
    m["gains"] = f(np.broadcast_to(g[:, None, :], (6, 128, D)))
    cw = np.concatenate([np.asarray(inp["ml_conv_w"][0]), np.asarray(inp["ml_conv_b"][0])[None]], 0)
    m["convw"] = f(cw.reshape(5, 8, 128).transpose(2, 1, 0))
    m["gateb"] = f(np.asarray(inp["ml_gate_b"][0]).reshape(2, 4).T)
    m["wr"] = f(np.concatenate([np.asarray(inp["moe_w_group"][0]), np.asarray(inp["moe_w_router"][0])], 1))
    rb = np.concatenate([np.asarray(inp["moe_b_group"][0]), np.asarray(inp["moe_b_router"][0])])
    m["rb"] = f(np.broadcast_to(rb[None], (128, 36)))
    for k, v in host_consts().items():
        m["c_" + k] = v
    return m


def kernel(**inputs):
    ncores = 8
    x = np.asarray(inputs["x"], dtype=np.float32)
    mem = np.asarray(inputs["mem"], dtype=np.float32)
    B = x.shape[0]
    NS = B // ncores
    NCH = x.shape[1] // 128
    key = (NS, NCH)
    if key not in _CACHE:
        _CACHE[key] = build_program(NS, NCH)[0]
    nc = _CACHE[key]
    shared = _host_layout(inputs)
    in_maps = []
    for c in range(ncores):
        m = dict(shared)
        m["x"] = np.ascontiguousarray(x[c * NS:(c + 1) * NS])
        m["mem"] = np.ascontiguousarray(mem[c * NS:(c + 1) * NS])
        in_maps.append(m)
    res = run_bass_kernel_spmd(nc, in_maps, core_ids=list(range(ncores)))
    return np.concatenate([r["out"] for r in res.results], axis=0).astype(np.float32)
```

```python
from contextlib import ExitStack
import numpy as np
import concourse.bass as bass
import concourse.mybir as mybir
from concourse.bass_utils import run_bass_kernel_spmd

F32 = mybir.dt.float32
BF16 = mybir.dt.bfloat16
I32 = mybir.dt.int32
U8 = mybir.dt.uint8
ALU = mybir.AluOpType
AF = mybir.ActivationFunctionType
AX = mybir.AxisListType

ENGS = ("pe", "act", "dve", "pool", "sp")
D = 1024
S = 2048
CAP = 768
NEXP = 32
EPS = 1e-6


class _Op:
    __slots__ = ("eng", "fn", "deps", "is_dma", "chan", "sig", "val", "waits", "bar", "f32", "chain")


class Prog:
    def __init__(self, nc):
        self.nc = nc
        self.ops = []
        self.last_w = {}
        self.readers = {}
        self._rec = None
        self._stack = []
        self.keymap = None

    def record(self):
        self._stack.append(self._rec)
        self._rec = []

    def stop(self):
        r = self._rec
        self._rec = self._stack.pop()
        return r

    def replay(self, items):
        for it in items:
            self._add(*it)

    @staticmethod
    def _block_heads(l):
        heads = []
        for j, it in enumerate(l):
            if j > 0 and it[0] == "pe" and l[j - 1][0] == "pe" and it[3] and l[j - 1][3] and it[3][0] == l[j - 1][3][0]:
                heads.append(heads[-1])
            else:
                heads.append(j)
        return heads

    def merge_list(self, lists):
        allops = []
        for li, l in enumerate(lists):
            n = max(len(l), 1)
            hd = self._block_heads(l)
            for j, it in enumerate(l):
                allops.append(((hd[j] + 0.5) / n, li, j, it))
        allops.sort(key=lambda t: (t[0], t[1], t[2]))
        return [t[3] for t in allops]

    def merge(self, lists):
        allops = []
        for li, l in enumerate(lists):
            n = max(len(l), 1)
            hd = self._block_heads(l)
            for j, it in enumerate(l):
                allops.append(((hd[j] + 0.5) / n, li, j, it))
        allops.sort(key=lambda t: (t[0], t[1], t[2]))
        self.replay([t[3] for t in allops])

    def schedule(self, lists, win=3):
        from collections import defaultdict
        COST = {"pe": 0.16, "act": 0.45, "dve": 0.35, "pool": 0.9, "sp": 0.05}
        lists = [[(it[0], it[1], self._expand(it[2]), self._expand(it[3]), it[4], it[5]) for it in l] for l in lists]
        written = set()
        for l in lists:
            for it in l:
                written.update(it[3])
        cnt = defaultdict(lambda: defaultdict(int))
        opkeys = []
        for t, l in enumerate(lists):
            ok = []
            for it in l:
                ks = [k for k in set(it[2]) | set(it[3]) if k in written]
                ok.append(ks)
                for k in ks:
                    cnt[k][t] += 1
            opkeys.append(ok)
        tiles_of = {k: sorted(d) for k, d in cnt.items()}
        fptr = {k: 0 for k in cnt}

        def front(k):
            arr = tiles_of[k]
            p = fptr[k]
            while p < len(arr) and cnt[k][arr[p]] == 0:
                p += 1
            fptr[k] = p
            return arr[p] if p < len(arr) else 1 << 30

        heads = [self._block_heads(l) for l in lists]
        pos = [0] * len(lists)
        eng_free = defaultdict(float)
        kw = defaultdict(float)
        kr = defaultdict(float)
        order = []
        lo = 0
        n = len(lists)
        while lo < n:
            while lo < n and pos[lo] >= len(lists[lo]):
                lo += 1
            if lo >= n:
                break
            best = None
            for t in range(lo, min(n, lo + win)):
                if pos[t] >= len(lists[t]):
                    continue
                j = pos[t]
                blocked = False
                jj = j
                while True:
                    if any(front(k) < t for k in opkeys[t][jj]):
                        blocked = True
                        break
                    jj += 1
                    if jj >= len(lists[t]) or heads[t][jj] != heads[t][j]:
                        break
                if blocked:
                    continue
                it = lists[t][j]
                st_ = eng_free[it[0]]
                for r in it[2]:
                    st_ = max(st_, kw[r])
                for w in it[3]:
                    st_ = max(st_, kw[w], kr[w])
                if best is None or st_ < best[0]:
                    best = (st_, t)
            t = best[1]
            j = pos[t]
            hd = heads[t][j]
            while True:
                it = lists[t][j]
                eng = it[0]
                st_ = eng_free[eng]
                for r in it[2]:
                    st_ = max(st_, kw[r])
                for w in it[3]:
                    st_ = max(st_, kw[w], kr[w])
                if it[4]:
                    eng_free[eng] = st_ + 0.05
                    fin = st_ + 2.5
                else:
                    fin = st_ + COST.get(eng, 0.3)
                    eng_free[eng] = fin
                for r in it[2]:
                    kr[r] = max(kr[r], fin + 0.1)
                for w in it[3]:
                    kw[w] = fin + 0.1
                    kr[w] = 0.0
                for k in opkeys[t][j]:
                    cnt[k][t] -= 1
                order.append(it)
                j += 1
                if j >= len(lists[t]) or heads[t][j] != hd:
                    break
            pos[t] = j
        self.replay(order)

    def pipeline(self, lists, frac=0.5):
        allops = []
        L = max(len(l) for l in lists)
        stride = L * frac
        for t, l in enumerate(lists):
            hd = self._block_heads(l)
            for j, it in enumerate(l):
                allops.append((t * stride + hd[j], t, j, it))
        allops.sort(key=lambda t: (t[0], t[1], t[2]))
        self.replay([t[3] for t in allops])

    def _add(self, eng, fn, reads, writes, is_dma=False, chan=None):
        if self.keymap is not None:
            ks, sfx = self.keymap
            reads = [k + sfx if k in ks else k for k in reads]
            writes = [k + sfx if k in ks else k for k in writes]
            if chan is not None and chan != "f32" and not chan.startswith("chain") and chan[2:] in ks:
                chan = chan + sfx
        if self._rec is not None:
            km, self.keymap = self.keymap, None
            self._rec.append((eng, fn, reads, writes, is_dma, chan))
            self.keymap = km
            return None
        km, self.keymap = self.keymap, None
        try:
            return self._add2(eng, fn, reads, writes, is_dma, chan)
        finally:
            self.keymap = km

    @staticmethod
    def _expand(keys):
        out = []
        for k in keys:
            if len(k) == 3 and k[:2] == "pp" and k[2].isdigit():
                out.append(k + "a")
                out.append(k + "b")
            else:
                out.append(k)
        return out

    def _add2(self, eng, fn, reads, writes, is_dma=False, chan=None):
        reads = self._expand(reads)
        writes = self._expand(writes)
        o = _Op()
        o.eng, o.fn, o.is_dma, o.chan = eng, fn, is_dma, chan
        o.f32 = (not is_dma) and chan == "f32"
        o.chain = chan[5:] if ((not is_dma) and chan is not None and chan.startswith("chain")) else ""
        if o.f32 and eng == "pe":
            o.chain = "A"
        o.sig = False
        o.val = None
        if eng == "pe" and not is_dma:
            lp = getattr(self, "_last_pe", None)
            self._last_pe = o
        else:
            lp = None
        deps = []
        for k in reads:
            w = self.last_w.get(k)
            if w is not None:
                deps.append(w)
        keep_readers = set()
        for k in writes:
            w = self.last_w.get(k)
            if w is not None:
                if is_dma and w.is_dma and w.chan == chan:
                    keep_readers.add(k)
                else:
                    deps.append(w)
            deps.extend(self.readers.get(k, ()))
        seen = set()
        o.deps = []
        for d in deps:
            if d is o or id(d) in seen:
                continue
            if o.chain and getattr(d, "chain", "") == o.chain:
                continue
            seen.add(id(d))
            o.deps.append(d)
        for k in writes:
            self.last_w[k] = o
            if k not in keep_readers:
                self.readers[k] = []
        for k in reads:
            if k not in writes:
                self.readers.setdefault(k, []).append(o)
        self.ops.append(o)
        return o

    def op(self, eng, fn, reads=(), writes=(), f32=False, chain=False):
        if chain is True:
            chain = "A"
        return self._add(eng, fn, list(reads), list(writes), False, "f32" if f32 else (("chain" + chain) if chain else None))

    def dma(self, eng, out, in_, reads=(), writes=(), chan=None, **kw):
        reads, writes = list(reads), list(writes)
        if chan is None:
            chan = "c:" + (writes[0] if writes else reads[0])
        nc = self.nc
        q = {"pool": nc.gpsimd, "sp": nc.sync, "act": nc.scalar}[eng]
        fn = lambda e: q.dma_start(out=out, in_=in_, **kw)
        return self._add(eng, fn, reads, writes, is_dma=True, chan=chan)

    def dma_fn(self, eng, fn, reads=(), writes=(), chan=None):
        reads, writes = list(reads), list(writes)
        if chan is None:
            chan = "c:" + (writes[0] if writes else reads[0])
        return self._add(eng, fn, reads, writes, is_dma=True, chan=chan)

    def wait_for(self, eng, keys):
        return self._add(eng, None, list(keys), [])

    def barrier(self):
        last = {}
        for o in self.ops:
            if o.fn is None:
                continue
            last[(o.chan if o.is_dma else o.eng)] = o
        deps = list(last.values())
        for e in ENGS:
            o = _Op()
            o.eng, o.fn, o.is_dma, o.chan, o.sig, o.val = e, None, False, None, False, None
            o.deps = deps
            o.f32 = False
            o.chain = ""
            o.waits = None
            self.ops.append(o)

    def emit(self, stack):
        nc = self.nc
        for o in self.ops:
            for d in o.deps:
                d.sig = True
        cnt = {e: 0 for e in ENGS}
        chan_cnt = {}
        for o in self.ops:
            if o.fn is None:
                continue
            if o.is_dma:
                chan_cnt[o.chan] = chan_cnt.get(o.chan, 0) + 16
                o.val = chan_cnt[o.chan]
            elif o.sig:
                cnt[o.eng] += 1
                o.val = cnt[o.eng]
        sems = {}
        for e in ENGS:
            sems["e:" + e] = stack.enter_context(nc.semaphore("sem_" + e))
        for i, c in enumerate(sorted(chan_cnt)):
            sems[c] = stack.enter_context(nc.semaphore("semc_%d" % i))
        self.n_sems = len(sems)
        waited = {e: {} for e in ENGS}
        per_eng = {e: [] for e in ENGS}
        for o in self.ops:
            ws = {}
            for d in o.deps:
                if d.val is None:
                    continue
                s = d.chan if d.is_dma else "e:" + d.eng
                if ws.get(s, 0) < d.val:
                    ws[s] = d.val
            o.waits = []
            isbar = (o.fn is None and len(o.deps) > 8)
            for s, v in ws.items():
                if isbar or waited[o.eng].get(s, 0) < v:
                    waited[o.eng][s] = v
                    o.waits.append((s, v))
            per_eng[o.eng].append(o)
        block = stack.enter_context(nc.Block())
        self.counts = {e: len(per_eng[e]) for e in ENGS}

        def run(e, eng):
            for o in per_eng[e]:
                for s, v in o.waits:
                    eng.wait_ge(sems[s], v)
                if o.fn is None:
                    continue
                ins = o.fn(eng)
                if o.is_dma:
                    ins.then_inc(sems[o.chan], 16)
                elif o.sig:
                    ins.then_inc(sems["e:" + e], 1)

        @block.tensor
        def _(eng):
            run("pe", eng)

        @block.scalar
        def _(eng):
            run("act", eng)

        @block.vector
        def _(eng):
            run("dve", eng)

        @block.gpsimd
        def _(eng):
            run("pool", eng)

        @block.sync
        def _(eng):
            run("sp", eng)


_DTSZ = {F32: 4, BF16: 2, I32: 4}


class SB:
    def __init__(self, nc, nbytes):
        self.t = nc.alloc_sbuf_tensor("sbuf_all", [128, nbytes], U8)
        self.off = 0
        self.cap = nbytes
        self.hi = 0

    def a(self, free, dt, parts=128):
        if isinstance(free, int):
            free = [free]
        n = int(np.prod(free)) * _DTSZ[dt]
        off = (self.off + 31) // 32 * 32
        self.off = off + n
        self.hi = max(self.hi, self.off)
        assert self.off <= self.cap, ("SBUF overflow", self.off, self.cap)
        ap = self.t[0:parts, off:off + n].bitcast(dt)
        if len(free) == 2:
            ap = ap.rearrange("p (a b) -> p a b", a=free[0], b=free[1])
        elif len(free) == 3:
            ap = ap.rearrange("p (a b c) -> p a b c", a=free[0], b=free[1], c=free[2])
        return ap


def bc(ap, shape):
    return ap.to_broadcast(list(shape))


def host_consts():
    c = {}
    c["identb"] = np.eye(128, dtype=np.float32)
    log_g = np.log1p(-np.exp2(-5.0 - np.arange(4, dtype=np.float64)))
    n = np.arange(128, dtype=np.float64)
    diff = n[:, None] - n[None, :]
    dmat = np.where(diff[None] >= 0, np.exp(log_g[:, None, None] * np.maximum(diff, 0.0)[None]), 0.0)
    c["dmatT"] = np.ascontiguousarray(dmat.transpose(2, 0, 1)).astype(np.float32)
    gq = np.exp((n[None, :] + 1) * log_g[:, None])
    gqT = np.zeros((128, 2, 128))
    for p in range(2):
        gqT[:64, p, :] = gq[2 * p][None, :]
        gqT[64:, p, :] = gq[2 * p + 1][None, :]
    c["gqT"] = gqT.astype(np.float32)
    c["gkc"] = (np.exp((127 - n)[:, None] * log_g[None, :]) * 0.125).astype(np.float32)
    dc = np.zeros((128, 2))
    for p in range(2):
        dc[:64, p] = np.exp(128 * log_g[2 * p])
        dc[64:, p] = np.exp(128 * log_g[2 * p + 1])
    c["dc"] = dc.astype(np.float32)
    half = 32
    inv = 10000.0 ** (-np.arange(half, dtype=np.float64) / half)
    ang = np.arange(S, dtype=np.float64)[:, None] * inv[None, :]
    ang = ang.astype(np.float32).astype(np.float64)
    c["cos"] = np.ascontiguousarray(np.cos(ang).reshape(16, 128, 32).transpose(1, 0, 2)).astype(np.float32)
    c["sin"] = np.ascontiguousarray(np.sin(ang).reshape(16, 128, 32).transpose(1, 0, 2)).astype(np.float32)
    s_idx = np.arange(128)
    c["maskneg"] = np.where(s_idx[:, None] <= s_idx[None, :], 0.0, -30000.0).astype(np.float32)
    eh = np.zeros((4, 4, 128), np.float32)
    for h in range(4):
        eh[h, h, :] = 1.0
    c["eh"] = eh
    c["ident4"] = np.eye(4, dtype=np.float32)
    c["triu"] = (s_idx[:, None] < s_idx[None, :]).astype(np.float32)
    c["ecap"] = np.broadcast_to((np.arange(NEXP) * CAP).astype(np.float32)[None, :], (128, NEXP)).copy()
    return c


CONST_SHAPES = {
    "identb": [128, 128], "dmatT": [128, 4, 128], "gqT": [128, 2, 128], "gkc": [128, 4], "dc": [128, 2],
    "cos": [128, 16, 32], "sin": [128, 16, 32], "maskneg": [128, 128], "eh": [4, 4, 128], "ident4": [4, 4],
    "triu": [128, 128], "ecap": [128, NEXP],
}


def build_program(NS=4, NCH=16, phases=("mix", "xa", "moe"), dbg=False):
    SL = NCH * 128
    NT = NS * NCH
    NTOK = NT * 128
    nc = bass.Bass("TRN2", target_bir_lowering=False)

    def din(name, shape, dt=F32):
        return nc.dram_tensor(name, list(shape), dt, kind="ExternalInput").ap()

    x_d = din("x", [NS, SL, D])
    mem_d = din("mem", [NS, 256, D])
    w_in_d = din("w_in", [D, 3592])
    w_out_d = din("w_out", [D, D])
    wq_d = din("xa_wq", [D, D])
    wkv_d = din("xa_wkv", [D, 2 * D])
    wo_d = din("xa_wo", [D, D])
    wg_d = din("moe_w_gate", [NEXP, D, 512])
    wu_d = din("moe_w_up", [NEXP, D, 512])
    wd_d = din("moe_w_down", [NEXP, 512, D])
    gains_d = din("gains", [6, 128, D])
    convw_d = din("convw", [128, 8, 5])
    gateb_d = din("gateb", [4, 2])
    wr_d = din("wr", [D, 36])
    rb_d = din("rb", [128, 36])
    cd = {k: din("c_" + k, v) for k, v in CONST_SHAPES.items()}
    out_d = nc.dram_tensor("out", [NS, SL, D], F32, kind="ExternalOutput").ap()
    x1_d = nc.dram_tensor("x1_scr", [NTOK, D], F32, kind="Internal").ap()
    x2_d = nc.dram_tensor("x2_scr", [NTOK, D], F32, kind="Internal").ap()
    xs_d = nc.dram_tensor("xs_scr", [NEXP * CAP, D], BF16, kind="Internal").ap()
    ys_d = nc.dram_tensor("ys_scr", [NEXP * CAP, D], BF16, kind="Internal").ap()

    st = ExitStack()
    P = Prog(nc)
    sb = SB(nc, 212800)
    pp = [nc.alloc_psum_tensor("pp%d" % i, [128, 1024], F32) for i in range(4)]
    ppb = [t[:, 0:512].bitcast(BF16).rearrange("p (a b) -> p a b", a=8, b=128) for t in pp]
    ppb2 = [t[:, 512:1024].bitcast(BF16).rearrange("p (a b) -> p a b", a=8, b=128) for t in pp]
    rot = {}
    ppset = [(0, 1, 2, 3)]

    def nextpp(full=True):
        sset = ppset[0]
        if len(sset) == 1 and not full:
            r = rot.get((sset, "h"), 0)
            rot[(sset, "h")] = r + 1
            i = sset[0]
            if r % 2 == 0:
                return pp[i][:, 0:512], "pp%da" % i
            return pp[i][:, 512:1024], "pp%db" % i
        r = rot.get(sset, 0)
        rot[sset] = r + 1
        i = sset[r % len(sset)]
        return pp[i], "pp%d" % i

    def nextpt():
        sset = ppset[0]
        if len(sset) == 1:
            r = rot.get((sset, "h"), 0)
            rot[(sset, "h")] = r + 1
            i = sset[0]
            if r % 2 == 0:
                return ppb[i], "pp%da" % i
            return ppb2[i], "pp%db" % i
        r = rot.get(sset, 0)
        rot[sset] = r + 1
        i = sset[r % len(sset)]
        return ppb[i], "pp%d" % i

    def load_const(name, dt=F32, parts=128, eng="sp"):
        shp = CONST_SHAPES[name]
        t = sb.a(shp[1:], dt, parts=shp[0])
        if dt == F32:
            P.dma(eng, t, cd[name], writes=[name])
        else:
            P.dma("pool", t, cd[name], writes=[name])
        return t

    identb = load_const("identb", BF16)
    gains = {}

    def load_gain(i, name):
        t = sb.a([D], F32)
        P.dma("sp", t, gains_d[i], writes=[name])
        gains[name] = t
        return t

    dbg_out = {}
    mark0 = sb.off

    def phase_mix():
        w_in = sb.a([8, 3592], BF16)
        w_out = sb.a([8, D], BF16)
        wv = w_in_d.rearrange("(kc p) n -> p kc n", p=128)
        for kc in range(8):
            P.dma("pool", w_in[:, kc, 0:2048], wv[:, kc, 0:2048], writes=["w_in"])
            P.dma("pool", w_in[:, kc, 2048:3592], wv[:, kc, 2048:3592], writes=["w_in"])
        wv = w_out_d.rearrange("(kc p) n -> p kc n", p=128)
        for kc in range(8):
            P.dma("pool", w_out[:, kc, :], wv[:, kc, :], writes=["w_out"])
        gmix = load_gain(0, "gmix")
        ghn = load_gain(5, "ghn")
        dmatT = load_const("dmatT")
        gqT = load_const("gqT")
        gkc = load_const("gkc")
        dcc = load_const("dc")
        cosT = load_const("cos")
        sinT = load_const("sin")
        maskneg = load_const("maskneg", BF16)
        eh = load_const("eh")
        ident4 = load_const("ident4")
        convw = sb.a([8, 5], F32)
        P.dma("sp", convw, convw_d, writes=["convw"])
        gateb = sb.a([2], F32, parts=4)
        P.dma("sp", gateb, gateb_d, writes=["gateb"])
        ones4 = sb.a([128], F32, parts=4)
        P.op("pool", lambda e: e.memset(ones4, 1.0), writes=["ones4"])
        epsc = sb.a([1], F32)
        P.op("pool", lambda e: e.memset(epsc, EPS), writes=["epsc"])

        SLOTKA = {"xa", "ss", "rstd", "hb", "hT", "qk_f", "rt0", "rt1", "qkr", "qT", "kT", "qdT", "kdec", "rv", "sc_bf", "ro",
                  "cenR", "sqR", "st4R", "nm4R", "rs4R", "cenM", "sqM", "st4M", "nm4M", "rs4M", "sg", "mix", "cb", "acc", "tmpc",
                  "mqk_tok", "qkT", "mv", "so", "g_ig", "g_fp", "g_t1", "g_t2", "g_b", "g_u", "g_cu", "g_M", "g_nM", "g_in",
                  "g_fp", "g_ig", "g_t1", "s4", "dg", "cols", "sbcs", "DT", "Pm", "qTs", "kw", "hm", "dn4", "tmpC"}
        ST = [(sb.a([2, 128], F32), sb.a([2, 128], BF16), sb.a([4, 129], F32), sb.a([4, 129], BF16), sb.a([1], F32, parts=4))
              for _ in range(2)]
        CBS = [None, None]

        def make_slot(slot):
            xt = sb.a([D], F32)
            ss = sb.a([1], F32)
            rstd = sb.a([1], F32)
            hb = sb.a([D], BF16)
            hT = sb.a([8, 128], BF16)
            qk_f = sb.a([8, 2, 32], BF16)
            rt = [sb.a([8, 32], BF16) for _ in range(2)]
            qkr = sb.a([8, 2, 32], BF16)
            qT = sb.a([4, 128], BF16)
            kT = sb.a([2, 128], BF16)
            qdT = sb.a([4, 128], BF16)
            kdec = sb.a([4, 64], BF16)
            rv = sb.a([512], BF16)
            sc_bf = sb.a([4, 128], BF16)
            ro = sb.a([4, 128], F32)
            cen = sb.a([4, 128], F32)
            sq = sb.a([4, 128], BF16)
            st4 = sb.a([4], F32)
            nm4 = sb.a([4], F32)
            rs4 = sb.a([4], F32)
            sg = sb.a([512], BF16)
            mix = sb.a([D], BF16)
            cb = sb.a([8, 131], BF16)
            CBS[slot] = cb
            acc = sb.a([8, 128], F32)
            tmpc = sb.a([8, 128], BF16)
            mqk_tok = sb.a([D], BF16)
            junk = mqk_tok
            qkT = sb.a([8, 128], BF16)
            mv = sb.a([4, 129], BF16)
            so = sb.a([512], BF16)
            g_ig = sb.a([128], F32, parts=4)
            g_fp = sb.a([128], F32, parts=4)
            g_t1 = sb.a([128], F32, parts=4)
            g_t2 = sb.a([128], F32, parts=4)
            g_b = sb.a([128], F32, parts=4)
            g_u = sb.a([128], F32, parts=4)
            g_cu = sb.a([128], F32, parts=4)
            g_M = sb.a([128], F32, parts=4)
            g_nM = sb.a([128], F32, parts=4)
            g_in = sb.a([128], F32, parts=4)
            g_em = g_fp
            g_wa = g_ig
            g_mb = g_t1
            s4 = sb.a([4], F32, parts=4)
            dg = sb.a([2, 4], F32, parts=4)
            cols = sb.a([3, 4], F32)
            sbcs = sb.a([2, 4], F32)
            DT = sb.a([4, 128], BF16)
            Pm = sb.a([4, 128], BF16)
            qTs = sb.a([4, 128], BF16)
            kw = sb.a([4, 128], BF16)
            hm = sb.a([4, 128], F32)
            dn4 = sb.a([4], F32)
            tmpC = sb.a([4, 129], BF16)
            y1 = xt

            P.keymap = (SLOTKA, "_%d" % slot)
            P.op("pool", lambda e: e.memset(mv, 1.0), writes=["mv"])
            P.op("pool", lambda e: e.memset(qT, 0.0), writes=["qT"])
            P.op("pool", lambda e: e.memset(qdT, 0.0), writes=["qdT"])
            P.keymap = None
            DKS = 128.0 ** -0.5

            def rmsnorm_to_hb(src, srck, gain, gk):
                P.op("act", lambda e: e.activation(out=junk, in_=src, func=AF.Square, accum_out=ss),
                     reads=[srck], writes=["mqk_tok", "ss"])
                P.op("act", lambda e: e.activation(out=rstd, in_=ss, func=AF.Sqrt, bias=epsc, scale=1.0 / D),
                     reads=["ss", "epsc"], writes=["rstd"])
                P.op("dve", lambda e: e.reciprocal(out=rstd, in_=rstd), reads=["rstd"], writes=["rstd"])
                P.op("dve", lambda e: e.scalar_tensor_tensor(out=hb, in0=src, scalar=rstd, in1=gain,
                                                               op0=ALU.mult, op1=ALU.mult),
                     reads=[srck, "rstd", gk], writes=["hb"])

            def transpose8(src, srck, dst, dstk):
                ptile, ptk = nextpt()
                for j in range(8):
                    P.op("pe", lambda e, j=j: e.transpose(out=ptile[:, j, :], in_=src[:, j * 128:(j + 1) * 128],
                                                          identity=identb),
                         reads=[srck, "identb"], writes=[ptk], chain=True)
                P.op("act", lambda e: e.copy(out=dst, in_=ptile), reads=[ptk], writes=[dstk])

            hn_tmp = {"R": (st4, nm4, rs4, cen, sq),
                      "M": (sb.a([4], F32), sb.a([4], F32), sb.a([4], F32), sb.a([4, 128], F32), sb.a([4, 128], BF16))}

            def head_norm(src, srck, gain_sl, gate, gatek, dst_sl, br="R"):
                st4, nm4, rs4, cen, sq = hn_tmp[br]
                return head_norm_(src, srck, gain_sl, gate, gatek, dst_sl, st4, nm4, rs4, cen, sq, br)

            def head_norm_(src, srck, gain_sl, gate, gatek, dst_sl, st4, nm4, rs4, cen, sq, br):
                head_norm__(src, srck, gain_sl, gate, gatek, dst_sl, st4, nm4, rs4, cen, sq, br)

            def head_norm__(src, srck, gain_sl, gate, gatek, dst_sl, st4, nm4, rs4, cen, sq, br):
                P.op("dve", lambda e: e.tensor_reduce(out=st4, in_=src, axis=AX.X, op=ALU.add),
                     reads=[srck], writes=["st4" + br])
                P.op("dve", lambda e: e.tensor_scalar(out=nm4, in0=st4, scalar1=-1.0 / 128, scalar2=None, op0=ALU.mult),
                     reads=["st4" + br], writes=["nm4" + br])
                P.op("dve", lambda e: e.tensor_tensor(out=cen, in0=src, in1=bc(nm4.unsqueeze(2), [128, 4, 128]),
                                                      op=ALU.add), reads=[srck, "nm4" + br], writes=["cen" + br])
                P.op("pool", lambda e: e.tensor_tensor(out=sq, in0=cen, in1=cen, op=ALU.mult),
                     reads=["cen" + br], writes=["sq" + br])
                P.op("dve", lambda e: e.tensor_reduce(out=st4, in_=sq, axis=AX.X, op=ALU.add),
                     reads=["sq" + br], writes=["st4" + br])
                P.op("act", lambda e: e.activation(out=rs4, in_=st4, func=AF.Sqrt, bias=epsc, scale=1.0 / 128),
                     reads=["st4" + br, "epsc"], writes=["rs4" + br])
                P.op("dve", lambda e: e.reciprocal(out=rs4, in_=rs4), reads=["rs4" + br], writes=["rs4" + br])
                P.op("dve", lambda e: e.tensor_tensor(out=cen, in0=cen, in1=bc(rs4.unsqueeze(2), [128, 4, 128]),
                                                      op=ALU.mult), reads=["cen" + br, "rs4" + br], writes=["cen" + br])
                P.op("pool", lambda e: e.tensor_tensor(out=cen, in0=cen, in1=gain_sl, op=ALU.mult),
                     reads=["cen" + br, "ghn"], writes=["cen" + br])
                P.op("dve", lambda e: e.tensor_tensor(out=dst_sl, in0=cen, in1=gate, op=ALU.mult),
                     reads=["cen" + br, gatek], writes=["mix"])

            def mix_tile(b, c):
                if True:
                    it = b * NCH + c
                    Rf, Rbf, Cf, Cbf, mprev = ST[b % 2]
                    sk = lambda n: n + str(b % 2)
                    xk = "xa"
                    PA, PB = (2 * slot,), (2 * slot + 1,)
                    if c == 0:
                        P.op("pool", lambda e: e.memset(Rf, 0.0), writes=[sk("Rf")])
                        P.op("pool", lambda e: e.memset(Rbf, 0.0), writes=[sk("Rbf")])
                        P.op("pool", lambda e: e.memset(Cf, 0.0), writes=[sk("Cf")])
                        P.op("pool", lambda e: e.memset(Cbf, 0.0), writes=[sk("Cbf")])
                        P.op("pool", lambda e: e.memset(mprev, 0.0), writes=[sk("mprev")])
                        P.op("pool", lambda e: e.memset(cb[:, :, 0:3], 0.0), writes=["cbh%d" % slot])
                    P.dma("sp", xt, x_d[b, c * 128:(c + 1) * 128, :], writes=[xk])
                    ppset[0] = PA
                    rmsnorm_to_hb(xt, xk, gmix, "gmix")
                    transpose8(hb, "hb", hT, "hT")

                    def proj_tok(lo):
                        pst, psk = nextpp(full=False)
                        for kc in range(8):
                            P.op("pe", lambda e, kc=kc: e.matmul(pst[:, 0:512], lhsT=hT[:, kc, :],
                                                                 rhs=w_in[:, kc, lo:lo + 512],
                                                                 start=(kc == 0), stop=(kc == 7)),
                                 reads=["hT", "w_in"], writes=[psk], chain=True)
                        return pst, psk

                    G.pre = P.stop()
                    P.record()
                    ppset[0] = PA
                    pqk, pqkk = proj_tok(0)
                    P.op("act", lambda e: e.copy(out=qk_f.rearrange("p a b c -> p (a b c)"), in_=pqk[:, 0:512]),
                         reads=[pqkk], writes=["qk_f"])
                    cs = bc(cosT[:, c, :].unsqueeze(1), [128, 8, 32])
                    sn = bc(sinT[:, c, :].unsqueeze(1), [128, 8, 32])
                    x1v, x2v = qk_f[:, :, 0, :], qk_f[:, :, 1, :]
                    P.op("dve", lambda e: e.tensor_tensor(out=rt[0], in0=x1v, in1=cs, op=ALU.mult),
                         reads=["qk_f", "cos"], writes=["rt0"])
                    P.op("pool", lambda e: e.tensor_tensor(out=rt[1], in0=x2v, in1=sn, op=ALU.mult),
                         reads=["qk_f", "sin"], writes=["rt1"])
                    P.op("dve", lambda e: e.tensor_tensor(out=qkr[:, :, 0, :], in0=rt[0], in1=rt[1], op=ALU.subtract),
                         reads=["rt0", "rt1"], writes=["qkr"])
                    P.op("pool", lambda e: e.tensor_tensor(out=rt[0], in0=x1v, in1=sn, op=ALU.mult),
                         reads=["qk_f", "sin"], writes=["rt0"])
                    P.op("dve", lambda e: e.tensor_tensor(out=rt[1], in0=x2v, in1=cs, op=ALU.mult),
                         reads=["qk_f", "cos"], writes=["rt1"])
                    P.op("dve", lambda e: e.tensor_tensor(out=qkr[:, :, 1, :], in0=rt[0], in1=rt[1], op=ALU.add),
                         reads=["rt0", "rt1"], writes=["qkr"])
                    qkr2 = qkr.rearrange("p a b c -> p (a b c)")
                    ptq, ptqk = nextpt()
                    for j in range(4):
                        P.op("pe", lambda e, j=j: e.transpose(out=ptq[:, j, :], in_=qkr2[:, j * 128:(j + 1) * 128],
                                                              identity=identb),
                             reads=["qkr", "identb"], writes=[ptqk], chain=True)
                    P.op("act", lambda e: e.copy(out=qT[0:64, 0:4:2, :], in_=ptq[0:64, 0:2, :]), reads=[ptqk], writes=["qT"])
                    P.op("act", lambda e: e.copy(out=qT[64:128, 1:4:2, :], in_=ptq[64:128, 0:2, :]), reads=[ptqk], writes=["qT"])
                    import os as _os
                    _v = int(_os.environ.get("DBGV", "0"))
                    if _v != 1:
                        P.op("act", lambda e: e.mul(out=kT, in_=ptq[:, 2:4, :], mul=0.125), reads=[ptqk], writes=["kT"])
                    P.op("dve", lambda e: e.tensor_tensor(out=qdT[0:64, 0:4:2, :], in0=qT[0:64, 0:4:2, :], in1=gqT[0:64, :, :],
                                                          op=ALU.mult), reads=["qT", "gqT"], writes=["qdT"])
                    P.op("dve", lambda e: e.tensor_tensor(out=qdT[64:128, 1:4:2, :], in0=qT[64:128, 1:4:2, :],
                                                          in1=gqT[64:128, :, :], op=ALU.mult), reads=["qT", "gqT"], writes=["qdT"])
                    P.op("dve", lambda e: e.tensor_tensor(out=kdec, in0=qkr2[:, 256:512].rearrange("p (h d) -> p h d", h=4),
                                                           in1=bc(gkc.unsqueeze(2), [128, 4, 64]), op=ALU.mult),
                         reads=["qkr", "gkc"], writes=["kdec"])
                    prv, prvk = proj_tok(512)
                    P.op("act", lambda e: e.copy(out=rv, in_=prv[:, 0:512]), reads=[prvk], writes=["rv"])
                    prg, prgk = proj_tok(1024)
                    P.op("act", lambda e: e.activation(out=sg, in_=prg[:, 0:512], func=AF.Silu),
                         reads=[prgk], writes=["sg"])
                    psc, psck = nextpp(full=False)
                    for h in range(4):
                        p_, off = h // 2, (h % 2) * 64
                        P.op("pe", lambda e, h=h, p_=p_, off=off: e.matmul(
                            psc[:, h * 128:(h + 1) * 128], lhsT=kT[:, p_, :], rhs=qT[:, h, :],
                            start=True, stop=True), reads=["kT", "qT"], writes=[psck], chain=True)
                    P.op("dve", lambda e: e.tensor_tensor(out=sc_bf.rearrange("p a b -> p (a b)"), in0=psc[:, 0:512],
                                                          in1=dmatT.rearrange("p a b -> p (a b)"), op=ALU.mult),
                         reads=[psck, "dmatT"], writes=["sc_bf"])
                    pro, prok = nextpp(full=False)
                    for h in range(4):
                        p_, off = h // 2, (h % 2) * 64
                        P.op("pe", lambda e, h=h: e.matmul(pro[:, h * 128:(h + 1) * 128], lhsT=sc_bf[:, h, :],
                                                           rhs=rv[:, h * 128:(h + 1) * 128], start=True, stop=False),
                             reads=["sc_bf", "rv"], writes=[prok], chain=True)
                        P.op("pe", lambda e, h=h, p_=p_, off=off: e.matmul(
                            pro[:, h * 128:(h + 1) * 128], lhsT=qdT[:, h, :], rhs=Rbf[:, p_, :],
                            start=False, stop=True), reads=["qdT", sk("Rbf")], writes=[prok], chain=True)
                    P.op("act", lambda e: e.copy(out=ro.rearrange("p a b -> p (a b)"), in_=pro[:, 0:512]),
                         reads=[prok], writes=["ro"])
                    pkv, pkvk = nextpp(full=False)
                    for h in range(4):
                        p_ = h // 2
                        P.op("pe", lambda e, h=h, p_=p_: e.matmul(
                            pkv[:, h * 128:(h + 1) * 128], lhsT=kdec[:, 2 * p_:2 * p_ + 2, :].rearrange("p a b -> p (a b)"),
                            rhs=rv[:, h * 128:(h + 1) * 128], start=True, stop=True),
                            reads=["kdec", "rv"], writes=[pkvk], chain=True)
                    for h in range(4):
                        p_, off = h // 2, (h % 2) * 64
                        P.op("dve", lambda e, h=h, p_=p_, off=off: e.scalar_tensor_tensor(
                            out=Rf[off:off + 64, p_, :], in0=Rf[off:off + 64, p_, :], scalar=dcc[off:off + 64, p_:p_ + 1],
                            in1=pkv[off:off + 64, h * 128:(h + 1) * 128], op0=ALU.mult, op1=ALU.add),
                            reads=[sk("Rf"), "dc", pkvk], writes=[sk("Rf")])
                    P.op("pool", lambda e: e.tensor_copy(out=Rbf, in_=Rf), reads=[sk("Rf")], writes=[sk("Rbf")])
                    head_norm(ro, "ro", ghn[:, 0:512].rearrange("p (a b) -> p a b", a=4),
                              sg.rearrange("p (a b) -> p a b", a=4), "sg",
                              mix[:, 0:512].rearrange("p (a b) -> p a b", a=4))

                    listR = P.stop()
                    P.record()
                    ppset[0] = PB
                    pg, pgk = nextpp(full=False)
                    for gi in range(2):
                        for kc in range(8):
                            P.op("pe", lambda e, gi=gi, kc=kc: e.matmul(
                                pg[0:4, gi * 128:(gi + 1) * 128], lhsT=w_in[:, kc, 3584 + 4 * gi:3588 + 4 * gi],
                                rhs=hT[:, kc, :], start=(kc == 0), stop=(kc == 7)),
                                reads=["hT", "w_in"], writes=[pgk], chain=True)
                    P.op("act", lambda e: e.activation(out=g_ig, in_=pg[0:4, 0:128], func=AF.Identity, bias=gateb[:, 0:1]),
                         reads=[pgk, "gateb"], writes=["g_ig"])
                    P.op("act", lambda e: e.activation(out=g_fp, in_=pg[0:4, 128:256], func=AF.Identity, bias=gateb[:, 1:2]),
                         reads=[pgk, "gateb"], writes=["g_fp"])
                    P.op("act", lambda e: e.activation(out=g_t1, in_=g_fp, func=AF.Abs),
                         reads=["g_fp"], writes=["g_t1"])
                    P.op("act", lambda e: e.activation(out=g_t1, in_=g_t1, func=AF.Exp, scale=-1.0),
                         reads=["g_t1"], writes=["g_t1"])
                    P.op("act", lambda e: e.activation(out=g_t1, in_=g_t1, func=AF.Ln, bias=1.0),
                         reads=["g_t1"], writes=["g_t1"])
                    P.op("dve", lambda e: e.tensor_scalar(out=g_t2, in0=g_fp, scalar1=0.0, scalar2=None, op0=ALU.min),
                         reads=["g_fp"], writes=["g_t2"])
                    P.op("dve", lambda e: e.tensor_tensor(out=g_t2, in0=g_t2, in1=g_t1, op=ALU.subtract),
                         reads=["g_t2", "g_t1"], writes=["g_t2"])
                    P.op("dve", lambda e: e.tensor_tensor_scan(out=g_b, data0=ones4, data1=g_t2, initial=0.0,
                                                               op0=ALU.mult, op1=ALU.add),
                         reads=["ones4", "g_t2"], writes=["g_b"])
                    P.op("dve", lambda e: e.tensor_tensor(out=g_u, in0=g_ig, in1=g_b, op=ALU.subtract),
                         reads=["g_ig", "g_b"], writes=["g_u"])
                    P.op("dve", lambda e: e.tensor_tensor_scan(out=g_cu, data0=ones4, data1=g_u, initial=-1e30,
                                                               op0=ALU.mult, op1=ALU.max),
                         reads=["ones4", "g_u"], writes=["g_cu"])
                    P.op("dve", lambda e: e.tensor_scalar(out=g_M, in0=g_cu, scalar1=mprev, scalar2=None, op0=ALU.max),
                         reads=["g_cu", sk("mprev")], writes=["g_M"])
                    P.op("dve", lambda e: e.tensor_scalar(out=g_nM, in0=g_M, scalar1=-1.0, scalar2=None, op0=ALU.mult),
                         reads=["g_M"], writes=["g_nM"])
                    P.op("act", lambda e: e.activation(out=g_in, in_=g_M, func=AF.Exp, scale=-1.0, bias=mprev),
                         reads=["g_M", sk("mprev")], writes=["g_in"])
                    P.op("dve", lambda e: e.tensor_tensor(out=g_mb, in0=g_M, in1=g_b, op=ALU.add),
                         reads=["g_M", "g_b"], writes=["g_t1"])
                    P.op("act", lambda e: e.activation(out=g_em, in_=g_mb, func=AF.Exp, scale=-1.0),
                         reads=["g_t1"], writes=["g_fp"])
                    P.op("dve", lambda e: e.tensor_scalar(out=s4[:, 1:2], in0=g_cu[:, 127:128],
                                                          scalar1=-1.0, scalar2=None, op0=ALU.mult),
                         reads=["g_cu"], writes=["s4"])
                    P.op("dve", lambda e: e.tensor_scalar(out=s4[:, 2:3], in0=g_M[:, 127:128], scalar1=-1.0, scalar2=None,
                                                          op0=ALU.mult), reads=["g_M"], writes=["s4"])
                    P.op("act", lambda e: e.activation(out=g_wa, in_=g_u, func=AF.Exp, bias=s4[:, 1:2]),
                         reads=["g_u", "s4"], writes=["g_ig"])
                    P.op("act", lambda e: e.activation(out=s4[:, 3:4], in_=g_cu[:, 127:128], func=AF.Exp, bias=s4[:, 2:3]),
                         reads=["g_cu", "s4"], writes=["s4"])
                    P.op("dve", lambda e: e.tensor_scalar(out=dg[:, 0, :], in0=ident4, scalar1=g_in[:, 127:128], scalar2=None,
                                                          op0=ALU.mult), reads=["ident4", "g_in"], writes=["dg"])
                    P.op("dve", lambda e: e.tensor_scalar(out=dg[:, 1, :], in0=ident4, scalar1=s4[:, 3:4], scalar2=None,
                                                          op0=ALU.mult), reads=["ident4", "s4"], writes=["dg"])
                    P.op("dve", lambda e: e.tensor_copy(out=mprev, in_=g_mb[:, 127:128]),
                         reads=["g_t1", "g_in", "g_M"], writes=[sk("mprev")])
                    pc, pck = nextpp(full=False)
                    P.op("pe", lambda e: e.matmul(pc[:, 0:4], lhsT=g_wa, rhs=ident4, start=True, stop=True),
                         reads=["g_ig", "ident4"], writes=[pck], f32=True)
                    P.op("pe", lambda e: e.matmul(pc[:, 8:12], lhsT=g_em, rhs=ident4, start=True, stop=True),
                         reads=["g_fp", "ident4"], writes=[pck], f32=True)
                    P.op("pe", lambda e: e.matmul(pc[:, 16:20], lhsT=ones4, rhs=dg[:, 0, :], start=True, stop=True),
                         reads=["ones4", "dg"], writes=[pck], f32=True)
                    P.op("pe", lambda e: e.matmul(pc[:, 20:24], lhsT=ones4, rhs=dg[:, 1, :], start=True, stop=True),
                         reads=["ones4", "dg"], writes=[pck], f32=True)
                    P.op("dve", lambda e: e.tensor_scalar(out=cols[:, 0, :], in0=pc[:, 0:4], scalar1=DKS, scalar2=None,
                                                          op0=ALU.mult), reads=[pck], writes=["cols"])
                    P.op("dve", lambda e: e.tensor_copy(out=cols[:, 2, :], in_=pc[:, 8:12]), reads=[pck], writes=["cols"])
                    P.op("dve", lambda e: e.tensor_copy(out=sbcs.rearrange("p a b -> p (a b)"), in_=pc[:, 16:24]),
                         reads=[pck], writes=["sbcs"])

                    pm, pmk = nextpp()
                    for g in range(2):
                        for kc in range(8):
                            P.op("pe", lambda e, g=g, kc=kc: e.matmul(
                                pm[:, g * 512:(g + 1) * 512], lhsT=hT[:, kc, :],
                                rhs=w_in[:, kc, 1536 + g * 512:1536 + (g + 1) * 512], start=(kc == 0), stop=(kc == 7)),
                                reads=["hT", "w_in"], writes=[pmk], chain=True)
                    P.op("act", lambda e: e.copy(out=mqk_tok, in_=pm[:, :]), reads=[pmk], writes=["mqk_tok"])
                    ptm, ptmk = nextpt()
                    for j in range(8):
                        P.op("pe", lambda e, j=j: e.transpose(out=ptm[:, j, :], in_=mqk_tok[:, j * 128:(j + 1) * 128],
                                                              identity=identb),
                             reads=["mqk_tok", "identb"], writes=[ptmk], chain=True)
                    P.op("act", lambda e: e.copy(out=cb[:, :, 3:131], in_=ptm), reads=[ptmk], writes=["cb"])
                    if c + 1 < NCH:
                        P.op("pool", lambda e: e.tensor_copy(out=CBS[1 - slot][:, :, 0:3], in_=cb[:, :, 128:131]),
                             reads=["cb"], writes=["cbh%d" % (1 - slot)])
                    P.op("dve", lambda e: e.tensor_tensor(out=acc, in0=cb[:, :, 3:131],
                                                          in1=bc(convw[:, :, 3:4], [128, 8, 128]), op=ALU.mult),
                         reads=["cb", "cbh%d" % slot, "convw"], writes=["acc"])
                    for tap in range(3):
                        P.op("pool", lambda e, tap=tap: e.tensor_tensor(out=tmpc, in0=cb[:, :, tap:tap + 128],
                                                                        in1=bc(convw[:, :, tap:tap + 1], [128, 8, 128]),
                                                                        op=ALU.mult),
                             reads=["cb", "cbh%d" % slot, "convw"], writes=["tmpc"])
                        P.op("dve", lambda e: e.tensor_tensor(out=acc, in0=acc, in1=tmpc, op=ALU.add),
                             reads=["acc", "tmpc"], writes=["acc"])
                    for j in range(8):
                        P.op("act", lambda e, j=j: e.activation(out=qkT[:, j, :], in_=acc[:, j, :], func=AF.Silu,
                                                                bias=convw[:, j, 4:5]),
                             reads=["acc", "convw"], writes=["qkT"])
                    pmv, pmvk = proj_tok(2560)
                    P.op("act", lambda e: e.copy(out=mv[:, :, 0:128], in_=pmv[:, 0:512].rearrange("p (a b) -> p a b", a=4)),
                         reads=[pmvk], writes=["mv"])
                    pmo, pmok = proj_tok(3072)
                    P.op("act", lambda e: e.activation(out=so, in_=pmo[:, 0:512], func=AF.Sigmoid),
                         reads=[pmok], writes=["so"])
                    ptk_, ptkk = nextpt()
                    for h in range(4):
                        P.op("pe", lambda e, h=h: e.transpose(out=ptk_[:, h, :], in_=qkT[:, 4 + h, :], identity=identb),
                             reads=["qkT", "identb"], writes=[ptkk], chain=True)
                    P.op("act", lambda e: e.copy(out=kw, in_=ptk_[:, 0:4, :]), reads=[ptkk], writes=["kw"])
                    P.op("dve", lambda e: e.tensor_tensor(out=kw, in0=kw,
                                                          in1=bc(cols[:, 0, :].unsqueeze(2), [128, 4, 128]), op=ALU.mult),
                         reads=["kw", "cols"], writes=["kw"])
                    ps_, psk_ = nextpp()
                    for h in range(4):
                        P.op("pe", lambda e, h=h: e.matmul(ps_[:, h * 128:(h + 1) * 128], lhsT=qkT[:, 4 + h, :],
                                                           rhs=qkT[:, h, :], start=True, stop=True),
                             reads=["qkT"], writes=[psk_], chain=True)
                    for h in range(4):
                        P.op("pe", lambda e, h=h: e.matmul(ps_[:, 512 + h * 128:512 + (h + 1) * 128], lhsT=g_u,
                                                           rhs=eh[:, h, :], start=True, stop=False),
                             reads=["g_u", "eh"], writes=[psk_], f32=True)
                        P.op("pe", lambda e, h=h: e.matmul(ps_[:, 512 + h * 128:512 + (h + 1) * 128], lhsT=eh[:, h, :],
                                                           rhs=g_nM, start=False, stop=False),
                             reads=["g_nM", "eh"], writes=[psk_], f32=True)
                        P.op("pe", lambda e, h=h: e.matmul(ps_[:, 512 + h * 128:512 + (h + 1) * 128], lhsT=identb,
                                                           rhs=maskneg, start=False, stop=True),
                             reads=["identb", "maskneg"], writes=[psk_], chain=True)
                    P.op("act", lambda e: e.activation(out=DT.rearrange("p a b -> p (a b)"), in_=ps_[:, 512:1024], func=AF.Exp),
                         reads=[psk_], writes=["DT"])
                    P.op("dve", lambda e: e.scalar_tensor_tensor(out=Pm.rearrange("p a b -> p (a b)"), in0=ps_[:, 0:512],
                                                                 scalar=DKS, in1=DT.rearrange("p a b -> p (a b)"),
                                                                 op0=ALU.mult, op1=ALU.mult),
                         reads=[psk_, "DT"], writes=["Pm"])
                    pib, pibk = nextpp(full=False)
                    for h in range(4):
                        P.op("pe", lambda e, h=h: e.matmul(pib[:, h * 128:(h + 1) * 128], lhsT=eh[:, h, :], rhs=g_in,
                                                           start=True, stop=True), reads=["eh", "g_in"], writes=[pibk], f32=True)
                    P.op("dve", lambda e: e.tensor_tensor(out=qTs.rearrange("p a b -> p (a b)"),
                                                          in0=qkT[:, 0:4, :].rearrange("p a b -> p (a b)"),
                                                          in1=pib[:, 0:512], op=ALU.mult),
                         reads=["qkT", pibk], writes=["qTs"])
                    pn_, pnk = nextpp()
                    for h in range(4):
                        P.op("pe", lambda e, h=h: e.matmul(pn_[:, h * 256:h * 256 + 129], lhsT=Pm[:, h, :], rhs=mv[:, h, :],
                                                           start=True, stop=False), reads=["Pm", "mv"], writes=[pnk], chain=True)
                        P.op("pe", lambda e, h=h: e.matmul(pn_[:, h * 256:h * 256 + 129], lhsT=qTs[:, h, :], rhs=Cbf[:, h, :],
                                                           start=False, stop=True), reads=["qTs", sk("Cbf")], writes=[pnk], chain=True)
                    pn3 = pn_[:, :].rearrange("p (a b) -> p a b", a=4)
                    P.op("act", lambda e: e.activation(out=dn4, in_=pn3[:, :, 128], func=AF.Abs),
                         reads=[pnk], writes=["dn4"])
                    P.op("dve", lambda e: e.tensor_tensor(out=dn4, in0=dn4, in1=cols[:, 2, :], op=ALU.max),
                         reads=["dn4", "cols"], writes=["dn4"])
                    P.op("dve", lambda e: e.reciprocal(out=dn4, in_=dn4), reads=["dn4"], writes=["dn4"])
                    P.op("dve", lambda e: e.tensor_tensor(out=hm, in0=pn3[:, :, 0:128],
                                                          in1=bc(dn4.unsqueeze(2), [128, 4, 128]), op=ALU.mult),
                         reads=[pnk, "dn4"], writes=["hm"])
                    pkc, pkck = nextpp()
                    for h in range(4):
                        P.op("pe", lambda e, h=h: e.matmul(pkc[:, h * 256:h * 256 + 129], lhsT=kw[:, h, :], rhs=mv[:, h, :],
                                                           start=True, stop=True), reads=["kw", "mv"], writes=[pkck], chain=True)
                    pk3 = pkc[:, :].rearrange("p (a b) -> p a b", a=4)
                    P.op("pool", lambda e: e.tensor_tensor(out=Cf, in0=Cf, in1=bc(sbcs[:, 0, :].unsqueeze(2), [128, 4, 129]),
                                                           op=ALU.mult), reads=[sk("Cf"), "sbcs", sk("Cbf")], writes=[sk("Cf")])
                    P.op("dve", lambda e: e.tensor_tensor(out=tmpC, in0=pk3[:, :, 0:129],
                                                          in1=bc(sbcs[:, 1, :].unsqueeze(2), [128, 4, 129]), op=ALU.mult),
                         reads=[pkck, "sbcs"], writes=["tmpC"])
                    P.op("pool", lambda e: e.tensor_tensor(out=Cf, in0=Cf, in1=tmpC, op=ALU.add),
                         reads=[sk("Cf"), "tmpC"], writes=[sk("Cf")])
                    P.op("pool", lambda e: e.tensor_copy(out=Cbf, in_=Cf), reads=[sk("Cf")], writes=[sk("Cbf")])
                    head_norm(hm, "hm", ghn[:, 512:1024].rearrange("p (a b) -> p a b", a=4),
                              so.rearrange("p (a b) -> p a b", a=4), "so",
                              mix[:, 512:1024].rearrange("p (a b) -> p a b", a=4), br="M")
                    listM = P.stop()
                    ppset[0] = PA
                    G.RM = (listR, listM)
                    P.record()

                    transpose8(mix, "mix", hT, "hT")
                    py, pyk = nextpp()
                    for g in range(2):
                        for kc in range(8):
                            P.op("pe", lambda e, g=g, kc=kc: e.matmul(py[:, g * 512:(g + 1) * 512], lhsT=hT[:, kc, :],
                                                                      rhs=w_out[:, kc, g * 512:(g + 1) * 512],
                                                                      start=(kc == 0), stop=(kc == 7)),
                                 reads=["hT", "w_out"], writes=[pyk], chain=True)
                    for g in range(2):
                        P.op("dve", lambda e, g=g: e.tensor_tensor(out=y1[:, g * 512:(g + 1) * 512],
                                                                   in0=py[:, g * 512:(g + 1) * 512],
                                                                   in1=xt[:, g * 512:(g + 1) * 512], op=ALU.add),
                             reads=[pyk, xk], writes=[xk])
                    P.dma("sp", x1_d[it * 128:(it + 1) * 128, :], y1, reads=[xk], writes=["x1s%d" % it], chan="c:x1s%d" % slot)
            return mix_tile

        tile_fns = [make_slot(0), make_slot(1)]
        tilesA = []
        for b in range(NS):
            for c in range(NCH):
                it = b * NCH + c
                P.record()
                P.keymap = (SLOTKA, "_%d" % (it % 2))
                tile_fns[it % 2](b, c)
                P.keymap = None
                suf = P.stop()
                tilesA.append((G.pre, G.RM[0], G.RM[1], suf))
        import os as _os2
        _sa = _os2.environ.get('SCHEDA', '1')
        if _sa == '2':
            P.schedule([l for tl in tilesA for l in tl], win=10)
        elif _sa == '1':
            P.schedule([tl[0] + P.merge_list([tl[1], tl[2]]) + tl[3] for tl in tilesA], win=3)
        else:
            P.pipeline([tl[0] + P.merge_list([tl[1], tl[2]]) + tl[3] for tl in tilesA],
                       frac=float(_os2.environ.get('FRACA', '0.5')))
        ppset[0] = (0, 1, 2, 3)
        P.barrier()
        sb.off = mark0


    NB_ = NEXP * CAP

    class G:
        reg = None

    def breg(e):
        if G.reg is None:
            G.reg = nc.gpsimd.to_reg(NB_ - 1)
        return G.reg

    if "mix" in phases:
        phase_mix()

    def phase_xa():
        wq = sb.a([8, D], BF16)
        wo = sb.a([8, D], BF16)
        wkv = sb.a([8, 2 * D], BF16)
        for (wt, wd_, nm, ncol) in ((wq, wq_d, "wq", D), (wo, wo_d, "wo", D), (wkv, wkv_d, "wkv", 2 * D)):
            wv = wd_.rearrange("(kc p) n -> p kc n", p=128)
            for kc in range(8):
                for c0 in range(0, ncol, 1024):
                    P.dma("pool", wt[:, kc, c0:c0 + 1024], wv[:, kc, c0:c0 + 1024], writes=[nm])
        gxa = load_gain(1, "gxa")
        gmem = load_gain(2, "gmem")
        gmoe = load_gain(3, "gmoe")
        identf = sb.a([128], F32)
        P.dma("sp", identf, cd["identb"], writes=["identf"])
        triu = load_const("triu", BF16)
        ecap = load_const("ecap")
        onesb = sb.a([128], BF16)
        P.op("pool", lambda e: e.memset(onesb, 1.0), writes=["onesb"])
        epsc = sb.a([1], F32)
        P.op("pool", lambda e: e.memset(epsc, EPS), writes=["epsc"])
        wr = sb.a([8, 36], F32)
        P.dma("sp", wr, wr_d.rearrange("(kc p) n -> p kc n", p=128), writes=["wr"])
        rbb = sb.a([36], F32)
        P.dma("sp", rbb, rb_d, writes=["rb"])
        desti = sb.a([NT, 2], I32)
        wts = sb.a([NT, 2], F32)
        G.desti, G.wts = desti, wts
        G.mark_b = sb.off
        base = sb.a([NEXP], F32)
        P.op("pool", lambda e: e.memset(base, 0.0), writes=["base"])
        zt = sb.a([8 * D], BF16)
        P.op("pool", lambda e: e.memset(zt, 0.0), writes=["zt"])
        xsv = xs_d.rearrange("(n p r) d -> n p (r d)", p=128, r=8)
        for n_ in range(NB_ // 1024):
            P.dma("pool", xsv[n_], zt, reads=["zt"], writes=["xs_scr"], chan="c:xszero")
        SLOTK = {"xa", "junk", "ss", "rstd", "hb", "hT", "qxT", "mx4", "pe", "ssum", "pn", "pT", "oT", "x2t", "h2f", "h2b",
                 "h2T", "lg", "gmax", "goh", "ngmax", "gex", "gs", "pen", "lm", "m1", "oh0", "lm2", "m2", "oh1", "d12",
                 "cntb", "pos", "ovf", "tmp32", "destf"}

        def mkset(full):
            W = {}
            W["junk"] = sb.a([D], BF16)
            W["ss"] = sb.a([1], F32)
            W["rstd"] = sb.a([1], F32)
            W["hb"] = sb.a([D], BF16)
            W["xa"] = sb.a([D], F32)
            if not full:
                return W
            W["hT"] = sb.a([8, 128], BF16)
            W["qxT"] = sb.a([8, 128], BF16)
            W["mx4"] = sb.a([4], F32)
            W["pe"] = sb.a([4, 256], F32)
            W["ssum"] = sb.a([4], F32)
            W["pn"] = sb.a([4, 256], BF16)
            W["pT"] = sb.a([8, 128], BF16)
            W["oT"] = sb.a([8, 128], BF16)
            W["x2t"] = sb.a([D], F32)
            W["h2f"] = sb.a([D], F32)
            W["h2b"] = sb.a([D], BF16)
            W["h2T"] = sb.a([8, 128], F32)
            W["lg"] = sb.a([36], F32)
            W["r1"] = [sb.a([1], F32) for _ in range(8)]
            for nm_, n_ in (("goh", 4), ("gex", 4), ("pen", 4), ("lm2", 32), ("oh0", 32), ("oh1", 32), ("pos", 32),
                            ("ovf", 32), ("tmp32", 32), ("destf", 2)):
                W[nm_] = sb.a([n_], F32)
            W["lm"] = sb.a([4, 8], F32)
            W["cntb"] = sb.a([32], BF16)
            return W

        WS = [mkset(True), mkset(True), mkset(False)]
        memT = sb.a([8, 256], BF16)
        KTs = [sb.a([8, 256], BF16), sb.a([8, 256], BF16)]
        Vvs = [sb.a([2, D], BF16), sb.a([2, D], BF16)]

        def rmsnorm_b(W, src, srck, gain, gk, dst, dstk):
            junk, ss, rstd = W["junk"], W["ss"], W["rstd"]
            P.op("act", lambda e: e.activation(out=junk, in_=src, func=AF.Square, accum_out=ss),
                 reads=[srck], writes=["junk", "ss"])
            P.op("act", lambda e: e.activation(out=rstd, in_=ss, func=AF.Sqrt, bias=epsc, scale=1.0 / D),
                 reads=["ss", "epsc"], writes=["rstd"])
            P.op("dve", lambda e: e.reciprocal(out=rstd, in_=rstd), reads=["rstd"], writes=["rstd"])
            P.op("dve", lambda e: e.scalar_tensor_tensor(out=dst, in0=src, scalar=rstd, in1=gain,
                                                           op0=ALU.mult, op1=ALU.mult),
                 reads=[srck, "rstd", gk], writes=[dstk])

        def transpose8b(src, srck, dst, dstk):
            ptile, ptk = nextpt()
            for j in range(8):
                P.op("pe", lambda e, j=j: e.transpose(out=ptile[:, j, :], in_=src[:, j * 128:(j + 1) * 128],
                                                      identity=identb),
                     reads=[srck, "identb"], writes=[ptk], chain=True)
            P.op("act", lambda e: e.copy(out=dst, in_=ptile), reads=[ptk], writes=[dstk])

        def kv_seq(b):
            W = WS[2]
            KT, Vv = KTs[b % 2], Vvs[b % 2]
            ktk, vvk = "KT%d" % (b % 2), "Vv%d" % (b % 2)
            for mt in range(2):
                P.dma("sp", W["xa"], mem_d[b, mt * 128:(mt + 1) * 128, :], writes=["xa"])
                rmsnorm_b(W, W["xa"], "xa", gmem, "gmem", W["hb"], "hb")
                transpose8b(W["hb"], "hb", memT[:, :, mt * 128:(mt + 1) * 128], "memT")
            for half in range(2):
                pk, pkk = nextpp()
                for j4 in range(4):
                    jc = half * 4 + j4
                    for kc in range(8):
                        P.op("pe", lambda e, j4=j4, jc=jc, kc=kc, pk=pk: e.matmul(
                            pk[:, j4 * 256:(j4 + 1) * 256], lhsT=wkv[:, kc, jc * 128:(jc + 1) * 128],
                            rhs=memT[:, kc, :], start=(kc == 0), stop=(kc == 7)),
                            reads=["wkv", "memT"], writes=[pkk], chain=True)
                P.op("act", lambda e, half=half, pk=pk: e.copy(out=KT[:, half * 4:(half + 1) * 4, :].rearrange("p a b -> p (a b)"),
                                                               in_=pk[:, :]), reads=[pkk], writes=[ktk])
            for mt in range(2):
                pv, pvk = nextpp()
                for g in range(2):
                    for kc in range(8):
                        P.op("pe", lambda e, mt=mt, g=g, kc=kc, pv=pv: e.matmul(
                            pv[:, g * 512:(g + 1) * 512], lhsT=memT[:, kc, mt * 128:(mt + 1) * 128],
                            rhs=wkv[:, kc, D + g * 512:D + (g + 1) * 512], start=(kc == 0), stop=(kc == 7)),
                            reads=["wkv", "memT"], writes=[pvk], chain=True)
                P.op("act", lambda e, mt=mt, pv=pv: e.copy(out=Vv[:, mt, :], in_=pv[:, :]), reads=[pvk], writes=[vvk])

        def xa_tile(b, c):
            it = b * NCH + c
            W = WS[it % 2]
            KT, Vv = KTs[b % 2], Vvs[b % 2]
            ktk, vvk = "KT%d" % (b % 2), "Vv%d" % (b % 2)
            xt, hb, hT, qxT, mx4, pe_, ssum, pn, pT, oT = (W[k] for k in ("xa", "hb", "hT", "qxT", "mx4", "pe", "ssum", "pn", "pT", "oT"))
            x2t, h2f, h2b, h2T, lg = (W[k] for k in ("x2t", "h2f", "h2b", "h2T", "lg"))
            goh, gex, pen, lm, lm2, oh0, oh1, cntb, pos, ovf, tmp32, destf = (W[k] for k in (
                "goh", "gex", "pen", "lm", "lm2", "oh0", "oh1", "cntb", "pos", "ovf", "tmp32", "destf"))
            xk = "xa"
            P.dma("sp", xt, x1_d[it * 128:(it + 1) * 128, :], reads=["x1s%d" % it], writes=[xk])
            rmsnorm_b(W, xt, xk, gxa, "gxa", hb, "hb")
            transpose8b(hb, "hb", hT, "hT")
            pq, pqk_ = nextpp()
            for g in range(2):
                for kc in range(8):
                    P.op("pe", lambda e, g=g, kc=kc: e.matmul(pq[:, g * 512:(g + 1) * 512], lhsT=hT[:, kc, :],
                                                              rhs=wq[:, kc, g * 512:(g + 1) * 512],
                                                              start=(kc == 0), stop=(kc == 7)),
                         reads=["wq", "hT"], writes=[pqk_], chain=True)
            q_tok = W["junk"]
            P.op("act", lambda e: e.mul(out=q_tok, in_=pq[:, :], mul=1.0 / 16), reads=[pqk_], writes=["junk"])
            ptq_, ptqk_ = nextpt()
            for j in range(8):
                P.op("pe", lambda e, j=j: e.transpose(out=ptq_[:, j, :], in_=q_tok[:, j * 128:(j + 1) * 128], identity=identb),
                     reads=["junk", "identb"], writes=[ptqk_], chain=True)
            P.op("act", lambda e: e.copy(out=qxT, in_=ptq_), reads=[ptqk_], writes=["qxT"])
            pl, plk = nextpp()
            for hh in range(4):
                for i in range(2):
                    P.op("pe", lambda e, hh=hh, i=i: e.matmul(pl[:, hh * 256:(hh + 1) * 256], lhsT=qxT[:, 2 * hh + i, :],
                                                              rhs=KT[:, 2 * hh + i, :], start=(i == 0), stop=(i == 1)),
                         reads=["qxT", ktk], writes=[plk], chain=True)
            pl3 = pl[:, :].rearrange("p (a b) -> p a b", a=4)
            P.op("dve", lambda e: e.tensor_reduce(out=mx4, in_=pl3, axis=AX.X, op=ALU.max),
                 reads=[plk], writes=["mx4"])
            P.op("dve", lambda e: e.tensor_scalar(out=mx4, in0=mx4, scalar1=-1.0, scalar2=None, op0=ALU.mult),
                 reads=["mx4"], writes=["mx4"])
            for hh in range(4):
                P.op("act", lambda e, hh=hh: e.activation(out=pe_[:, hh, :], in_=pl3[:, hh, :], func=AF.Exp,
                                                          bias=mx4[:, hh:hh + 1], accum_out=ssum[:, hh:hh + 1]),
                     reads=[plk, "mx4"], writes=["pe", "ssum"])
            P.op("dve", lambda e: e.reciprocal(out=ssum, in_=ssum), reads=["ssum"], writes=["ssum"])
            P.op("dve", lambda e: e.tensor_tensor(out=pn, in0=pe_, in1=bc(ssum.unsqueeze(2), [128, 4, 256]), op=ALU.mult),
                 reads=["pe", "ssum"], writes=["pn"])
            pn2 = pn.rearrange("p a b -> p (a b)")
            ptp, ptpk = nextpt()
            for j in range(8):
                P.op("pe", lambda e, j=j: e.transpose(out=ptp[:, j, :], in_=pn2[:, j * 128:(j + 1) * 128], identity=identb),
                     reads=["pn", "identb"], writes=[ptpk], chain=True)
            P.op("act", lambda e: e.copy(out=pT, in_=ptp), reads=[ptpk], writes=["pT"])
            po, pok = nextpp()
            for hh in range(4):
                for dcc_ in range(2):
                    j = hh * 2 + dcc_
                    for mc in range(2):
                        P.op("pe", lambda e, hh=hh, dcc_=dcc_, j=j, mc=mc: e.matmul(
                            po[:, j * 128:(j + 1) * 128], lhsT=Vv[:, mc, hh * 256 + dcc_ * 128:hh * 256 + (dcc_ + 1) * 128],
                            rhs=pT[:, hh * 2 + mc, :], start=(mc == 0), stop=(mc == 1)),
                            reads=[vvk, "pT"], writes=[pok], chain=True)
            P.op("act", lambda e: e.copy(out=oT.rearrange("p a b -> p (a b)"), in_=po[:, :]), reads=[pok], writes=["oT"])
            py, pyk = nextpp()
            for g in range(2):
                for kc in range(8):
                    P.op("pe", lambda e, g=g, kc=kc: e.matmul(py[:, g * 512:(g + 1) * 512], lhsT=oT[:, kc, :],
                                                              rhs=wo[:, kc, g * 512:(g + 1) * 512],
                                                              start=(kc == 0), stop=(kc == 7)),
                         reads=["oT", "wo"], writes=[pyk], chain=True)
            for g in range(2):
                P.op("dve", lambda e, g=g: e.tensor_tensor(out=x2t[:, g * 512:(g + 1) * 512], in0=py[:, g * 512:(g + 1) * 512],
                                                           in1=xt[:, g * 512:(g + 1) * 512], op=ALU.add),
                     reads=[pyk, xk], writes=["x2t"])
            P.dma("sp", x2_d[it * 128:(it + 1) * 128, :], x2t, reads=["x2t"], writes=["x2s%d" % it], chan="c:x2t")
            if "moe" not in phases:
                return
            rmsnorm_b(W, x2t, "x2t", gmoe, "gmoe", h2f, "h2f")
            P.op("pool", lambda e: e.tensor_copy(out=h2b, in_=h2f), reads=["h2f"], writes=["h2b"])
            ph, phk = nextpp()
            for j in range(8):
                P.op("pe", lambda e, j=j: e.transpose(out=ph[:, j * 128:(j + 1) * 128], in_=h2f[:, j * 128:(j + 1) * 128],
                                                      identity=identf), reads=["h2f", "identf"], writes=[phk], f32=True)
            P.op("act", lambda e: e.copy(out=h2T.rearrange("p a b -> p (a b)"), in_=ph[:, :]), reads=[phk], writes=["h2T"])
            pr, prk = nextpp()
            for kc in range(8):
                P.op("pe", lambda e, kc=kc: e.matmul(pr[:, 0:36], lhsT=h2T[:, kc, :], rhs=wr[:, kc, :],
                                                     start=(kc == 0), stop=(kc == 7)), reads=["h2T", "wr"], writes=[prk], f32=True)
            P.op("dve", lambda e: e.tensor_tensor(out=lg, in0=pr[:, 0:36], in1=rbb, op=ALU.add),
                 reads=[prk, "rb"], writes=["lg"])
            gmax, ngmax, gs, m1, m2, d12, w0t, _ = W["r1"]
            V_ = lambda fn, r, w: P.op("dve", fn, reads=r, writes=w)
            V_(lambda e: e.tensor_reduce(out=gmax, in_=lg[:, 0:4], axis=AX.X, op=ALU.max), ["lg"], ["gmax"])
            V_(lambda e: e.tensor_scalar(out=goh, in0=lg[:, 0:4], scalar1=gmax, scalar2=None, op0=ALU.is_equal),
               ["lg", "gmax"], ["goh"])
            V_(lambda e: e.tensor_scalar(out=ngmax, in0=gmax, scalar1=-1.0, scalar2=None, op0=ALU.mult), ["gmax"], ["ngmax"])
            P.op("act", lambda e: e.activation(out=gex, in_=lg[:, 0:4], func=AF.Exp, bias=ngmax, accum_out=gs),
                 reads=["lg", "ngmax"], writes=["gex", "gs"])
            V_(lambda e: e.reciprocal(out=gs, in_=gs), ["gs"], ["gs"])
            V_(lambda e: e.tensor_scalar(out=pen, in0=goh, scalar1=-1.0, scalar2=1e9, op0=ALU.add, op1=ALU.mult),
               ["goh"], ["pen"])
            V_(lambda e: e.tensor_tensor(out=lm, in0=lg[:, 4:36].rearrange("p (a b) -> p a b", a=4),
                                         in1=bc(pen.unsqueeze(2), [128, 4, 8]), op=ALU.add), ["lg", "pen"], ["lm"])
            lmf = lm.rearrange("p a b -> p (a b)")
            V_(lambda e: e.tensor_reduce(out=m1, in_=lmf, axis=AX.X, op=ALU.max), ["lm"], ["m1"])
            V_(lambda e: e.tensor_scalar(out=oh0, in0=lmf, scalar1=m1, scalar2=None, op0=ALU.is_equal), ["lm", "m1"], ["oh0"])
            V_(lambda e: e.scalar_tensor_tensor(out=lm2, in0=oh0, scalar=-2e9, in1=lmf, op0=ALU.mult, op1=ALU.add),
               ["oh0", "lm"], ["lm2"])
            V_(lambda e: e.tensor_reduce(out=m2, in_=lm2, axis=AX.X, op=ALU.max), ["lm2"], ["m2"])
            V_(lambda e: e.tensor_scalar(out=oh1, in0=lm2, scalar1=m2, scalar2=None, op0=ALU.is_equal), ["lm2", "m2"], ["oh1"])
            V_(lambda e: e.tensor_tensor(out=d12, in0=m2, in1=m1, op=ALU.subtract), ["m1", "m2"], ["d12"])
            P.op("act", lambda e: e.activation(out=d12, in_=d12, func=AF.Exp), reads=["d12"], writes=["d12"])
            V_(lambda e: e.tensor_scalar(out=d12, in0=d12, scalar1=1.0, scalar2=None, op0=ALU.add), ["d12"], ["d12"])
            V_(lambda e: e.reciprocal(out=d12, in_=d12), ["d12"], ["d12"])
            V_(lambda e: e.tensor_tensor(out=wts[:, it, 0:1], in0=d12, in1=gs, op=ALU.mult), ["d12", "gs"], ["wts%d" % it])
            V_(lambda e: e.tensor_tensor(out=wts[:, it, 1:2], in0=gs, in1=wts[:, it, 0:1], op=ALU.subtract),
               ["gs", "wts%d" % it], ["wts%d" % it])
            V_(lambda e: e.tensor_tensor(out=cntb, in0=oh0, in1=oh1, op=ALU.add), ["oh0", "oh1"], ["cntb"])
            pp_, ppk = nextpp()
            P.op("pe", lambda e: e.matmul(pp_[:, 0:32], lhsT=triu, rhs=cntb, start=True, stop=True),
                 reads=["triu", "cntb"], writes=[ppk], chain=True)
            P.op("pe", lambda e: e.matmul(pp_[:, 32:64], lhsT=onesb, rhs=cntb, start=True, stop=True),
                 reads=["onesb", "cntb"], writes=[ppk], chain=True)
            V_(lambda e: e.tensor_tensor(out=pos, in0=pp_[:, 0:32], in1=base, op=ALU.add), [ppk, "base"], ["pos"])
            V_(lambda e: e.tensor_tensor(out=base, in0=pp_[:, 32:64], in1=base, op=ALU.add), [ppk, "base"], ["base"])
            V_(lambda e: e.tensor_scalar(out=ovf, in0=pos, scalar1=float(CAP), scalar2=1e6, op0=ALU.is_ge, op1=ALU.mult),
               ["pos"], ["ovf"])
            V_(lambda e: e.tensor_tensor(out=pos, in0=pos, in1=ecap, op=ALU.add), ["pos", "ecap"], ["pos"])
            V_(lambda e: e.tensor_tensor(out=pos, in0=pos, in1=ovf, op=ALU.add), ["pos", "ovf"], ["pos"])
            V_(lambda e: e.scalar_tensor_tensor(out=tmp32, in0=oh0, scalar=1.0, in1=pos, op0=ALU.mult, op1=ALU.mult,
                                                accum_out=destf[:, 0:1]), ["oh0", "pos"], ["tmp32", "destf"])
            V_(lambda e: e.scalar_tensor_tensor(out=tmp32, in0=oh1, scalar=1.0, in1=pos, op0=ALU.mult, op1=ALU.mult,
                                                accum_out=destf[:, 1:2]), ["oh1", "pos"], ["tmp32", "destf"])
            V_(lambda e: e.tensor_copy(out=desti[:, it, :], in_=destf), ["destf"], ["desti%d" % it])
            for k in range(2):
                P.dma_fn("pool", lambda e, k=k: nc.gpsimd.indirect_dma_start(
                    out=xs_d, out_offset=bass.IndirectOffsetOnAxis(ap=desti[:, it, k:k + 1], axis=0),
                    in_=h2b, in_offset=None, bounds_check=breg(e), oob_is_err=False),
                    reads=["h2b", "desti%d" % it, "xs_scr"], writes=["xsd%d_%d" % (it, k)], chan="c:xsd%d" % (it % 2))

        tiles = []
        for b in range(NS):
            for c in range(NCH):
                it = b * NCH + c
                P.record()
                if c == 0:
                    P.keymap = (SLOTK, "_2")
                    ppset[0] = (0, 1) if it % 2 == 0 else (2, 3)
                    kv_seq(b)
                P.keymap = (SLOTK, "_%d" % (it % 2))
                ppset[0] = (0, 1) if it % 2 == 0 else (2, 3)
                xa_tile(b, c)
                P.keymap = None
                tiles.append(P.stop())
        import os as _os3
        if _os3.environ.get('SCHED', '1') == '1':
            P.schedule(tiles)
        else:
            P.pipeline(tiles, frac=float(_os3.environ.get('FRACB', '0.5')))
        ppset[0] = (0, 1, 2, 3)
        P.barrier()
        sb.off = G.mark_b

    if "xa" in phases:
        phase_xa()

    def phase_moe():
        desti, wts = G.desti, G.wts
        GS = 256 if CAP % 256 == 0 else 128
        wbuf = [(sb.a([8, 512], BF16), sb.a([8, 512], BF16), sb.a([4, D], BF16)) for _ in range(2)]
        NTB = GS // 128
        xb = [[sb.a([D], BF16) for _ in range(NTB)] for _ in range(2)]
        xbT_ = [sb.a([8, GS], BF16) for _ in range(2)]
        sgl_ = [sb.a([4 * GS], F32) for _ in range(2)]
        hid_ = [sb.a([4, GS], BF16) for _ in range(2)]
        ysb = [sb.a([D], BF16), sb.a([D], BF16)]
        NG = CAP // GS

        def load_w(e_):
            wg_t, wu_t, wd_t = wbuf[e_ % 2]
            k = "wb%d" % (e_ % 2)
            P.dma("pool", wg_t, wg_d[e_].rearrange("(kc p) n -> p kc n", p=128), writes=[k])
            P.dma("pool", wu_t, wu_d[e_].rearrange("(kc p) n -> p kc n", p=128), writes=[k])
            P.dma("pool", wd_t, wd_d[e_].rearrange("(kc p) n -> p kc n", p=128), writes=[k])

        def load_x(gi):
            e_, grp = divmod(gi, NG)
            r0 = e_ * CAP + grp * GS
            for tb in range(NTB):
                P.dma("sp", xb[gi % 2][tb], xs_d[r0 + tb * 128:r0 + (tb + 1) * 128, :], reads=["xs_scr"],
                      writes=["xb%d_%d" % (gi % 2, tb)])

        def group(gi):
            e_, grp = divmod(gi, NG)
            wg_t, wu_t, wd_t = wbuf[e_ % 2]
            wk = "wb%d" % (e_ % 2)
            r0 = e_ * CAP + grp * GS
            xbT, sgl, hid = xbT_[gi % 2], sgl_[gi % 2], hid_[gi % 2]
            xbTk, sglk, hidk = "xbT%d" % (gi % 2), "sgl%d" % (gi % 2), "hid%d" % (gi % 2)
            for tb in range(NTB):
                xt_ = xb[gi % 2][tb]
                xk_ = "xb%d_%d" % (gi % 2, tb)
                ptx, ptxk = nextpt()
                for j in range(8):
                    P.op("pe", lambda e, j=j, xt_=xt_, ptx=ptx: e.transpose(out=ptx[:, j, :],
                                                                            in_=xt_[:, j * 128:(j + 1) * 128], identity=identb),
                         reads=[xk_, "identb"], writes=[ptxk], chain=True)
                P.op("act", lambda e, tb=tb, ptx=ptx: e.copy(out=xbT[:, :, tb * 128:(tb + 1) * 128], in_=ptx),
                     reads=[ptxk], writes=[xbTk])
            pa, pak = nextpp()
            pb_, pbk = nextpp()
            for (pdst, pdk, wt_) in ((pa, pak, wg_t), (pb_, pbk, wu_t)):
                for fc in range(4):
                    for kc in range(8):
                        P.op("pe", lambda e, pdst=pdst, wt_=wt_, fc=fc, kc=kc: e.matmul(
                            pdst[:, fc * GS:(fc + 1) * GS], lhsT=wt_[:, kc, fc * 128:(fc + 1) * 128], rhs=xbT[:, kc, :],
                            start=(kc == 0), stop=(kc == 7)), reads=[wk, xbTk], writes=[pdk], chain=True)
            P.op("act", lambda e, pa=pa: e.activation(out=sgl, in_=pa[:, 0:4 * GS], func=AF.Silu), reads=[pak], writes=[sglk])
            P.op("dve", lambda e, pb_=pb_: e.tensor_tensor(out=hid.rearrange("p a b -> p (a b)"), in0=pb_[:, 0:4 * GS], in1=sgl,
                                                           op=ALU.mult), reads=[pbk, sglk], writes=[hidk])
            for tb in range(NTB):
                pc_, pck_ = nextpp()
                for g in range(2):
                    for fc in range(4):
                        P.op("pe", lambda e, g=g, fc=fc, tb=tb, pc_=pc_: e.matmul(
                            pc_[:, g * 512:(g + 1) * 512], lhsT=hid[:, fc, tb * 128:(tb + 1) * 128],
                            rhs=wd_t[:, fc, g * 512:(g + 1) * 512], start=(fc == 0), stop=(fc == 3)),
                            reads=[hidk, wk], writes=[pck_], chain=True)
                yi = tb % 2
                if tb % 2 == 0:
                    P.op("act", lambda e, yi=yi, pc_=pc_: e.copy(out=ysb[yi], in_=pc_[:, :]), reads=[pck_], writes=["ysb%d" % yi])
                else:
                    P.op("dve", lambda e, yi=yi, pc_=pc_: e.tensor_copy(out=ysb[yi], in_=pc_[:, :]), reads=[pck_],
                         writes=["ysb%d" % yi])
                P.dma("pool", ys_d[r0 + tb * 128:r0 + (tb + 1) * 128, :], ysb[yi], reads=["ysb%d" % yi],
                      writes=["ys_scr"], chan="c:ysb%d" % yi)

        load_w(0)
        glists = []
        for gi in range(NEXP * NG):
            e_, grp = divmod(gi, NG)
            P.record()
            if grp == 0 and e_ + 1 < NEXP:
                load_w(e_ + 1)
            load_x(gi)
            ppset[0] = (0, 1) if gi % 2 == 0 else (2, 3)
            group(gi)
            glists.append(P.stop())
        ppset[0] = (0, 1, 2, 3)
        P.schedule(glists, win=3)
        P.barrier()
        sb.off = G.mark_b
        gfin = load_gain(4, "gfin")
        epsd = sb.a([1], F32)
        P.op("pool", lambda e: e.memset(epsd, EPS), writes=["epsd"])
        xd = [sb.a([D], F32), sb.a([D], F32)]
        yk = [[sb.a([D], BF16), sb.a([D], BF16)] for _ in range(2)]
        zt2 = [sb.a([D], F32), sb.a([D], F32)]
        junkd2 = [sb.a([D], BF16), sb.a([D], BF16)]
        ssd2 = [sb.a([1], F32), sb.a([1], F32)]
        rsd2 = [sb.a([1], F32), sb.a([1], F32)]
        ot = [sb.a([D], F32), sb.a([D], F32)]
        for par in range(2):
            for k in range(2):
                P.op("pool", lambda e, par=par, k=k: e.memset(yk[par][k], 0.0), writes=["yk%d%d" % (par, k)])

        def fin_load(it):
            par = it % 2
            P.dma("sp", xd[par], x2_d[it * 128:(it + 1) * 128, :], reads=["x2s%d" % it], writes=["xd%d" % par])
            for k in range(2):
                P.dma_fn("pool", lambda e, k=k: nc.gpsimd.indirect_dma_start(
                    out=yk[par][k], out_offset=None, in_=ys_d,
                    in_offset=bass.IndirectOffsetOnAxis(ap=desti[:, it, k:k + 1], axis=0),
                    bounds_check=breg(e), oob_is_err=False),
                    reads=["ys_scr"], writes=["yk%d%d" % (par, k)])

        def fin_tile(it):
            b, c = divmod(it, NCH)
            par = it % 2
            zt_, junkd, ssd, rsd = zt2[par], junkd2[par], ssd2[par], rsd2[par]
            P.keymap = ({"zt_", "junkd", "ssd", "rsd"}, str(par))
            try:
                fin_tile_(it, b, c, par, zt_, junkd, ssd, rsd)
            finally:
                P.keymap = None

        def fin_tile_(it, b, c, par, zt_, junkd, ssd, rsd):
            P.op("dve", lambda e: e.scalar_tensor_tensor(out=zt_, in0=yk[par][0], scalar=wts[:, it, 0:1], in1=xd[par],
                                                         op0=ALU.mult, op1=ALU.add),
                 reads=["yk%d0" % par, "xd%d" % par], writes=["zt_"])
            P.op("dve", lambda e: e.scalar_tensor_tensor(out=zt_, in0=yk[par][1], scalar=wts[:, it, 1:2], in1=zt_,
                                                          op0=ALU.mult, op1=ALU.add),
                 reads=["yk%d1" % par, "zt_"], writes=["zt_"])
            P.op("act", lambda e: e.activation(out=junkd, in_=zt_, func=AF.Square, accum_out=ssd),
                 reads=["zt_"], writes=["junkd", "ssd"])
            P.op("act", lambda e: e.activation(out=rsd, in_=ssd, func=AF.Sqrt, bias=epsd, scale=1.0 / D),
                 reads=["ssd", "epsd"], writes=["rsd"])
            P.op("dve", lambda e: e.reciprocal(out=rsd, in_=rsd), reads=["rsd"], writes=["rsd"])
            P.op("dve", lambda e: e.scalar_tensor_tensor(out=ot[par], in0=zt_, scalar=rsd, in1=gfin, op0=ALU.mult,
                                                         op1=ALU.mult), reads=["zt_", "rsd", "gfin"], writes=["ot%d" % par])
            P.dma("sp", out_d[b, c * 128:(c + 1) * 128, :], ot[par], reads=["ot%d" % par], writes=["out%d" % it],
                  chan="c:ot%d" % par)

        flists = []
        for it in range(NT):
            P.record()
            fin_load(it)
            fin_tile(it)
            flists.append(P.stop())
        P.schedule(flists, win=3)
        P.wait_for("sp", ["out%d" % it for it in range(NT)])

    if "moe" in phases:
        phase_moe()
    else:
        src_d, skey = (x2_d, "x2s%d") if "xa" in phases else (x1_d, "x1s%d")
        t = sb.a([D], F32)
        for it in range(NT):
            b, c = divmod(it, NCH)
            P.dma("sp", t, src_d[it * 128:(it + 1) * 128, :], reads=[skey % it], writes=["dbgt"])
            P.dma("sp", out_d[b, c * 128:(c + 1) * 128, :], t, reads=["dbgt"], writes=["out%d" % it], chan="c:out")
        P.wait_for("sp", ["out%d" % it for it in range(NT)])

    P.emit(st)
    st.close()
    return nc, P, sb


_CACHE = {}


def _host_layout(inp):
    f = lambda a: np.ascontiguousarray(np.asarray(a, dtype=np.float32))
    m = {}
    m["w_in"] = f(inp["w_in"][0])
    m["w_out"] = f(inp["w_out"][0])
    m["xa_wq"] = f(inp["xa_wq"][0])
    m["xa_wkv"] = f(inp["xa_wkv"][0])
    m["xa_wo"] = f(inp["xa_wo"][0])
    m["moe_w_gate"] = f(inp["moe_w_gate"][0])
    m["moe_w_up"] = f(inp["moe_w_up"][0])
    m["moe_w_down"] = f(inp["moe_w_down"][0])
    hn = np.concatenate([np.asarray(inp["ret_norm_w"][0]), np.asarray(inp["ml_norm_w"][0])])
    g = np.stack([np.asarray(inp["norm_mix_w"][0]), np.asarray(inp["norm_xa_w"][0]), np.asarray(inp["norm_mem_w"][0]),
                  np.asarray(inp["norm_moe_w"][0]), np.asarray(inp["norm_final_w"]), hn])
    m["gains"] = f(np.broadcast_to(g[:, None, :], (6, 128, D)))
    cw = np.concatenate([np.asarray(inp["ml_conv_w"][0]), np.asarray(inp["ml_conv_b"][0])[None]], 0)
    m["convw"] = f(cw.reshape(5, 8, 128).transpose(2, 1, 0))
    m["gateb"] = f(np.asarray(inp["ml_gate_b"][0]).reshape(2, 4).T)
    m["wr"] = f(np.concatenate([np.asarray(inp["moe_w_group"][0]), np.asarray(inp["moe_w_router"][0])], 1))
    rb = np.concatenate([np.asarray(inp["moe_b_group"][0]), np.asarray(inp["moe_b_router"][0])])
    m["rb"] = f(np.broadcast_to(rb[None], (128, 36)))
    for k, v in host_consts().items():
        m["c_" + k] = v
    return m


def kernel(**inputs):
    ncores = 8
    x = np.asarray(inputs["x"], dtype=np.float32)
    mem = np.asarray(inputs["mem"], dtype=np.float32)
    B = x.shape[0]
    NS = B // ncores
    NCH = x.shape[1] // 128
    key = (NS, NCH)
    if key not in _CACHE:
        _CACHE[key] = build_program(NS, NCH)[0]
    nc = _CACHE[key]
    shared = _host_layout(inputs)
    in_maps = []
    for c in range(ncores):
        m = dict(shared)
        m["x"] = np.ascontiguousarray(x[c * NS:(c + 1) * NS])
        m["mem"] = np.ascontiguousarray(mem[c * NS:(c + 1) * NS])
        in_maps.append(m)
    res = run_bass_kernel_spmd(nc, in_maps, core_ids=list(range(ncores)))
    return np.concatenate([r["out"] for r in res.results], axis=0).astype(np.float32)
```
